# Optimizing a Trainium2 kernel written in Bass

```python
import math
import jax, jax.numpy as jnp
from jax import lax
import numpy as np

D_MODEL = 1024
BATCH = 2
SEQ = 8192
DEPTH = 1

MIX_WIDTH = D_MODEL
ATTN_WIDTH = MIX_WIDTH // 2
CONV_WIDTH = MIX_WIDTH - ATTN_WIDTH
N_DIFF_HEADS = 4
DIFF_HEAD_DIM = ATTN_WIDTH // (2 * N_DIFF_HEADS)
V_HEAD_DIM = 2 * DIFF_HEAD_DIM
IN_PROJ_WIDTH = 3 * ATTN_WIDTH + 3 * CONV_WIDTH
CONV_K = 3
N_BUCKETS = 32
MAX_DISTANCE = 128
Q_BLOCK = 128
N_GROUPS = 4
EXPERTS_PER_GROUP = 4
N_EXPERTS = N_GROUPS * EXPERTS_PER_GROUP
TOP_K = 2
D_EXPERT = D_MODEL // 2
NORM_EPS = 1e-6
SUBLN_EPS = 1e-5
NEG_INF = -1e30

kernel_name = "hybrid_diffattn_shortconv_hmoe_adaln"


def rmsnorm(x, g, eps=NORM_EPS):
    xf = x.astype(jnp.float32)
    y = xf * lax.rsqrt(jnp.mean(xf * xf, axis=-1, keepdims=True) + eps) * g.astype(jnp.float32)
    return y.astype(x.dtype)


def lambda_init_fn(layer_idx):
    return 0.8 - 0.6 * math.exp(-0.3 * layer_idx)


def rel_bucket(n):
    n = jnp.maximum(n, 0)
    max_exact = N_BUCKETS // 2
    is_small = n < max_exact
    nf = jnp.maximum(n, 1).astype(jnp.float32)
    large = max_exact + (jnp.log(nf / max_exact) / math.log(MAX_DISTANCE / max_exact)
                         * (N_BUCKETS - max_exact)).astype(jnp.int32)
    large = jnp.minimum(large, N_BUCKETS - 1)
    return jnp.where(is_small, n, large)


def diff_attention(q, k, v, positions, rel_bias, lq1, lk1, lq2, lk2, subln_g, lam_init):
    B, S, _ = q.shape
    H2 = 2 * N_DIFF_HEADS
    qh = q.reshape(B, S, H2, DIFF_HEAD_DIM).transpose(0, 2, 1, 3).astype(jnp.float32)
    kh = k.reshape(B, S, H2, DIFF_HEAD_DIM).transpose(0, 2, 1, 3).astype(jnp.float32)
    vh = v.reshape(B, S, N_DIFF_HEADS, V_HEAD_DIM).transpose(0, 2, 1, 3).astype(jnp.float32)
    lam = (jnp.exp(jnp.sum(lq1.astype(jnp.float32) * lk1.astype(jnp.float32)))
           - jnp.exp(jnp.sum(lq2.astype(jnp.float32) * lk2.astype(jnp.float32))) + lam_init)
    scale = DIFF_HEAD_DIM ** -0.5
    nb = S // Q_BLOCK
    q_blk = qh.reshape(B, H2, nb, Q_BLOCK, DIFF_HEAD_DIM).transpose(2, 0, 1, 3, 4)
    pos_blk = positions.reshape(B, nb, Q_BLOCK).transpose(1, 0, 2)
    starts = jnp.arange(nb, dtype=jnp.int32) * Q_BLOCK
    kidx = jnp.arange(S, dtype=jnp.int32)
    bias_table = rel_bias.astype(jnp.float32).T

    def block(args):
        qb, pq, s0 = args
        logits = jnp.einsum('bhqd,bhkd->bhqk', qb, kh) * scale
        bucket = rel_bucket(pq[:, :, None] - positions[:, None, :])
        bias = jnp.take(bias_table, bucket, axis=1).transpose(1, 0, 2, 3)
        qidx = s0 + jnp.arange(Q_BLOCK, dtype=jnp.int32)
        causal = kidx[None, :] <= qidx[:, None]
        logits = jnp.where(causal[None, None], logits + bias, NEG_INF)
        p = jax.nn.softmax(logits, axis=-1).reshape(B, N_DIFF_HEADS, 2, Q_BLOCK, S)
        a = p[:, :, 0] - lam * p[:, :, 1]
        return jnp.einsum('bhqk,bhkv->bhqv', a, vh)

    o = lax.map(block, (q_blk, pos_blk, starts))
    o = o.transpose(1, 0, 3, 2, 4).reshape(B, S, N_DIFF_HEADS, V_HEAD_DIM)
    o = o * lax.rsqrt(jnp.mean(o * o, axis=-1, keepdims=True) + SUBLN_EPS)
    o = o * subln_g.astype(jnp.float32) * (1.0 - lam_init)
    return o.reshape(B, S, ATTN_WIDTH).astype(q.dtype)


def short_conv(gate_b, gate_c, h_in, conv_w):
    u = gate_c * h_in
    conv = lax.conv_general_dilated(
        u, conv_w[:, None, :].astype(u.dtype), window_strides=(1,), padding=[(CONV_K - 1, 0)],
        dimension_numbers=('NWC', 'WIO', 'NWC'), feature_group_count=CONV_WIDTH)
    return gate_b * conv


def hier_moe(h, rgw, rgb, rew, reb, w_gate, w_up, w_down):
    B, S, D = h.shape
    ht = h.reshape(B * S, D)
    glog = (ht @ rgw + rgb).astype(jnp.float32)
    pg = jax.nn.softmax(glog, axis=-1)
    gsel = jnp.argmax(glog, axis=-1)
    pg_sel = jnp.take_along_axis(pg, gsel[:, None], axis=1)[:, 0]
    elog = (ht @ rew + reb).astype(jnp.float32).reshape(-1, N_GROUPS, EXPERTS_PER_GROUP)
    sel = jnp.take_along_axis(elog, gsel[:, None, None], axis=1)[:, 0]
    vals, idx = lax.top_k(sel, TOP_K)
    w = jax.nn.softmax(vals, axis=-1) * pg_sel[:, None]
    eid = gsel[:, None] * EXPERTS_PER_GROUP + idx
    gates = jnp.einsum('tkn,tk->tn', jax.nn.one_hot(eid, N_EXPERTS, dtype=jnp.float32), w)
    y = jnp.zeros((B * S, D), jnp.float32)
    for e in range(N_EXPERTS):
        hid = jax.nn.silu(ht @ w_gate[e]) * (ht @ w_up[e])
        y = y + gates[:, e:e + 1] * (hid @ w_down[e]).astype(jnp.float32)
    return y.astype(h.dtype).reshape(B, S, D)


def setup_inputs(seed: int = 0) -> dict:
    key = jax.random.key(seed)
    ks = jax.random.split(key, 24)
    L, D = DEPTH, D_MODEL
    nrm = lambda k, shape, s: jax.random.normal(k, shape, jnp.float32) * s
    return {
        "x": nrm(ks[0], (BATCH, SEQ, D), 1.0),
        "c": nrm(ks[1], (BATCH, D), 1.0),
        "positions": (jnp.arange(SEQ, dtype=jnp.int32)[None, :]
                      + jax.random.randint(ks[2], (BATCH, 1), 0, 4096, dtype=jnp.int32)),
        "rel_bias": nrm(ks[3], (N_BUCKETS, 2 * N_DIFF_HEADS), 0.5),
        "ada_w": nrm(ks[4], (L, D, 6 * D), 0.5 * D ** -0.5),
        "ada_b": nrm(ks[5], (L, 6 * D), 0.02),
        "norm1_g": 1.0 + nrm(ks[6], (L, D), 0.02),
        "w_in": nrm(ks[7], (L, D, IN_PROJ_WIDTH), D ** -0.5),
        "lambda_q1": nrm(ks[8], (L, DIFF_HEAD_DIM), 0.1),
        "lambda_k1": nrm(ks[9], (L, DIFF_HEAD_DIM), 0.1),
        "lambda_q2": nrm(ks[10], (L, DIFF_HEAD_DIM), 0.1),
        "lambda_k2": nrm(ks[11], (L, DIFF_HEAD_DIM), 0.1),
        "subln_g": 1.0 + nrm(ks[12], (L, V_HEAD_DIM), 0.02),
        "conv_w": nrm(ks[13], (L, CONV_K, CONV_WIDTH), CONV_K ** -0.5),
        "w_out": nrm(ks[14], (L, MIX_WIDTH, D), MIX_WIDTH ** -0.5),
        "norm2_g": 1.0 + nrm(ks[15], (L, D), 0.02),
        "router_group_w": nrm(ks[16], (L, D, N_GROUPS), D ** -0.5),
        "router_group_b": nrm(ks[17], (L, N_GROUPS), 0.01),
        "router_expert_w": nrm(ks[18], (L, D, N_EXPERTS), D ** -0.5),
        "router_expert_b": nrm(ks[19], (L, N_EXPERTS), 0.01),
        "expert_w_gate": nrm(ks[20], (L, N_EXPERTS, D, D_EXPERT), D ** -0.5),
        "expert_w_up": nrm(ks[21], (L, N_EXPERTS, D, D_EXPERT), D ** -0.5),
        "expert_w_down": nrm(ks[22], (L, N_EXPERTS, D_EXPERT, D), D_EXPERT ** -0.5),
        "final_g": 1.0 + nrm(ks[23], (D,), 0.02),
    }


def reference(x, c, positions, rel_bias, ada_w, ada_b, norm1_g, w_in, lambda_q1, lambda_k1,
              lambda_q2, lambda_k2, subln_g, conv_w, w_out, norm2_g, router_group_w,
              router_group_b, router_expert_w, router_expert_b, expert_w_gate, expert_w_up,
              expert_w_down, final_g):
    A, C = ATTN_WIDTH, CONV_WIDTH
    split_pts = [A, 2 * A, 3 * A, 3 * A + C, 3 * A + 2 * C]
    for l in range(DEPTH):
        lam_init = lambda_init_fn(l)
        ada = jax.nn.silu(c) @ ada_w[l] + ada_b[l]
        sh1, sc1, g1, sh2, sc2, g2 = jnp.split(ada[:, None, :], 6, axis=-1)
        h = rmsnorm(x, norm1_g[l]) * (1.0 + sc1) + sh1
        proj = h @ w_in[l]
        q, k, v, gb, gc, hc = jnp.split(proj, split_pts, axis=-1)
        attn = diff_attention(q, k, v, positions, rel_bias, lambda_q1[l], lambda_k1[l],
                              lambda_q2[l], lambda_k2[l], subln_g[l], lam_init)
        conv = short_conv(gb, gc, hc, conv_w[l])
        mix = jnp.concatenate([attn, conv], axis=-1) @ w_out[l]
        x = x + g1 * mix
        h = rmsnorm(x, norm2_g[l]) * (1.0 + sc2) + sh2
        x = x + g2 * hier_moe(h, router_group_w[l], router_group_b[l], router_expert_w[l],
                              router_expert_b[l], expert_w_gate[l], expert_w_up[l],
                              expert_w_down[l])
    return rmsnorm(x, final_g)
```

```python
import contextlib
import math

import numpy as np
import concourse.bass as bass
import concourse.mybir as mybir
from concourse.bass_utils import run_bass_kernel_spmd

F32 = mybir.dt.float32
BF16 = mybir.dt.bfloat16
U8 = mybir.dt.uint8
AF = mybir.ActivationFunctionType
ALU = mybir.AluOpType
AX = mybir.AxisListType

S = 8192
DM = 1024
NOWN = 16
LAM_INIT = 0.8 - 0.6 * math.exp(0.0)
NORM_EPS = 1e-6
SUBLN_EPS = 1e-5
BIG = 1.0e30
LO = [0, 1, 2, 3, 4, 5, 6, 7, 8, 9, 10, 11, 12, 13, 14, 15, 16, 19, 21, 24, 27, 31, 35, 40, 46, 52,
      59, 67, 77, 87, 99, 113]


class Prog:
    ENGS = ("pe", "act", "dve", "pool", "sp")

    def __init__(self, n_dma_sems=48):
        self.ops = []
        self.lw = {}
        self.rd = {}
        self.n_dma_sems = n_dma_sems
        self.bar = set()
        self.dma_since = []
        self.last_eng = {}

    def op(self, eng, fn, r=(), w=(), dma=False, after=()):
        idx = len(self.ops)
        deps = set(after) | self.bar
        for b in r:
            x = self.lw.get(b)
            if x is not None:
                deps.add(x)
        for b in w:
            x = self.lw.get(b)
            if x is not None:
                deps.add(x)
            deps.update(self.rd.get(b, ()))
        for b in r:
            self.rd.setdefault(b, []).append(idx)
        for b in w:
            self.lw[b] = idx
            self.rd[b] = []
        deps.discard(idx)
        self.ops.append(dict(eng=eng, fn=fn, deps=deps, dma=dma, idx=idx))
        if dma:
            self.dma_since.append(idx)
        elif fn is not None:
            self.last_eng[eng] = idx
        return idx

    def barrier(self):
        self.bar = set(self.dma_since) | set(self.last_eng.values())
        self.dma_since = []

    def emit(self, nc, es):
        ops = self.ops
        needed = set()
        for o in ops:
            needed.update(o["deps"])
        eng_sem = {e: es.enter_context(nc.semaphore("s_" + e)) for e in self.ENGS}
        dma_sems = [es.enter_context(nc.semaphore("d%d" % i)) for i in range(self.n_dma_sems)]
        cnt = {e: 0 for e in self.ENGS}
        dcnt = [0] * self.n_dma_sems
        dma_rr = 0
        ev = {}
        for o in ops:
            o["pre"] = []
            if o["dma"]:
                k = dma_rr % self.n_dma_sems
                dma_rr += 1
                if dcnt[k] > 0:
                    o["pre"].append((("d", k), dcnt[k]))
                o["dsem"] = k
                dcnt[k] += 16
                ev[o["idx"]] = (("d", k), dcnt[k])
            elif o["idx"] in needed and o["fn"] is not None:
                cnt[o["eng"]] += 1
                ev[o["idx"]] = (("e", o["eng"]), cnt[o["eng"]])
        per_eng = {e: [] for e in self.ENGS}
        for o in ops:
            per_eng[o["eng"]].append(o)

        def semh(key):
            return eng_sem[key[1]] if key[0] == "e" else dma_sems[key[1]]

        def run(engname, e):
            waited = {}
            for o in per_eng[engname]:
                best = {}
                for key, val in o["pre"]:
                    if val > best.get(key, 0):
                        best[key] = val
                for d in o["deps"]:
                    if d in ev:
                        key, val = ev[d]
                        if val > best.get(key, 0):
                            best[key] = val
                for key, val in best.items():
                    if engname == "pe" and key == ("e", "pe"):
                        continue
                    if waited.get(key, 0) < val:
                        e.wait_ge(semh(key), val)
                        waited[key] = val
                if o["fn"] is None:
                    continue
                ins = o["fn"](e)
                if o["dma"]:
                    ins.then_inc(dma_sems[o["dsem"]], 16)
                elif o["idx"] in ev:
                    ins.then_inc(eng_sem[engname], 1)

        block = es.enter_context(nc.Block())

        @block.tensor
        def _(e):
            run("pe", e)

        @block.scalar
        def _(e):
            run("act", e)

        @block.vector
        def _(e):
            run("dve", e)

        @block.gpsimd
        def _(e):
            run("pool", e)

        @block.sync
        def _(e):
            run("sp", e)


def build_program(dbg=()):
    nc = bass.Bass("TRN2", target_bir_lowering=False)
    P = Prog()

    def DI(name, shape, dt=F32):
        return nc.dram_tensor(name, list(shape), dt, kind="ExternalInput").ap()

    xT = DI("xT", [DM, S])
    xo = DI("xo", [DM, NOWN, 130])
    vecs = DI("vecs", [128, 48])
    adab = DI("adab", [128, 48])
    ada_w = DI("ada_w", [DM, 6 * DM])
    w_in = DI("w_in", [DM, 3072])
    w_out = DI("w_out", [DM, DM])
    lam4 = DI("lam4", [1, 256])
    rw = DI("rw", [DM, 20])
    rb = DI("rb", [1, 20])
    relb = DI("relb", [32, 8])
    relb31 = DI("relb31", [8, 1])
    wg = DI("wg", [16, DM, 512])
    wu = DI("wu", [16, DM, 512])
    wd = DI("wd", [16, 512, DM])
    ident_d = DI("ident", [128, 128])
    antiid_d = DI("antiid", [128, 128])
    lo_d = DI("lo", [32, 1])
    dvec_d = DI("dvec", [1, 768])
    yT = nc.dram_tensor("yT", [DM, 2048], F32, kind="ExternalOutput").ap()
    wscr = nc.dram_tensor("wscr", [8, 768], F32).ap()
    mscr = nc.dram_tensor("mscr", [128, 5 * 1024], BF16).ap()
    dbg_out = {}

    es = contextlib.ExitStack()
    ARENA = 208896
    arena = nc.alloc_sbuf_tensor("arena", [128, ARENA], U8)

    def carve(off, shape, dt):
        n = int(np.prod(shape[1:]))
        bpe = 4 if dt == F32 else 2
        assert off % 4 == 0 and off + n * bpe <= ARENA, (off, shape)
        v = arena[0:shape[0], off:off + n * bpe].bitcast(dt)
        if len(shape) == 3:
            v = v.rearrange("p (a b) -> p a b", a=shape[1])
        elif len(shape) == 4:
            v = v.rearrange("p (a b c) -> p a b c", a=shape[1], b=shape[2])
        return v

    class Region:
        def __init__(self, lo, hi):
            self.lo, self.hi, self.cur = lo, hi, lo

        def alloc(self, shape, dt):
            n = int(np.prod(shape[1:])) * (4 if dt == F32 else 2)
            n = (n + 63) // 64 * 64
            off = self.cur
            self.cur += n
            assert self.cur <= self.hi, ("region overflow", self.cur, self.hi)
            return carve(off, shape, dt)

    K = 1024
    RC = Region(0, 8 * K)
    ident = RC.alloc([128, 128], F32)
    ones_bf = RC.alloc([128, 128], BF16)
    ones_f = RC.alloc([128, 128], F32)
    vec = RC.alloc([128, 48], F32)
    adaT = RC.alloc([128, 48], F32)
    der = RC.alloc([128, 32], F32)
    bias_q = RC.alloc([128, 4], F32)
    bias_k = RC.alloc([128, 4], F32)
    bias_c = RC.alloc([128, 12], F32)
    bias_vb = RC.alloc([128, 512], F32)
    rbias_b = RC.alloc([128, 20], F32)
    bge = RC.alloc([128, 16, 8], F32)
    CP, N1G, N2G, FG, CW, HM, SUBG = 0, 8, 16, 24, 32, 44, 45
    SH1, SC1, G1, SH2, SC2, G2 = 0, 8, 16, 24, 32, 40
    GSC1, GSC2, LAMC, NLAM, SUBS = 0, 8, 16, 17, 18

    psd = [es.enter_context(nc.psum_tensor("psd%d" % i, [128, 1024], F32)) for i in range(4)]
    psb = [psd[i // 2][:, (i % 2) * 512:(i % 2 + 1) * 512] for i in range(8)]

    def PSK(i):
        return ("ps", i)

    def dump(name, ap_, dt=F32):
        if name not in dbg:
            return
        P.barrier()
        shp = list(ap_.shape)
        d = nc.dram_tensor("dbg_" + name, shp, dt, kind="ExternalOutput").ap()
        dbg_out[name] = d
        P.op("sp", lambda e: e.dma_start(out=d, in_=ap_), dma=True)
        P.barrier()

    P.op("sp", lambda e: e.dma_start(out=ident, in_=ident_d[:, :]), w=["ident"], dma=True)
    P.op("sp", lambda e: e.dma_start(out=vec, in_=vecs[:, :]), w=["vec"], dma=True)
    P.op("sp", lambda e: e.dma_start(out=adaT, in_=adab[:, :]), w=["adaT"], dma=True)
    P.op("pool", lambda e: e.memset(ones_bf, 1.0 / 1024.0), w=["ones_bf"])
    P.op("pool", lambda e: e.memset(ones_f, 1.0), w=["ones_f"])
    P.op("act", lambda e: e.activation(out=vec[:, CP:CP + 8], in_=vec[:, CP:CP + 8], func=AF.Silu),
         r=["vec"], w=["vec"])

    RS = Region(8 * K, 200 * K)
    ada_stg = [RS.alloc([128, 8, 512], F32) for _ in range(2)]
    lamt = RS.alloc([128, 256], F32)
    lamp = RS.alloc([128, 128], F32)
    lams = RS.alloc([128, 2], F32)
    ada_src = ada_w.rearrange("(kc p) n -> p kc n", p=128)
    ada_pend = []

    def ada_flush():
        while ada_pend:
            pc, rowbuf, rowkey = ada_pend.pop(0)

            def mt(e, pc=pc, rowbuf=rowbuf):
                ins = None
                for nn in range(4):
                    col = pc * 4 + nn
                    ins = e.matmul(psb[1][:, col:col + 1], lhsT=rowbuf[0:1, nn * 128:(nn + 1) * 128], rhs=ones_f[0:1, 0:1],
                                   start=True, stop=True)
                return ins
            P.op("pe", mt, r=[rowkey, "ones_f"], w=[("ps1ada", pc)])

    def ada_piece(pc, sg, sgkey, rowbuf, rowkey):
        P.op("sp", lambda e: e.dma_start(out=sg, in_=ada_src[:, :, pc * 512:(pc + 1) * 512]), w=[sgkey], dma=True)

        def mm(e):
            ins = None
            for kc in range(8):
                ins = e.matmul(psb[0][0:1, 0:512], lhsT=vec[:, CP + kc:CP + kc + 1], rhs=sg[:, kc, :], start=(kc == 0),
                               stop=(kc == 7))
            return ins
        P.op("pe", mm, r=[sgkey, "vec"], w=["ps0row"])
        P.op("act", lambda e: e.activation(out=rowbuf[0:1, :], in_=psb[0][0:1, 0:512], func=AF.Copy), r=["ps0row"], w=[rowkey])
        ada_flush()
        ada_pend.append((pc, rowbuf, rowkey))

    ada_row = [RS.alloc([1, 512], F32) for _ in range(2)]
    for pc in range(4):
        ada_piece(pc, ada_stg[pc % 2], ("adastg", pc % 2), ada_row[pc % 2], ("adarow", pc % 2))
    ada_flush()
    for pc in []:
        sg = ada_stg[pc % 2]

        def mm(e, sg=sg, pc=pc):
            ins = None
            for nn in range(4):
                col = pc * 4 + nn
                for kc in range(8):
                    ins = e.matmul(psb[0][:, col:col + 1], lhsT=sg[:, kc, nn * 128:(nn + 1) * 128],
                                   rhs=vec[:, CP + kc:CP + kc + 1], start=(kc == 0), stop=(kc == 7))
            return ins
        P.op("pe", mm, r=[("adastg", pc % 2), "vec"], w=[PSK(0)])
    P.op("dve", lambda e: e.tensor_tensor(out=adaT[:, 0:16], in0=adaT[:, 0:16], in1=psb[1][:, 0:16], op=ALU.add),
         r=[("ps1ada", pc) for pc in range(4)] + ["adaT"], w=["adaT"])
    P.op("dve", lambda e: e.scalar_tensor_tensor(out=der[:, GSC1:GSC1 + 8], in0=adaT[:, SC1:SC1 + 8], scalar=1.0,
                                                  in1=vec[:, N1G:N1G + 8], op0=ALU.add, op1=ALU.mult),
         r=["adaT", "vec"], w=["der"])
    P.op("dve", lambda e: e.tensor_scalar(out=der[:, SUBS:SUBS + 1], in0=vec[:, SUBG:SUBG + 1],
                                          scalar1=(1.0 - LAM_INIT), scalar2=None, op0=ALU.mult),
         r=["vec", "der"], w=["der"])
    P.op("sp", lambda e: e.dma_start(out=lamt, in_=lam4[0:1, :].partition_broadcast(128)), w=["lamt"], dma=True)
    P.op("dve", lambda e: e.tensor_tensor(out=lamp.rearrange("p (a d) -> p a d", a=2),
                                          in0=lamt.rearrange("p (a b d) -> p a b d", a=2, b=2)[:, :, 0, :],
                                          in1=lamt.rearrange("p (a b d) -> p a b d", a=2, b=2)[:, :, 1, :],
                                          op=ALU.mult), r=["lamt"], w=["lamp"])
    P.op("dve", lambda e: e.tensor_reduce(out=lams, in_=lamp.rearrange("p (a d) -> p a d", a=2), axis=AX.X,
                                          op=ALU.add), r=["lamp"], w=["lams"])
    P.op("act", lambda e: e.activation(out=lams, in_=lams, func=AF.Exp), r=["lams"], w=["lams"])
    P.op("dve", lambda e: e.scalar_tensor_tensor(out=der[:, LAMC:LAMC + 1], in0=lams[:, 0:1], scalar=LAM_INIT,
                                                  in1=lams[:, 1:2], op0=ALU.add, op1=ALU.subtract),
         r=["lams", "der"], w=["der"])
    P.op("dve", lambda e: e.tensor_scalar(out=der[:, NLAM:NLAM + 1], in0=der[:, LAMC:LAMC + 1], scalar1=-1.0,
                                          scalar2=None, op0=ALU.mult), r=["der"], w=["der"])
    antiid = RC.alloc([128, 128], F32)
    dv32 = RS.alloc([32, 768], F32)
    Sm = RS.alloc([32, 768], F32)
    w8 = RS.alloc([8, 768], F32)
    c8 = RS.alloc([8, 768], F32)
    rb0 = RS.alloc([32, 8], F32)
    rb1 = RS.alloc([32, 8], F32)
    lo_t = RS.alloc([32, 1], F32)
    nb31 = RS.alloc([8, 1], F32)

    P.op("sp", lambda e: e.dma_start(out=antiid, in_=antiid_d[:, :]), w=["antiid"], dma=True)
    P.op("sp", lambda e: e.dma_start(out=dv32, in_=dvec_d[0:1, :].partition_broadcast(32)), w=["dv32"], dma=True)
    P.op("sp", lambda e: e.dma_start(out=lo_t, in_=lo_d[:, :]), w=["lo_t"], dma=True)
    P.op("sp", lambda e: e.dma_start(out=rb0, in_=relb[:, :]), w=["rb0"], dma=True)
    P.op("pool", lambda e: e.memset(rb1, 0.0), w=["rb1"])
    P.op("sp", lambda e: e.dma_start(out=rb1[1:32, :], in_=relb[0:31, :]), w=["rb1"], dma=True)
    P.op("sp", lambda e: e.dma_start(out=nb31, in_=relb31[:, :]), w=["nb31"], dma=True)
    P.op("dve", lambda e: e.tensor_scalar(out=nb31, in0=nb31, scalar1=-1.0, scalar2=None, op0=ALU.mult),
         r=["nb31"], w=["nb31"])
    P.op("dve", lambda e: e.tensor_tensor(out=rb0, in0=rb0, in1=rb1, op=ALU.subtract), r=["rb0", "rb1"], w=["rb0"])
    P.op("dve", lambda e: e.tensor_scalar(out=Sm, in0=dv32, scalar1=lo_t[:, 0:1], scalar2=None, op0=ALU.is_ge),
         r=["dv32", "lo_t"], w=["Sm"])
    P.op("dve", lambda e: e.tensor_scalar(out=c8, in0=dv32[0:8, :], scalar1=0.0, scalar2=None, op0=ALU.is_ge),
         r=["dv32"], w=["c8"])

    def mmw(e):
        e.matmul(psb[2][0:8, 0:512], lhsT=rb0[:, :], rhs=Sm[:, 0:512], start=True, stop=True)
        return e.matmul(psb[3][0:8, 0:256], lhsT=rb0[:, :], rhs=Sm[:, 512:768], start=True, stop=True)
    P.op("pe", mmw, r=["rb0", "Sm"], w=[PSK(2), PSK(3)])
    P.op("act", lambda e: e.activation(out=w8[:, 0:512], in_=psb[2][0:8, 0:512], func=AF.Exp, bias=nb31[:, 0:1], scale=1.0),
         r=[PSK(2), "nb31"], w=["w8a"])
    P.op("act", lambda e: e.activation(out=w8[:, 512:768], in_=psb[3][0:8, 0:256], func=AF.Exp, bias=nb31[:, 0:1], scale=1.0),
         r=[PSK(3), "nb31"], w=["w8b"])
    P.op("dve", lambda e: e.tensor_tensor(out=w8, in0=w8, in1=c8, op=ALU.mult), r=["w8a", "w8b", "c8"], w=["w8"])
    P.op("sp", lambda e: e.dma_start(out=wscr[:, :], in_=w8), r=["w8"], w=["wscr"], dma=True)
    dump("adaT", adaT)
    dump("der", der)
    P.barrier()

    KT = carve(8 * K, [128, 4, S], BF16)
    VA = carve(72 * K, [128, 64, 4, 129], BF16)
    QOFF = 72 * K + 66048 + 512
    qT = carve(QOFF, [128, 4, 2048], BF16)
    RA0 = QOFF + 16 * K

    w_in_src = w_in.rearrange("(fc p) n -> p fc n", p=128)

    def norm_stage(tag, xs, n, sq, rs, hn, sq_eng="pool"):
        if sq_eng == "pool":
            P.op("pool", lambda e: e.tensor_tensor(out=sq, in0=xs, in1=xs, op=ALU.mult),
                 r=[(tag, "xs")], w=[(tag, "sq")])
        else:
            P.op("act", lambda e: e.activation(out=sq, in_=xs, func=AF.Square), r=[(tag, "xs")], w=[(tag, "sq")])

    RW = Region(RA0, 200 * K)
    Wkv = RW.alloc([128, 8, 1024], BF16)
    WKV_END = RW.cur
    Wq = RW.alloc([128, 8, 512], BF16)
    shrep = carve(8 * K, [128, 8, 128], F32)
    wstg = [carve(16 * K + i * 16 * K, [128, 8, 512], F32) for i in range(2)]

    def make_rep(dst, col0, key):
        for fc in range(8):
            P.op("dve", lambda e, fc=fc: e.tensor_scalar(out=dst[:, fc, :], in0=ones_f,
                                                          scalar1=adaT[:, col0 + fc:col0 + fc + 1], scalar2=None,
                                                          op0=ALU.mult), r=["ones_f", "adaT"], w=[(key, fc)])

    make_rep(shrep, SH1, "shrep")
    brow_s = [carve(48 * K + k_ * 2 * K, [1, 512], F32) for k_ in range(2)]

    def cast_piece(stg, stgkey, dst, dcol, gcol, engs=("dve", "act"), width=512):
        for fc in range(8):
            eng = engs[fc % len(engs)]
            if eng == "act":
                P.op("act", lambda e, fc=fc: e.activation(out=dst[:, fc, dcol:dcol + width], in_=stg[:, fc, 0:width],
                                                          func=AF.Identity, scale=gcol[:, fc:fc + 1]),
                     r=[stgkey, "der"], w=[("wdst", id(dst), fc, dcol)])
            else:
                P.op(eng, lambda e, fc=fc: e.tensor_scalar(out=dst[:, fc, dcol:dcol + width], in0=stg[:, fc, 0:width],
                                                           scalar1=gcol[:, fc:fc + 1], scalar2=None, op0=ALU.mult),
                     r=[stgkey, "der"], w=[("wdst", id(dst), fc, dcol)])

    def bias_pp(stg, stgkey, shcol, bias_dst, bcol0, psi, bkey, brow, psi2=None, width=512):
        psi2 = psi if psi2 is None else psi2
        nch = width // 128

        def mm(e):
            ins = None
            for fc in range(8):
                ins = e.matmul(psb[psi][0:1, 0:width], lhsT=adaT[:, shcol + fc:shcol + fc + 1], rhs=stg[:, fc, 0:width],
                               start=(fc == 0), stop=(fc == 7))
            return ins
        P.op("pe", mm, r=[stgkey, "adaT"], w=[PSK(psi)])
        P.op("act", lambda e: e.activation(out=brow[0:1, 0:width], in_=psb[psi][0:1, 0:width], func=AF.Copy), r=[PSK(psi)],
             w=[("brow", id(brow))])

        def mt(e):
            ins = None
            for mc in range(nch):
                ins = e.matmul(psb[psi2][:, mc:mc + 1], lhsT=brow[0:1, mc * 128:(mc + 1) * 128], rhs=ones_f[0:1, 0:1],
                               start=True, stop=True)
            return ins
        P.op("pe", mt, r=[("brow", id(brow)), "ones_f"], w=[PSK(psi2)])
        P.op("act", lambda e: e.activation(out=bias_dst[:, bcol0:bcol0 + nch], in_=psb[psi2][:, 0:nch], func=AF.Copy),
             r=[PSK(psi2)], w=[bkey])

    def bias_bc(stg, stgkey, rep, repkey, ncols, dst, psi, bkey):
        def mm(e):
            ins = None
            for fc in range(8):
                ins = e.matmul(psb[psi][:, 0:ncols], lhsT=rep[:, fc, :], rhs=stg[:, fc, 0:ncols], start=(fc == 0),
                               stop=(fc == 7))
            return ins
        P.op("pe", mm, r=[stgkey] + [(repkey, fc) for fc in range(8)], w=[PSK(psi)])
        P.op("act", lambda e: e.activation(out=dst, in_=psb[psi][:, 0:ncols], func=AF.Copy), r=[PSK(psi)], w=[bkey])

    for pi, (c0, dst, dcol) in enumerate([(0, Wq, 0), (512, Wkv, 0), (1024, Wkv, 512)]):
        sg = wstg[pi % 2]
        sk = ("wstg", pi % 2)
        P.op("sp", lambda e, sg=sg, c0=c0: e.dma_start(out=sg, in_=w_in_src[:, :, c0:c0 + 512]), w=[sk], dma=True)
        cast_piece(sg, sk, dst, dcol, der[:, GSC1:GSC1 + 8])
        if pi == 0:
            bias_pp(sg, sk, SH1, bias_q, 0, 2, "bias_q", brow_s[0], 4)
        elif pi == 1:
            bias_pp(sg, sk, SH1, bias_k, 0, 3, "bias_k", brow_s[1], 5)
        else:
            bias_bc(sg, sk, shrep, "shrep", 512, bias_vb, 6, "bias_vb")
    dump("bias_q", bias_q)
    dump("bias_vb", bias_vb)
    dump("Wq", Wq, BF16)

    RAO = Region(RW.cur, 200 * K)
    xos = [RAO.alloc([128, 8, 130], F32) for _ in range(2)]
    sqo = [RAO.alloc([128, 8, 130], BF16) for _ in range(2)]
    rso = [RAO.alloc([128, 130], F32) for _ in range(2)]
    hno = [RAO.alloc([128, 8, 130], BF16) for _ in range(2)]
    xo_src = xo.rearrange("(fc p) i t -> p fc i t", p=128)

    def own_norm(i, xs_t, sq_t, rs_t, hn_t, psi, hnkey=None):
        b = i % 2
        hnkey = ("hno", b) if hnkey is None else hnkey
        P.op("sp", lambda e: e.dma_start(out=xs_t, in_=xo_src[:, :, i, :]), w=[("xos", b)], dma=True)
        P.op("act", lambda e: e.activation(out=sq_t, in_=xs_t, func=AF.Square), r=[("xos", b)], w=[("sqo", b)])

        def mm(e):
            ins = None
            for fc in range(8):
                ins = e.matmul(psb[psi][:, 0:130], lhsT=ones_bf, rhs=sq_t[:, fc, :], start=(fc == 0), stop=(fc == 7))
            return ins
        P.op("pe", mm, r=[("sqo", b), "ones_bf"], w=[PSK(psi)])
        P.op("act", lambda e: e.activation(out=rs_t, in_=psb[psi][:, 0:130], func=AF.Sqrt, bias=NORM_EPS, scale=1.0),
             r=[PSK(psi)], w=[("rso", b)])
        P.op("dve", lambda e: e.reciprocal(out=rs_t, in_=rs_t), r=[("rso", b)], w=[("rso", b)])
        P.op("dve", lambda e: e.tensor_tensor(out=hn_t, in0=xs_t, in1=rs_t.unsqueeze(1).to_broadcast([128, 8, 130]),
                                              op=ALU.mult), r=[("xos", b), ("rso", b)], w=[hnkey])

    ada_stg2 = [carve(72 * K + k_ * 16 * K, [128, 8, 512], F32) for k_ in range(2)]
    ada_row2 = [carve(72 * K + 32 * K + k_ * 2 * K, [1, 512], F32) for k_ in range(2)]

    def q_proj(i):
        b = i % 2
        pq = 6 + b

        def mmq(e):
            ins = None
            for mc in range(4):
                for fc in range(8):
                    ins = e.matmul(psb[pq][:, mc * 128:(mc + 1) * 128], lhsT=Wq[:, fc, mc * 128:(mc + 1) * 128],
                                   rhs=hno[b][:, fc, 2:130], start=(fc == 0), stop=(fc == 7))
            return ins
        P.op("pe", mmq, r=[("hno", b)] + [("wdst", id(Wq), fc, 0) for fc in range(8)], w=[PSK(pq)])
        for mc in range(4):
            P.op("act", lambda e, mc=mc: e.activation(
                out=qT[:, mc, i * 128:(i + 1) * 128], in_=psb[pq][:, mc * 128:(mc + 1) * 128], func=AF.Identity,
                bias=bias_q[:, mc:mc + 1], scale=1.0), r=[PSK(pq), "bias_q"], w=[("qT", i, mc)])

    Hs_l = [carve(72 * K + 40 * K + k_ * 4 * K, [128, 8, 128], F32) for k_ in range(2)]
    Mt_stage = carve(72 * K + 48 * K, [128, 5, 1024], BF16)

    def mtile(sp_):
        off = 512 - sp_ * 128
        src = bass.AP(wscr.tensor, off, [[1, 128], [768, 8], [1, 128]])
        Hs = Hs_l[sp_ % 2]
        hk = ("Hs", sp_ % 2)
        P.op("sp", lambda e: e.dma_start(out=Hs, in_=src), w=[hk], dma=True)

        def mmf(e):
            hs2 = Hs.rearrange("p m q -> p (m q)")
            e.matmul(psb[2][:, :], lhsT=antiid, rhs=hs2[:, 0:512], start=True, stop=True)
            return e.matmul(psb[3][:, :], lhsT=antiid, rhs=hs2[:, 512:1024], start=True, stop=True)
        P.op("pe", mmf, r=[hk, "antiid"], w=[PSK(2), PSK(3)])
        for hh in range(2):
            P.op("dve", lambda e, hh=hh: e.tensor_copy(
                out=Mt_stage[:, sp_, :].rearrange("p (c h q) -> p c h q", c=2, h=4)[:, :, 2 * hh:2 * hh + 2, :],
                in_=psb[2 + hh][:, :].rearrange("p (h c q) -> p c h q", h=2, c=2)),
                r=[PSK(2 + hh)], w=[("Mts", sp_, hh)])

    own_norm(0, xos[0], sqo[0], rso[0], hno[0], 4)
    for i in range(NOWN):
        if i + 1 < NOWN:
            b1 = (i + 1) % 2
            own_norm(i + 1, xos[b1], sqo[b1], rso[b1], hno[b1], 4 + b1)
        q_proj(i)
        if i % 3 == 1 and i // 3 < 5:
            mtile(i // 3)
        if i == 14:
            P.op("sp", lambda e: e.dma_start(out=mscr.rearrange("p (s x) -> p s x", s=5), in_=Mt_stage),
                 r=[("Mts", sp_, hh) for sp_ in range(5) for hh in range(2)], w=["mscr"], dma=True)
        if i % 2 == 0:
            pc = 4 + i // 2
            ada_piece(pc, ada_stg2[pc % 2], ("adastg2", pc % 2), ada_row2[pc % 2], ("adarow2", pc % 2))
    ada_flush()
    P.op("dve", lambda e: e.tensor_tensor(out=adaT[:, 16:48], in0=adaT[:, 16:48], in1=psb[1][:, 16:48], op=ALU.add),
         r=[("ps1ada", pc) for pc in range(4, 12)] + ["adaT"], w=["adaT"])
    P.op("dve", lambda e: e.scalar_tensor_tensor(out=der[:, GSC2:GSC2 + 8], in0=adaT[:, SC2:SC2 + 8], scalar=1.0,
                                                  in1=vec[:, N2G:N2G + 8], op0=ALU.add, op1=ALU.mult),
         r=["adaT", "vec", "der"], w=["der"])
    dump("qT", qT, BF16)
    P.barrier()

    RAK = Region(WKV_END, 200 * K)
    NCH = 32
    xs = [RAK.alloc([128, 8, 256], F32) for _ in range(2)]
    sqk1 = RAK.alloc([128, 8, 256], BF16)
    sqk = [sqk1, sqk1]
    rsk = [RAK.alloc([128, 256], F32) for _ in range(2)]
    hnk = [RAK.alloc([128, 8, 256], BF16) for _ in range(2)]
    xT_src = xT.rearrange("(fc p) t -> p fc t", p=128)
    P.op("pool", lambda e: e.memset(VA[:, :, :, 128:129], 1.0), w=["VAones"])

    def kv_load(c):
        b = c % 2
        P.op("sp", lambda e: e.dma_start(out=xs[b], in_=xT_src[:, :, c * 256:(c + 1) * 256]), w=[("xs", b)], dma=True)

    def kv_norm(c):
        b = c % 2
        P.op("act", lambda e: e.activation(out=sqk[b], in_=xs[b], func=AF.Square), r=[("xs", b)], w=["sqk"])

        def mm(e):
            ins = None
            for fc in range(8):
                ins = e.matmul(psb[b][:, 0:256], lhsT=ones_bf, rhs=sqk[b][:, fc, :], start=(fc == 0), stop=(fc == 7))
            return ins
        P.op("pe", mm, r=["sqk", "ones_bf"], w=[PSK(b)])
        P.op("act", lambda e: e.activation(out=rsk[b], in_=psb[b][:, 0:256], func=AF.Sqrt, bias=NORM_EPS, scale=1.0),
             r=[PSK(b)], w=[("rsk", b)])
        P.op("dve", lambda e: e.reciprocal(out=rsk[b], in_=rsk[b]), r=[("rsk", b)], w=[("rsk", b)])
        P.op("dve", lambda e: e.tensor_tensor(out=hnk[b], in0=xs[b], in1=rsk[b].unsqueeze(1).to_broadcast([128, 8, 256]),
                                              op=ALU.mult), r=[("xs", b), ("rsk", b)], w=[("hnk", b)])

    def kv_proj(c):
        b = c % 2
        for half in range(2):
            psi = 2 + half

            def mmk(e, half=half, psi=psi):
                ins = None
                for m2 in range(2):
                    mc = half * 2 + m2
                    for fc in range(8):
                        ins = e.matmul(psb[psi][:, m2 * 256:(m2 + 1) * 256], lhsT=Wkv[:, fc, mc * 128:(mc + 1) * 128],
                                       rhs=hnk[b][:, fc, :], start=(fc == 0), stop=(fc == 7))
                return ins
            P.op("pe", mmk, r=[("hnk", b)], w=[PSK(psi)])
            for m2 in range(2):
                mc = half * 2 + m2
                P.op("act", lambda e, mc=mc, m2=m2, psi=psi: e.activation(
                    out=KT[:, mc, c * 256:(c + 1) * 256], in_=psb[psi][:, m2 * 256:(m2 + 1) * 256], func=AF.Identity,
                    bias=bias_k[:, mc:mc + 1], scale=1.0), r=[PSK(psi), "bias_k"], w=[("KT", c, mc)])
        for tt in range(2):
            psi = 4 + tt

            def mmv(e, tt=tt, psi=psi):
                ins = None
                for fc in range(8):
                    ins = e.matmul(psb[psi][:, :], lhsT=hnk[b][:, fc, tt * 128:(tt + 1) * 128], rhs=Wkv[:, fc, 512:1024],
                                   start=(fc == 0), stop=(fc == 7))
                return ins
            P.op("pe", mmv, r=[("hnk", b)], w=[PSK(psi)])
            P.op("dve", lambda e, tt=tt, psi=psi: e.tensor_tensor(
                out=VA[:, 2 * c + tt, :, 0:128], in0=psb[psi][:, :].rearrange("p (h d) -> p h d", h=4),
                in1=bias_vb.rearrange("p (h d) -> p h d", h=4), op=ALU.add),
                r=[PSK(psi), "bias_vb"], w=[("VA", c, tt)])

    kv_load(0)
    kv_load(1)
    kv_norm(0)
    for c in range(NCH):
        if c + 2 < NCH:
            pass
        if c + 1 < NCH:
            kv_norm(c + 1)
        kv_proj(c)
        if c + 2 < NCH:
            kv_load(c + 2)
    dump("KT", KT, BF16)
    dump("VA", VA, BF16)
    P.barrier()

    RB = Region(RA0, 200 * K)
    attnT = RB.alloc([128, 4, 2048], BF16)
    Mt = RB.alloc([128, 5, 1024], BF16)
    UB0 = RB.cur
    Pt = [RB.alloc([128, 1024], BF16) for _ in range(3)]
    osb = RB.alloc([128, 8, 129], F32)
    att_l = [RB.alloc([128, 4, 128], F32) for _ in range(2)]
    att2_l = [RB.alloc([128, 4, 128], F32) for _ in range(2)]
    rz = RB.alloc([128, 8], F32)
    ssq_l = [RB.alloc([128, 4], F32) for _ in range(2)]
    RB = Region(UB0, 200 * K)
    P.op("sp", lambda e: e.dma_start(out=Mt, in_=mscr.rearrange("p (s x) -> p s x", s=5)), w=["Mt"], dma=True)
    dump("Mt", Mt, BF16)
    dump("w8", w8)
    OB = [5, 6, 7]
    TB = 4

    def omap(m):
        return psb[OB[m // 3]][:, (m % 3) * 129:(m % 3) * 129 + 129]

    pairs = []
    for i in range(NOWN):
        nk = 4 * i + 4
        for kb in range(nk):
            pairs.append((i, kb, kb == 0, kb == nk - 1))
    NP = len(pairs)
    osf = osb.rearrange("p m d -> p (m d)")
    OSK = ["osb0", "osb1", "osb2"]

    def stage_qk(n):
        i, kb, first, last = pairs[n]
        sset = n % 2
        pb = n % 3
        sA, sB = psb[2 * sset], psb[2 * sset + 1]
        gen = kb - (4 * i - 1)

        def mmqk(e):
            ins = None
            for h in range(4):
                e.matmul(sA[:, h * 128:(h + 1) * 128], lhsT=KT[0:64, h, kb * 128:(kb + 1) * 128],
                         rhs=qT[0:64, h, i * 128:(i + 1) * 128], start=True, stop=True)
                ins = e.matmul(sB[:, h * 128:(h + 1) * 128], lhsT=KT[64:128, h, kb * 128:(kb + 1) * 128],
                               rhs=qT[64:128, h, i * 128:(i + 1) * 128], start=True, stop=True)
            return ins
        P.op("pe", mmqk, r=[], w=[PSK(2 * sset), PSK(2 * sset + 1)])
        P.op("act", lambda e: e.activation(out=Pt[pb], in_=psd[sset][:, :], func=AF.Exp, scale=0.125),
             r=[PSK(2 * sset), PSK(2 * sset + 1)], w=[("Pt", pb, 0), ("Pt", pb, 1)])
        if gen >= 0:
            P.op("dve", lambda e: e.tensor_tensor(out=Pt[pb], in0=Pt[pb], in1=Mt[:, gen, :], op=ALU.mult),
                 r=[("Pt", pb, 0), ("Pt", pb, 1), "Mt"], w=[("Pt", pb, 0), ("Pt", pb, 1)])

    def stage_pv(n):
        i, kb, first, last = pairs[n]
        pb = n % 3
        par = i % 2
        att, att2, ssq = att_l[par], att2_l[par], ssq_l[par]
        ATK = [("att", par, h) for h in range(4)]

        def mmpv(e):
            ins = None
            for h in range(4):
                for c in range(2):
                    m = 2 * h + c
                    ins = e.matmul(omap(m), lhsT=Pt[pb][:, c * 512 + h * 128:c * 512 + (h + 1) * 128],
                                   rhs=VA[:, kb, h, :], start=(first and m % 3 == 0), stop=last,
                                   skip_group_check=True)
            return ins
        P.op("pe", mmpv, r=[("Pt", pb, 0), ("Pt", pb, 1)], w=[PSK(5), PSK(6), PSK(7)])
        if last:
            P.op("dve", lambda e: e.tensor_copy(out=osf[:, 0:387], in_=psb[5][:, 0:387]), r=[PSK(5)], w=["osb0"])
            P.op("dve", lambda e: e.tensor_copy(out=osf[:, 387:774], in_=psb[6][:, 0:387]), r=[PSK(6)], w=["osb1"])
            P.op("dve", lambda e: e.tensor_copy(out=osf[:, 774:1032], in_=psb[7][:, 0:258]), r=[PSK(7)], w=["osb2"])
            P.op("dve", lambda e: e.reciprocal(out=rz, in_=osb[:, :, 128]), r=OSK, w=["rz"])
            P.op("dve", lambda e: e.tensor_scalar(out=rz.rearrange("p (h c) -> p h c", c=2)[:, :, 1],
                                                  in0=rz.rearrange("p (h c) -> p h c", c=2)[:, :, 1],
                                                  scalar1=der[:, NLAM:NLAM + 1], scalar2=None, op0=ALU.mult),
                 r=["rz", "der"], w=["rz"])
            for h in range(4):
                P.op("dve", lambda e, h=h: e.tensor_scalar(out=att[:, h, :], in0=osb[:, 2 * h, 0:128],
                                                           scalar1=rz[:, 2 * h:2 * h + 1], scalar2=None, op0=ALU.mult),
                     r=OSK + ["rz"], w=[("att", par, h)])
                P.op("dve", lambda e, h=h: e.scalar_tensor_tensor(out=att[:, h, :], in0=osb[:, 2 * h + 1, 0:128],
                                                                  scalar=rz[:, 2 * h + 1:2 * h + 2], in1=att[:, h, :],
                                                                  op0=ALU.mult, op1=ALU.add),
                     r=OSK + ["rz", ("att", par, h)], w=[("att", par, h)])
            P.op("dve", lambda e: e.tensor_tensor(out=att2, in0=att, in1=att, op=ALU.mult), r=ATK, w=[("att2", par)])
            P.op("dve", lambda e: e.tensor_reduce(out=ssq, in_=att2, axis=AX.X, op=ALU.add), r=[("att2", par)], w=[("ssq", par)])

    def post_a(i):
        par = i % 2
        att, att2, ssq = att_l[par], att2_l[par], ssq_l[par]
        ATK = [("att", par, h) for h in range(4)]
        P.op("act", lambda e: e.activation(out=ssq, in_=ssq, func=AF.Ln, bias=SUBLN_EPS, scale=1.0 / 128.0),
             r=[("ssq", par)], w=[("ssq", par)])
        P.op("act", lambda e: e.activation(out=ssq, in_=ssq, func=AF.Exp, scale=-0.5),
             r=[("ssq", par)], w=[("ssq", par)])
        P.op("dve", lambda e: e.tensor_tensor(out=att2, in0=att, in1=ssq.unsqueeze(2).to_broadcast([128, 4, 128]),
                                              op=ALU.mult), r=ATK + [("ssq", par), ("att2", par)], w=[("att2", par)])

    def post_b(i):
        par = i % 2
        att2 = att2_l[par]
        def mmt(e):
            ins = None
            for h in range(4):
                ins = e.transpose(psb[TB][:, h * 128:(h + 1) * 128], att2[:, h, :], ident)
            return ins
        P.op("pe", mmt, r=[("att2", par), "ident"], w=[PSK(TB)])
        P.op("dve", lambda e: e.tensor_copy(out=attnT[:, :, i * 128:(i + 1) * 128],
                                            in_=psb[TB][:, :].rearrange("p (h q) -> p h q", h=4)),
             r=[PSK(TB)], w=[("attnT", i)])

    sched = {}
    for n in range(NP):
        i, kb, first, last = pairs[n]
        if last:
            sched.setdefault(n + 2 + 8, []).append(("a", i))
            sched.setdefault(n + 2 + 12, []).append(("b", i))
    PVLAG = 2
    for n in range(NP + PVLAG):
        if n < NP:
            stage_qk(n)
        if n >= PVLAG:
            stage_pv(n - PVLAG)
        for kind, i in sched.pop(n, []):
            (post_a if kind == "a" else post_b)(i)
    for n in sorted(sched):
        for kind, i in sched[n]:
            (post_a if kind == "a" else post_b)(i)
    P.barrier()

    dump("attnT", attnT, BF16)
    P.barrier()

    RC1 = Region(8 * K, RA0)
    x2T = RC1.alloc([128, 8, 2048], F32)
    convT = RC1.alloc([128, 4, 2048], BF16)
    Wc = RC1.alloc([128, 8, 1536], BF16)
    Wo = RC1.alloc([128, 8, 1024], BF16)
    cstg = [RC1.alloc([128, 8, 512], F32) for _ in range(1)]
    RC2 = Region(RA0 + 16 * K, ARENA)
    xos2 = [RC2.alloc([128, 8, 130], F32) for _ in range(2)]
    sqo2 = [RC2.alloc([128, 8, 130], BF16) for _ in range(2)]
    rso2 = [RC2.alloc([128, 130], F32) for _ in range(2)]
    hnp = [RC2.alloc([128, 8, 2, 130], BF16) for _ in range(2)]
    gcs = [RC2.alloc([128, 2, 130], F32) for _ in range(2)]
    ut = [RC2.alloc([128, 2, 130], F32) for _ in range(2)]
    tt_ = [RC2.alloc([128, 2, 128], F32) for _ in range(2)]
    hcs = [RC2.alloc([128, 2, 130], F32) for _ in range(2)]
    brow_c = RC2.alloc([1, 512], F32)
    w_out_src = w_out.rearrange("(fc p) n -> p fc n", p=128)

    def load_x2T(fcs):
        for fc in fcs:
            P.op("sp", lambda e, fc=fc: e.dma_start(out=x2T[:, fc, :].rearrange("p (i t) -> p i t", i=NOWN),
                                                    in_=xo_src[:, fc, :, 2:130]), w=[("x2T", fc)], dma=True)

    def load_wo(pi):
        sg = cstg[0]
        sk = ("cstg", 0)
        P.op("sp", lambda e: e.dma_start(out=sg, in_=w_out_src[:, :, pi * 512:(pi + 1) * 512]),
             w=[sk, ("cstgh", 0), ("cstgh", 1)], dma=True)
        for fc in range(8):
            dst = Wo[:, fc, pi * 512:(pi + 1) * 512]
            if fc < 4:
                if fc % 2 == 0:
                    P.op("dve", lambda e, fc=fc, dst=dst: e.tensor_scalar(out=dst, in0=sg[:, fc, :], scalar1=der[:, SUBS:SUBS + 1],
                                                                          scalar2=None, op0=ALU.mult),
                         r=[sk, "der"], w=[("Wo", fc, pi)])
                else:
                    P.op("act", lambda e, fc=fc, dst=dst: e.activation(out=dst, in_=sg[:, fc, :], func=AF.Identity,
                                                                       scale=der[:, SUBS:SUBS + 1]),
                         r=[sk, "der"], w=[("Wo", fc, pi)])
            else:
                if fc % 2 == 0:
                    P.op("dve", lambda e, fc=fc, dst=dst: e.tensor_copy(out=dst, in_=sg[:, fc, :]), r=[sk], w=[("Wo", fc, pi)])
                else:
                    P.op("act", lambda e, fc=fc, dst=dst: e.activation(out=dst, in_=sg[:, fc, :], func=AF.Copy),
                         r=[sk], w=[("Wo", fc, pi)])

    for hp in range(6):
        sgv = cstg[0][:, :, (hp % 2) * 256:(hp % 2) * 256 + 256]
        sk = ("cstgh", hp % 2)
        P.op("sp", lambda e, sgv=sgv, hp=hp: e.dma_start(out=sgv, in_=w_in_src[:, :, 1536 + hp * 256:1536 + (hp + 1) * 256]),
             w=[sk], dma=True)
        cast_piece(sgv, sk, Wc, hp * 256, der[:, GSC1:GSC1 + 8], width=256)
        bias_pp(sgv, sk, SH1, bias_c, hp * 2, 6, ("bias_c", hp), brow_c, 7, width=256)
    BCK = [("bias_c", hp) for hp in range(6)]
    WCK = [("wdst", id(Wc), fc, hp * 256) for fc in range(8) for hp in range(6)]
    def conv_norm(i):
        b = i % 2
        pp = (i // 2) % 2
        own_norm(i, xos2[b], sqo2[b], rso2[b], hnp[pp][:, :, i % 2, :], b, hnkey=("hnp", pp, i % 2))

    conv_norm(0)
    conv_norm(1)
    for p_ in range(NOWN // 2):
        pp = p_ % 2
        if p_ + 1 < NOWN // 2:
            conv_norm(2 * p_ + 2)
            conv_norm(2 * p_ + 3)
        if 1 <= p_ <= 4:
            load_x2T([2 * (p_ - 1), 2 * (p_ - 1) + 1])
        if p_ == 1:
            load_wo(0)
        if p_ == 3:
            load_wo(1)
        for cc in range(4):
            g = cc % 2
            bks = (2, 3, 4) if g == 0 else (5, 6, 7)

            def mmc(e, pp=pp, cc=cc, bks=bks):
                ins = None
                for br in range(3):
                    for fc in range(8):
                        ins = e.matmul(psb[bks[br]][:, 0:260], lhsT=Wc[:, fc, br * 512 + cc * 128:br * 512 + (cc + 1) * 128],
                                       rhs=hnp[pp][:, fc, :, :], start=(fc == 0), stop=(fc == 7))
                return ins
            P.op("pe", mmc, r=[("hnp", pp, 0), ("hnp", pp, 1)] + WCK, w=[PSK(bk) for bk in bks])

            def v3(ap_):
                return ap_.rearrange("p (j t) -> p j t", j=2)
            P.op("act", lambda e, g=g, bks=bks, cc=cc: e.activation(out=gcs[g], in_=v3(psb[bks[1]][:, 0:260]), func=AF.Identity,
                                                                    bias=bias_c[:, 4 + cc:5 + cc], scale=1.0),
                 r=[PSK(bks[1])] + BCK, w=[("gcs", g)])
            P.op("act", lambda e, g=g, bks=bks, cc=cc: e.activation(out=hcs[g], in_=v3(psb[bks[2]][:, 0:260]), func=AF.Identity,
                                                                    bias=bias_c[:, 8 + cc:9 + cc], scale=1.0),
                 r=[PSK(bks[2])] + BCK, w=[("hcs", g)])
            P.op("pool", lambda e, g=g: e.tensor_tensor(out=ut[g], in0=gcs[g], in1=hcs[g], op=ALU.mult),
                 r=[("gcs", g), ("hcs", g)], w=[("ut", g)])
            if p_ == 0:
                P.op("dve", lambda e, g=g: e.tensor_scalar(out=ut[g][:, 0, 0:2], in0=ut[g][:, 0, 0:2], scalar1=vec[:, HM:HM + 1],
                                                           scalar2=None, op0=ALU.mult), r=[("ut", g), "vec"], w=[("ut", g)])
            P.op("act", lambda e, g=g, cc=cc: e.activation(out=tt_[g], in_=ut[g][:, :, 2:130], func=AF.Identity,
                                                           scale=vec[:, CW + cc * 3 + 2:CW + cc * 3 + 3]),
                 r=[("ut", g), "vec"], w=[("tt", g)])
            P.op("dve", lambda e, g=g, cc=cc: e.scalar_tensor_tensor(out=tt_[g], in0=ut[g][:, :, 1:129],
                                                                      scalar=vec[:, CW + cc * 3 + 1:CW + cc * 3 + 2],
                                                                      in1=tt_[g], op0=ALU.mult, op1=ALU.add),
                 r=[("ut", g), "vec", ("tt", g)], w=[("tt", g)])
            P.op("dve", lambda e, g=g, cc=cc: e.scalar_tensor_tensor(out=tt_[g], in0=ut[g][:, :, 0:128],
                                                                      scalar=vec[:, CW + cc * 3:CW + cc * 3 + 1],
                                                                      in1=tt_[g], op0=ALU.mult, op1=ALU.add),
                 r=[("ut", g), "vec", ("tt", g)], w=[("tt", g)])
            for j2 in range(2):
                P.op("dve", lambda e, g=g, cc=cc, bks=bks, p_=p_, j2=j2: e.scalar_tensor_tensor(
                    out=convT[:, cc, (2 * p_ + j2) * 128:(2 * p_ + j2 + 1) * 128],
                    in0=psb[bks[0]][:, j2 * 130 + 2:j2 * 130 + 130], scalar=bias_c[:, cc:cc + 1], in1=tt_[g][:, j2, :],
                    op0=ALU.add, op1=ALU.mult), r=[PSK(bks[0]), ("tt", g)] + BCK, w=[("convT", 2 * p_ + j2, cc)])

    dump("convT", convT, BF16)
    for t in range(4):
        tsl = slice(t * 512, (t + 1) * 512)
        for dc in range(8):
            psi = 6 + (dc % 2)

            def mmo(e, dc=dc, psi=psi, tsl=tsl):
                ins = None
                for fc in range(8):
                    rhs = attnT[:, fc, tsl] if fc < 4 else convT[:, fc - 4, tsl]
                    ins = e.matmul(psb[psi][:, :], lhsT=Wo[:, fc, dc * 128:(dc + 1) * 128], rhs=rhs, start=(fc == 0),
                                   stop=(fc == 7))
                return ins
            P.op("pe", mmo, r=[("attnT", ii) for ii in range(4 * t, 4 * t + 4)] + [("convT", ii, cc) for ii in range(4 * t, 4 * t + 4) for cc in range(4)]
                 + [("Wo", fc, dc // 4) for fc in range(8)], w=[PSK(psi)])
            P.op("dve", lambda e, dc=dc, psi=psi, tsl=tsl: e.scalar_tensor_tensor(
                out=x2T[:, dc, tsl], in0=psb[psi][:, :], scalar=adaT[:, G1 + dc:G1 + dc + 1], in1=x2T[:, dc, tsl],
                op0=ALU.mult, op1=ALU.add), r=[PSK(psi), ("x2T", dc), "adaT"], w=[("x2T", dc)])
    P.barrier()
    dump("x2T", x2T)

    RD = Region(8 * K + 64 * K, ARENA)
    h2T = RD.alloc([128, 8, 2048], BF16)
    We = [[RD.alloc([128, 8, 512], BF16), RD.alloc([128, 8, 512], BF16), RD.alloc([128, 4, 1024], BF16)] for _ in range(2)]
    estg = [RD.alloc([128, 4, 512], F32) for _ in range(2)]
    gatT = RD.alloc([16, 2048], BF16)
    sel = RD.alloc([16, 16, 128], BF16)
    rwf = RD.alloc([128, 8, 20], F32)
    rwb = RD.alloc([128, 8, 20], BF16)
    UNION0 = RD.cur
    sq2 = RD.alloc([128, 8, 512], BF16)
    rs2_l = [RD.alloc([128, 512], F32) for _ in range(2)]
    htmp = [RD.alloc([128, 512], F32) for _ in range(3)]
    rl = RD.alloc([128, 16, 20], F32)
    gat = RD.alloc([128, 16, 16], F32)
    ml = RD.alloc([128, 16, 16], F32)
    ml2 = RD.alloc([128, 16, 16], F32)
    oh1 = RD.alloc([128, 16, 16], F32)
    oh2 = RD.alloc([128, 16, 16], F32)
    g4 = RD.alloc([128, 16, 4], F32)
    g4b = RD.alloc([128, 16, 4], F32)
    sc = RD.alloc([128, 8, 16], F32)
    RU = Region(UNION0, ARENA)
    hid = [[RU.alloc([128, 512], BF16) for _ in range(4)] for _ in range(2)]
    sl = [RU.alloc([128, 512], F32) for _ in range(2)]
    vv = [RU.alloc([128, 512], F32) for _ in range(2)]
    sq_f = RU.alloc([128, 8, 512], BF16)
    rs_f = RU.alloc([128, 512], F32)

    P.op("sp", lambda e: e.dma_start(out=rwf, in_=rw.rearrange("(fc p) n -> p fc n", p=128)), w=["rwf"], dma=True)
    P.op("sp", lambda e: e.dma_start(out=rbias_b, in_=rb[0:1, :].partition_broadcast(128)), w=["rbias_b"], dma=True)
    P.op("dve", lambda e: e.tensor_copy(out=rwb, in_=rwf), r=["rwf"], w=["rwb"])
    P.op("pool", lambda e: e.tensor_copy(out=sel, in_=ident[0:16, 0:16].unsqueeze(2).to_broadcast([16, 16, 128])),
         r=["ident"], w=["sel"])

    def stats_tile(t, par, psi, eps):
        tsl = slice(t * 512, (t + 1) * 512)
        rs2 = rs2_l[par]
        for fc in range(8):
            P.op("act", lambda e, fc=fc: e.activation(out=sq2[:, fc, :], in_=x2T[:, fc, tsl], func=AF.Square),
                 r=[("x2T", fc)], w=[("sq2", fc)])

        def mm(e):
            ins = None
            for fc in range(8):
                ins = e.matmul(psb[psi][:, :], lhsT=ones_bf, rhs=sq2[:, fc, :], start=(fc == 0), stop=(fc == 7))
            return ins
        P.op("pe", mm, r=[("sq2", fc) for fc in range(8)] + ["ones_bf"], w=[PSK(psi)])
        P.op("act", lambda e: e.activation(out=rs2, in_=psb[psi][:, :], func=AF.Sqrt, bias=eps, scale=1.0),
             r=[PSK(psi)], w=[("rs2", par)])
        P.op("dve", lambda e: e.reciprocal(out=rs2, in_=rs2), r=[("rs2", par)], w=[("rs2", par)])

    def route_section():
        stats_tile(0, 0, 1, NORM_EPS)
        for t in range(4):
            tsl = slice(t * 512, (t + 1) * 512)
            if t + 1 < 4:
                stats_tile(t + 1, (t + 1) % 2, (1, 4)[(t + 1) % 2], NORM_EPS)
            rs2 = rs2_l[t % 2]
            for fc in range(8):
                g = fc % 3
                eng = "dve"
                P.op(eng, lambda e, tsl=tsl, fc=fc, g=g, rs2=rs2: e.tensor_tensor(out=htmp[g], in0=x2T[:, fc, tsl], in1=rs2,
                                                                                 op=ALU.mult),
                     r=[("x2T", fc), ("rs2", t % 2)], w=[("htmp", g)])
                P.op("act", lambda e, tsl=tsl, fc=fc, g=g: e.activation(
                    out=h2T[:, fc, tsl], in_=htmp[g], func=AF.Identity, bias=adaT[:, SH2 + fc:SH2 + fc + 1],
                    scale=der[:, GSC2 + fc:GSC2 + fc + 1]),
                    r=[("htmp", g), "adaT", "der"], w=[("h2T", t, fc)])

            def mmr(e, t=t):
                ins = None
                for sub in range(4):
                    st = t * 4 + sub
                    for fc in range(8):
                        ins = e.matmul(psb[2][:, st * 32:st * 32 + 20], lhsT=h2T[:, fc, st * 128:(st + 1) * 128], rhs=rwb[:, fc, :],
                                       start=(fc == 0), stop=(fc == 7))
                return ins
            P.op("pe", mmr, r=[("h2T", t, fc) for fc in range(8)] + ["rwb"], w=[("psR", t)])
            if t == 0:
                chunk_cast(0, 0, eng="act")
                chunk_cast(0, 1, eng="act")
                chunk_dma(0, 2)
                chunk_dma(0, 3)
            if t == 2:
                chunk_cast(0, 2, eng="act")
                chunk_cast(0, 3, eng="act")
                chunk_dma(0, 4)
                chunk_dma(0, 5)
        chunk_cast(0, 4, eng="act")
        chunk_cast(0, 5, eng="act")
        PSR = [("psR", t) for t in range(4)]
        P.op("dve", lambda e: e.tensor_tensor(out=rl, in0=psb[2][:, :].rearrange("p (s c) -> p s c", c=32)[:, :, 0:20],
                                              in1=rbias_b.unsqueeze(1).to_broadcast([128, 16, 20]), op=ALU.add),
             r=PSR + ["rbias_b"], w=["rl"])
        gl = rl[:, :, 0:4]
        el = rl[:, :, 4:20]
        R = "rt"
        P.op("dve", lambda e: e.tensor_reduce(out=sc[:, 0, :], in_=gl, axis=AX.X, op=ALU.max), r=["rl"], w=[R])
        P.op("dve", lambda e: e.tensor_tensor(out=g4, in0=gl, in1=sc[:, 0, :].unsqueeze(2).to_broadcast([128, 16, 4]),
                                              op=ALU.subtract), r=["rl", R], w=[R])
        P.op("act", lambda e: e.activation(out=g4b, in_=g4, func=AF.Exp), r=[R], w=[R])
        P.op("dve", lambda e: e.tensor_reduce(out=sc[:, 1, :], in_=g4b, axis=AX.X, op=ALU.add), r=[R], w=[R])
        P.op("dve", lambda e: e.reciprocal(out=sc[:, 1, :], in_=sc[:, 1, :]), r=[R], w=[R])
        P.op("dve", lambda e: e.tensor_scalar(out=g4, in0=g4, scalar1=0.0, scalar2=None, op0=ALU.is_ge), r=[R], w=[R])
        P.op("dve", lambda e: e.tensor_scalar(out=g4, in0=g4, scalar1=-1.0, scalar2=BIG, op0=ALU.add, op1=ALU.mult),
             r=[R], w=[R])
        P.op("dve", lambda e: e.tensor_tensor(out=ml.rearrange("p s (g x) -> p s g x", g=4),
                                              in0=el.rearrange("p s (g x) -> p s g x", g=4),
                                              in1=g4.unsqueeze(3).to_broadcast([128, 16, 4, 4]), op=ALU.add),
             r=["rl", R], w=[R])
        P.op("dve", lambda e: e.tensor_reduce(out=sc[:, 2, :], in_=ml, axis=AX.X, op=ALU.max), r=[R], w=[R])
        P.op("dve", lambda e: e.tensor_tensor(out=oh1, in0=ml, in1=sc[:, 2, :].unsqueeze(2).to_broadcast([128, 16, 16]),
                                              op=ALU.is_ge), r=[R], w=[R])
        P.op("dve", lambda e: e.scalar_tensor_tensor(out=ml2, in0=oh1, scalar=-BIG, in1=ml, op0=ALU.mult, op1=ALU.add),
             r=[R], w=[R])
        P.op("dve", lambda e: e.tensor_reduce(out=sc[:, 3, :], in_=ml2, axis=AX.X, op=ALU.max), r=[R], w=[R])
        P.op("dve", lambda e: e.tensor_tensor(out=oh2, in0=ml2, in1=sc[:, 3, :].unsqueeze(2).to_broadcast([128, 16, 16]),
                                              op=ALU.is_ge), r=[R], w=[R])
        P.op("dve", lambda e: e.tensor_tensor(out=sc[:, 4, :], in0=sc[:, 3, :], in1=sc[:, 2, :], op=ALU.subtract),
             r=[R], w=[R])
        P.op("act", lambda e: e.activation(out=sc[:, 4, :], in_=sc[:, 4, :], func=AF.Exp), r=[R], w=[R])
        P.op("dve", lambda e: e.tensor_scalar(out=sc[:, 4, :], in0=sc[:, 4, :], scalar1=1.0, scalar2=None, op0=ALU.add),
             r=[R], w=[R])
        P.op("dve", lambda e: e.reciprocal(out=sc[:, 4, :], in_=sc[:, 4, :]), r=[R], w=[R])
        P.op("dve", lambda e: e.tensor_tensor(out=sc[:, 5, :], in0=sc[:, 4, :], in1=sc[:, 1, :], op=ALU.mult), r=[R], w=[R])
        P.op("dve", lambda e: e.tensor_tensor(out=sc[:, 6, :], in0=sc[:, 1, :], in1=sc[:, 5, :], op=ALU.subtract),
             r=[R], w=[R])
        P.op("dve", lambda e: e.tensor_tensor(out=gat, in0=oh1, in1=sc[:, 5, :].unsqueeze(2).to_broadcast([128, 16, 16]),
                                              op=ALU.mult), r=[R], w=[R])
        P.op("dve", lambda e: e.tensor_tensor(out=oh2, in0=oh2, in1=sc[:, 6, :].unsqueeze(2).to_broadcast([128, 16, 16]),
                                              op=ALU.mult), r=[R], w=[R])
        P.op("dve", lambda e: e.tensor_tensor(out=gat, in0=gat, in1=oh2, op=ALU.add), r=[R], w=[R])
        for t in range(4):
            def mmg(e, t=t):
                ins = None
                for sub in range(4):
                    st = t * 4 + sub
                    ins = e.transpose(psb[3][0:16, sub * 128:(sub + 1) * 128], gat[:, st, :], ident)
                return ins
            P.op("pe", mmg, r=[R, "ident"], w=[PSK(3)])
            P.op("act", lambda e, t=t: e.activation(out=gatT[:, t * 512:(t + 1) * 512], in_=psb[3][0:16, :], func=AF.Copy),
                 r=[PSK(3)], w=[("gatT", t)])
        dump("gat", gat)
        dump("h2T", h2T, BF16)

    wg_src = wg.rearrange("x (fc p) n -> p x fc n", p=128)
    wu_src = wu.rearrange("x (fc p) n -> p x fc n", p=128)
    wd_src = wd.rearrange("x (jc p) n -> p x jc n", p=128)
    stg_n = [0]

    def chunk_spec(ex, k):
        wb = We[ex % 2]
        if k < 4:
            nm, hf = k // 2, k % 2
            src = (wg_src, wu_src)[nm][:, ex, hf * 4:(hf + 1) * 4, :]
            dsts = [(wb[nm][:, hf * 4 + f4, :], ("We", ex % 2, nm, hf * 4 + f4)) for f4 in range(4)]
        else:
            hf = k - 4
            src = wd_src[:, ex, :, hf * 512:(hf + 1) * 512]
            dsts = [(wb[2][:, jc, hf * 512:(hf + 1) * 512], ("We", ex % 2, 2, jc, hf)) for jc in range(4)]
        return src, dsts

    def chunk_dma(ex, k):
        src, dsts = chunk_spec(ex, k)
        sg = estg[k % 2]
        P.op("sp", lambda e: e.dma_start(out=sg, in_=src), w=[("estg", k % 2)], dma=True)

    def chunk_cast(ex, k, eng="act"):
        src, dsts = chunk_spec(ex, k)
        sg = estg[k % 2]
        for f4, (dst, key) in enumerate(dsts):
            if eng == "act":
                P.op("act", lambda e, dst=dst, f4=f4: e.activation(out=dst, in_=sg[:, f4, :], func=AF.Copy),
                     r=[("estg", k % 2)], w=[key])
            else:
                P.op(eng, lambda e, dst=dst, f4=f4: e.tensor_copy(out=dst, in_=sg[:, f4, :]), r=[("estg", k % 2)], w=[key])

    def expert_load(ex):
        for k in range(6):
            chunk_dma(ex, k)
            chunk_cast(ex, k, eng=("act", "dve")[k % 2])

    PREF = {4: [("d", 0), ("d", 1)], 6: [("c", 0), ("d", 2)], 8: [("c", 1), ("d", 3)], 10: [("c", 2), ("d", 4)],
            12: [("c", 3), ("d", 5)], 14: [("c", 4)], 15: [("c", 5)]}

    def prefetch_step(ex, sub):
        for kind, k in PREF.get(sub, []):
            if kind == "d":
                chunk_dma(ex, k)
            else:
                chunk_cast(ex, k)

    def g_step(ex, t, jc):
        wb = We[ex % 2]
        WK = [("We", ex % 2, nm, fc) for nm in range(2) for fc in range(8)]
        tsl = slice(t * 512, (t + 1) * 512)
        hb = (ex * 4 + t) % 2
        H2K = [("h2T", t, fc) for fc in range(8)]
        if jc == 0:
            P.op("pe", lambda e: e.matmul(psb[1][:, :], lhsT=sel[:, ex, :], rhs=gatT[:, tsl], start=True, stop=True),
                 r=["sel", ("gatT", t)], w=[PSK(1)])
        pa = 2 + (jc % 2) * 2
        pu = pa + 1

        def mmgu(e):
            ins = None
            for fc in range(8):
                e.matmul(psb[pa][:, :], lhsT=wb[0][:, fc, jc * 128:(jc + 1) * 128], rhs=h2T[:, fc, tsl],
                         start=(fc == 0), stop=(fc == 7))
            for fc in range(8):
                ins = e.matmul(psb[pu][:, :], lhsT=wb[1][:, fc, jc * 128:(jc + 1) * 128], rhs=h2T[:, fc, tsl],
                               start=(fc == 0), stop=(fc == 7))
            return ins
        P.op("pe", mmgu, r=WK + H2K, w=[PSK(pa), PSK(pu)])
        g = jc % 2
        P.op("act", lambda e: e.activation(out=sl[g], in_=psb[pa][:, :], func=AF.Silu), r=[PSK(pa)], w=[("sl", g)])
        P.op("dve", lambda e: e.tensor_tensor(out=vv[g], in0=psb[pu][:, :], in1=sl[g], op=ALU.mult),
             r=[PSK(pu), ("sl", g)], w=[("vv", g)])
        P.op("dve", lambda e: e.tensor_tensor(out=hid[hb][jc], in0=vv[g], in1=psb[1][:, :], op=ALU.mult),
             r=[("vv", g), PSK(1)], w=[("hid", hb, jc)])

    def d_step(ex, t, dc):
        wb = We[ex % 2]
        WDK = [("We", ex % 2, 2, jc, hf) for jc in range(4) for hf in range(2)]
        tsl = slice(t * 512, (t + 1) * 512)
        hb = (ex * 4 + t) % 2
        py = 6 + (dc % 2)

        def mmd(e):
            ins = None
            for jc in range(4):
                ins = e.matmul(psb[py][:, :], lhsT=wb[2][:, jc, dc * 128:(dc + 1) * 128], rhs=hid[hb][jc],
                               start=(jc == 0), stop=(jc == 3))
            return ins
        P.op("pe", mmd, r=WDK + [("hid", hb, jc) for jc in range(4)], w=[PSK(py)])
        P.op("dve", lambda e: e.scalar_tensor_tensor(out=x2T[:, dc, tsl], in0=psb[py][:, :], scalar=adaT[:, G2 + dc:G2 + dc + 1],
                                                      in1=x2T[:, dc, tsl], op0=ALU.mult, op1=ALU.add),
             r=[PSK(py), ("x2T", dc), "adaT"], w=[("x2T", dc)])

    NE = 16
    chunk_dma(0, 0)
    chunk_dma(0, 1)
    route_section()
    P.barrier()
    units = [(ex, t) for ex in range(NE) for t in range(4)]
    yT_src = yT.rearrange("(dc p) t -> p dc t", p=128)

    def final_stats(t):
        tsl = slice(t * 512, (t + 1) * 512)
        for fc in range(8):
            P.op("act", lambda e, fc=fc: e.activation(out=sq_f[:, fc, :], in_=x2T[:, fc, tsl], func=AF.Square),
                 r=[("x2T", fc)], w=[("sqf", fc)])

        def mm(e):
            ins = None
            for fc in range(8):
                ins = e.matmul(psb[0][:, :], lhsT=ones_bf, rhs=sq_f[:, fc, :], start=(fc == 0), stop=(fc == 7))
            return ins
        P.op("pe", mm, r=[("sqf", fc) for fc in range(8)] + ["ones_bf"], w=[PSK(0)])
        P.op("act", lambda e: e.activation(out=rs_f, in_=psb[0][:, :], func=AF.Sqrt, bias=NORM_EPS, scale=1.0),
             r=[PSK(0)], w=["rs_f"])
        P.op("dve", lambda e: e.reciprocal(out=rs_f, in_=rs_f), r=["rs_f"], w=["rs_f"])

    def final_apply(t):
        tsl = slice(t * 512, (t + 1) * 512)
        for dc in range(8):
            P.op("dve", lambda e, dc=dc: e.scalar_tensor_tensor(
                out=x2T[:, dc, tsl], in0=x2T[:, dc, tsl], scalar=vec[:, FG + dc:FG + dc + 1], in1=rs_f,
                op0=ALU.mult, op1=ALU.mult), r=[("x2T", dc), "rs_f", "vec"], w=[("x2T", dc), ("x2Tout", t, dc)])
        P.op("sp", lambda e: e.dma_start(out=yT_src[:, :, tsl], in_=x2T[:, :, tsl]),
             r=[("x2Tout", t, fc) for fc in range(8)], dma=True)

    for u in range(len(units) + 1):
        if u < len(units):
            ex, t = units[u]
        prev = units[u - 1] if u >= 1 else None
        for jc in range(4):
            if u < len(units):
                g_step(ex, t, jc)
            if prev is not None:
                d_step(prev[0], prev[1], 2 * jc)
                d_step(prev[0], prev[1], 2 * jc + 1)
            if u < len(units) and ex + 1 < NE:
                prefetch_step(ex + 1, t * 4 + jc)
        if prev is not None and prev[0] == NE - 1:
            if prev[1] >= 1:
                final_apply(prev[1] - 1)
            final_stats(prev[1])

    final_apply(3)
    P.op("sp", None, after=[o["idx"] for o in P.ops if o["dma"]][-8:])
    P.emit(nc, es)
    es.close()
    return nc, list(dbg_out.keys())


_CACHE = {}


def _host_inputs(x, c, positions, rel_bias, ada_w, ada_b, norm1_g, w_in, lambda_q1, lambda_k1, lambda_q2, lambda_k2,
                 subln_g, conv_w, w_out, norm2_g, router_group_w, router_group_b, router_expert_w, router_expert_b,
                 expert_w_gate, expert_w_up, expert_w_down, final_g):
    f = lambda a: np.ascontiguousarray(np.asarray(a, dtype=np.float32))
    x = f(x)
    pk = lambda v: f(np.asarray(v, np.float32).reshape(-1, 128).T)
    shared = dict(
        adab=pk(ada_b[0]), ada_w=f(ada_w[0]), w_in=f(w_in[0]), w_out=f(w_out[0]),
        lam4=f(np.concatenate([np.asarray(lambda_q1[0]), np.asarray(lambda_k1[0]), np.asarray(lambda_q2[0]),
                               np.asarray(lambda_k2[0])]).reshape(1, 256)),
        rw=f(np.concatenate([np.asarray(router_group_w[0]), np.asarray(router_expert_w[0])], axis=1)),
        rb=f(np.concatenate([np.asarray(router_group_b[0]), np.asarray(router_expert_b[0])]).reshape(1, 20)),
        relb=f(rel_bias), relb31=f(np.asarray(rel_bias)[31].reshape(8, 1)),
        wg=f(expert_w_gate[0]), wu=f(expert_w_up[0]), wd=f(expert_w_down[0]),
        ident=np.eye(128, dtype=np.float32), antiid=np.ascontiguousarray(np.eye(128, dtype=np.float32)[::-1]),
        lo=np.asarray(LO, np.float32).reshape(32, 1),
    )
    cw = np.asarray(conv_w[0], np.float32)
    convw = cw.T.reshape(4, 128, 3).transpose(1, 0, 2).reshape(128, 12)
    in_maps = []
    xTs = [np.ascontiguousarray(x[b].T) for b in range(2)]
    for core in range(8):
        b, j = core // 4, core % 4
        xo = np.zeros((DM, NOWN, 130), np.float32)
        for i in range(NOWN):
            s = (4 * i + j) * 128
            if s == 0:
                xo[:, i, 2:] = xTs[b][:, 0:128]
            else:
                xo[:, i, :] = xTs[b][:, s - 2:s + 128]
        vecs = np.zeros((128, 48), np.float32)
        vecs[:, 0:8] = pk(np.asarray(c)[b])
        vecs[:, 8:16] = pk(norm1_g[0])
        vecs[:, 16:24] = pk(norm2_g[0])
        vecs[:, 24:32] = pk(final_g)
        vecs[:, 32:44] = convw
        vecs[:, 44] = 0.0 if j == 0 else 1.0
        vecs[:, 45] = np.asarray(subln_g[0], np.float32)
        dvec = (np.arange(768, dtype=np.float32) - 639.0 + (j + 1) * 128.0).reshape(1, 768)
        m = dict(shared)
        m.update(xT=xTs[b], xo=xo, vecs=vecs, dvec=dvec)
        in_maps.append(m)
    return in_maps


def kernel(**inputs):
    if "nc" not in _CACHE:
        _CACHE["nc"] = build_program()[0]
    nc = _CACHE["nc"]
    in_maps = _host_inputs(**inputs)
    res = run_bass_kernel_spmd(nc, in_maps, core_ids=list(range(8)))
    y = np.zeros((2, S, DM), np.float32)
    for core in range(8):
        b, j = core // 4, core % 4
        yT = np.asarray(res.results[core]["yT"])
        yv = yT.T.reshape(NOWN, 128, DM)
        for i in range(NOWN):
            s = (4 * i + j) * 128
            y[b, s:s + 128, :] = yv[i]
    return y
```

```python
import contextlib
import math

import numpy as np
import concourse.bass as bass
import concourse.mybir as mybir
from concourse.bass_utils import run_bass_kernel_spmd

F32 = mybir.dt.float32
BF16 = mybir.dt.bfloat16
U8 = mybir.dt.uint8
AF = mybir.ActivationFunctionType
ALU = mybir.AluOpType
AX = mybir.AxisListType

S = 8192
DM = 1024
NOWN = 16
LAM_INIT = 0.8 - 0.6 * math.exp(0.0)
NORM_EPS = 1e-6
SUBLN_EPS = 1e-5
BIG = 1.0e30
LO = [0, 1, 2, 3, 4, 5, 6, 7, 8, 9, 10, 11, 12, 13, 14, 15, 16, 19, 21, 24, 27, 31, 35, 40, 46, 52,
      59, 67, 77, 87, 99, 113]


class Prog:
    ENGS = ("pe", "act", "dve", "pool", "sp")

    def __init__(self, n_dma_sems=48):
        self.ops = []
        self.lw = {}
        self.rd = {}
        self.n_dma_sems = n_dma_sems
        self.bar = set()
        self.dma_since = []
        self.last_eng = {}

    def op(self, eng, fn, r=(), w=(), dma=False, after=()):
        idx = len(self.ops)
        deps = set(after) | self.bar
        for b in r:
            x = self.lw.get(b)
            if x is not None:
                deps.add(x)
        for b in w:
            x = self.lw.get(b)
            if x is not None:
                deps.add(x)
            deps.update(self.rd.get(b, ()))
        for b in r:
            self.rd.setdefault(b, []).append(idx)
        for b in w:
            self.lw[b] = idx
            self.rd[b] = []
        deps.discard(idx)
        self.ops.append(dict(eng=eng, fn=fn, deps=deps, dma=dma, idx=idx))
        if dma:
            self.dma_since.append(idx)
        elif fn is not None:
            self.last_eng[eng] = idx
        return idx

    def barrier(self):
        self.bar = set(self.dma_since) | set(self.last_eng.values())
        self.dma_since = []

    def emit(self, nc, es):
        ops = self.ops
        needed = set()
        for o in ops:
            needed.update(o["deps"])
        eng_sem = {e: es.enter_context(nc.semaphore("s_" + e)) for e in self.ENGS}
        dma_sems = [es.enter_context(nc.semaphore("d%d" % i)) for i in range(self.n_dma_sems)]
        cnt = {e: 0 for e in self.ENGS}
        dcnt = [0] * self.n_dma_sems
        dma_rr = 0
        ev = {}
        for o in ops:
            o["pre"] = []
            if o["dma"]:
                k = dma_rr % self.n_dma_sems
                dma_rr += 1
                if dcnt[k] > 0:
                    o["pre"].append((("d", k), dcnt[k]))
                o["dsem"] = k
                dcnt[k] += 16
                ev[o["idx"]] = (("d", k), dcnt[k])
            elif o["idx"] in needed and o["fn"] is not None:
                cnt[o["eng"]] += 1
                ev[o["idx"]] = (("e", o["eng"]), cnt[o["eng"]])
        per_eng = {e: [] for e in self.ENGS}
        for o in ops:
            per_eng[o["eng"]].append(o)

        def semh(key):
            return eng_sem[key[1]] if key[0] == "e" else dma_sems[key[1]]

        def run(engname, e):
            waited = {}
            for o in per_eng[engname]:
                best = {}
                for key, val in o["pre"]:
                    if val > best.get(key, 0):
                        best[key] = val
                for d in o["deps"]:
                    if d in ev:
                        key, val = ev[d]
                        if val > best.get(key, 0):
                            best[key] = val
                for key, val in best.items():
                    if engname == "pe" and key == ("e", "pe"):
                        continue
                    if waited.get(key, 0) < val:
                        e.wait_ge(semh(key), val)
                        waited[key] = val
                if o["fn"] is None:
                    continue
                ins = o["fn"](e)
                if o["dma"]:
                    ins.then_inc(dma_sems[o["dsem"]], 16)
                elif o["idx"] in ev:
                    ins.then_inc(eng_sem[engname], 1)

        block = es.enter_context(nc.Block())

        @block.tensor
        def _(e):
            run("pe", e)

        @block.scalar
        def _(e):
            run("act", e)

        @block.vector
        def _(e):
            run("dve", e)

        @block.gpsimd
        def _(e):
            run("pool", e)

        @block.sync
        def _(e):
            run("sp", e)


def build_program(dbg=()):
    nc = bass.Bass("TRN2", target_bir_lowering=False)
    P = Prog()

    def DI(name, shape, dt=F32):
        return nc.dram_tensor(name, list(shape), dt, kind="ExternalInput").ap()

    xT = DI("xT", [DM, S])
    xo = DI("xo", [DM, NOWN, 130])
    vecs = DI("vecs", [128, 48])
    adab = DI("adab", [128, 48])
    ada_w = DI("ada_w", [DM, 6 * DM])
    w_in = DI("w_in", [DM, 3072])
    w_out = DI("w_out", [DM, DM])
    lam4 = DI("lam4", [1, 256])
    rw = DI("rw", [DM, 20])
    rb = DI("rb", [1, 20])
    relb = DI("relb", [32, 8])
    relb31 = DI("relb31", [8, 1])
    wg = DI("wg", [16, DM, 512])
    wu = DI("wu", [16, DM, 512])
    wd = DI("wd", [16, 512, DM])
    ident_d = DI("ident", [128, 128])
    antiid_d = DI("antiid", [128, 128])
    lo_d = DI("lo", [32, 1])
    dvec_d = DI("dvec", [1, 768])
    yT = nc.dram_tensor("yT", [DM, 2048], F32, kind="ExternalOutput").ap()
    wscr = nc.dram_tensor("wscr", [8, 768], F32).ap()
    mscr = nc.dram_tensor("mscr", [128, 5 * 1024], BF16).ap()
    dbg_out = {}

    es = contextlib.ExitStack()
    ARENA = 208896
    arena = nc.alloc_sbuf_tensor("arena", [128, ARENA], U8)

    def carve(off, shape, dt):
        n = int(np.prod(shape[1:]))
        bpe = 4 if dt == F32 else 2
        assert off % 4 == 0 and off + n * bpe <= ARENA, (off, shape)
        v = arena[0:shape[0], off:off + n * bpe].bitcast(dt)
        if len(shape) == 3:
            v = v.rearrange("p (a b) -> p a b", a=shape[1])
        elif len(shape) == 4:
            v = v.rearrange("p (a b c) -> p a b c", a=shape[1], b=shape[2])
        return v

    class Region:
        def __init__(self, lo, hi):
            self.lo, self.hi, self.cur = lo, hi, lo

        def alloc(self, shape, dt):
            n = int(np.prod(shape[1:])) * (4 if dt == F32 else 2)
            n = (n + 63) // 64 * 64
            off = self.cur
            self.cur += n
            assert self.cur <= self.hi, ("region overflow", self.cur, self.hi)
            return carve(off, shape, dt)

    K = 1024
    RC = Region(0, 8 * K)
    ident = RC.alloc([128, 128], F32)
    ones_bf = RC.alloc([128, 128], BF16)
    ones_f = RC.alloc([128, 128], F32)
    vec = RC.alloc([128, 48], F32)
    adaT = RC.alloc([128, 48], F32)
    der = RC.alloc([128, 32], F32)
    bias_q = RC.alloc([128, 4], F32)
    bias_k = RC.alloc([128, 4], F32)
    bias_c = RC.alloc([128, 12], F32)
    bias_vb = RC.alloc([128, 512], F32)
    rbias_b = RC.alloc([128, 20], F32)
    bge = RC.alloc([128, 16, 8], F32)
    CP, N1G, N2G, FG, CW, HM, SUBG = 0, 8, 16, 24, 32, 44, 45
    SH1, SC1, G1, SH2, SC2, G2 = 0, 8, 16, 24, 32, 40
    GSC1, GSC2, LAMC, NLAM, SUBS = 0, 8, 16, 17, 18

    psd = [es.enter_context(nc.psum_tensor("psd%d" % i, [128, 1024], F32)) for i in range(4)]
    psb = [psd[i // 2][:, (i % 2) * 512:(i % 2 + 1) * 512] for i in range(8)]

    def PSK(i):
        return ("ps", i)

    def dump(name, ap_, dt=F32):
        if name not in dbg:
            return
        P.barrier()
        shp = list(ap_.shape)
        d = nc.dram_tensor("dbg_" + name, shp, dt, kind="ExternalOutput").ap()
        dbg_out[name] = d
        P.op("sp", lambda e: e.dma_start(out=d, in_=ap_), dma=True)
        P.barrier()

    P.op("sp", lambda e: e.dma_start(out=ident, in_=ident_d[:, :]), w=["ident"], dma=True)
    P.op("sp", lambda e: e.dma_start(out=vec, in_=vecs[:, :]), w=["vec"], dma=True)
    P.op("sp", lambda e: e.dma_start(out=adaT, in_=adab[:, :]), w=["adaT"], dma=True)
    P.op("pool", lambda e: e.memset(ones_bf, 1.0 / 1024.0), w=["ones_bf"])
    P.op("pool", lambda e: e.memset(ones_f, 1.0), w=["ones_f"])
    P.op("act", lambda e: e.activation(out=vec[:, CP:CP + 8], in_=vec[:, CP:CP + 8], func=AF.Silu),
         r=["vec"], w=["vec"])

    RS = Region(8 * K, 200 * K)
    ada_stg = [RS.alloc([128, 8, 512], F32) for _ in range(2)]
    lamt = RS.alloc([128, 256], F32)
    lamp = RS.alloc([128, 128], F32)
    lams = RS.alloc([128, 2], F32)
    ada_src = ada_w.rearrange("(kc p) n -> p kc n", p=128)
    ada_pend = []

    def ada_flush():
        while ada_pend:
            pc, rowbuf, rowkey = ada_pend.pop(0)

            def mt(e, pc=pc, rowbuf=rowbuf):
                ins = None
                for nn in range(4):
                    col = pc * 4 + nn
                    ins = e.matmul(psb[1][:, col:col + 1], lhsT=rowbuf[0:1, nn * 128:(nn + 1) * 128], rhs=ones_f[0:1, 0:1],
                                   start=True, stop=True)
                return ins
            P.op("pe", mt, r=[rowkey, "ones_f"], w=[("ps1ada", pc)])

    def ada_piece(pc, sg, sgkey, rowbuf, rowkey):
        P.op("sp", lambda e: e.dma_start(out=sg, in_=ada_src[:, :, pc * 512:(pc + 1) * 512]), w=[sgkey], dma=True)

        def mm(e):
            ins = None
            for kc in range(8):
                ins = e.matmul(psb[0][0:1, 0:512], lhsT=vec[:, CP + kc:CP + kc + 1], rhs=sg[:, kc, :], start=(kc == 0),
                               stop=(kc == 7))
            return ins
        P.op("pe", mm, r=[sgkey, "vec"], w=["ps0row"])
        P.op("act", lambda e: e.activation(out=rowbuf[0:1, :], in_=psb[0][0:1, 0:512], func=AF.Copy), r=["ps0row"], w=[rowkey])
        ada_flush()
        ada_pend.append((pc, rowbuf, rowkey))

    ada_row = [RS.alloc([1, 512], F32) for _ in range(2)]
    for pc in range(4):
        ada_piece(pc, ada_stg[pc % 2], ("adastg", pc % 2), ada_row[pc % 2], ("adarow", pc % 2))
    ada_flush()
    for pc in []:
        sg = ada_stg[pc % 2]

        def mm(e, sg=sg, pc=pc):
            ins = None
            for nn in range(4):
                col = pc * 4 + nn
                for kc in range(8):
                    ins = e.matmul(psb[0][:, col:col + 1], lhsT=sg[:, kc, nn * 128:(nn + 1) * 128],
                                   rhs=vec[:, CP + kc:CP + kc + 1], start=(kc == 0), stop=(kc == 7))
            return ins
        P.op("pe", mm, r=[("adastg", pc % 2), "vec"], w=[PSK(0)])
    P.op("dve", lambda e: e.tensor_tensor(out=adaT[:, 0:16], in0=adaT[:, 0:16], in1=psb[1][:, 0:16], op=ALU.add),
         r=[("ps1ada", pc) for pc in range(4)] + ["adaT"], w=["adaT"])
    P.op("dve", lambda e: e.scalar_tensor_tensor(out=der[:, GSC1:GSC1 + 8], in0=adaT[:, SC1:SC1 + 8], scalar=1.0,
                                                  in1=vec[:, N1G:N1G + 8], op0=ALU.add, op1=ALU.mult),
         r=["adaT", "vec"], w=["der"])
    P.op("dve", lambda e: e.tensor_scalar(out=der[:, SUBS:SUBS + 1], in0=vec[:, SUBG:SUBG + 1],
                                          scalar1=(1.0 - LAM_INIT), scalar2=None, op0=ALU.mult),
         r=["vec", "der"], w=["der"])
    P.op("sp", lambda e: e.dma_start(out=lamt, in_=lam4[0:1, :].partition_broadcast(128)), w=["lamt"], dma=True)
    P.op("dve", lambda e: e.tensor_tensor(out=lamp.rearrange("p (a d) -> p a d", a=2),
                                          in0=lamt.rearrange("p (a b d) -> p a b d", a=2, b=2)[:, :, 0, :],
                                          in1=lamt.rearrange("p (a b d) -> p a b d", a=2, b=2)[:, :, 1, :],
                                          op=ALU.mult), r=["lamt"], w=["lamp"])
    P.op("dve", lambda e: e.tensor_reduce(out=lams, in_=lamp.rearrange("p (a d) -> p a d", a=2), axis=AX.X,
                                          op=ALU.add), r=["lamp"], w=["lams"])
    P.op("act", lambda e: e.activation(out=lams, in_=lams, func=AF.Exp), r=["lams"], w=["lams"])
    P.op("dve", lambda e: e.scalar_tensor_tensor(out=der[:, LAMC:LAMC + 1], in0=lams[:, 0:1], scalar=LAM_INIT,
                                                  in1=lams[:, 1:2], op0=ALU.add, op1=ALU.subtract),
         r=["lams", "der"], w=["der"])
    P.op("dve", lambda e: e.tensor_scalar(out=der[:, NLAM:NLAM + 1], in0=der[:, LAMC:LAMC + 1], scalar1=-1.0,
                                          scalar2=None, op0=ALU.mult), r=["der"], w=["der"])
    antiid = RC.alloc([128, 128], F32)
    dv32 = RS.alloc([32, 768], F32)
    Sm = RS.alloc([32, 768], F32)
    w8 = RS.alloc([8, 768], F32)
    c8 = RS.alloc([8, 768], F32)
    rb0 = RS.alloc([32, 8], F32)
    rb1 = RS.alloc([32, 8], F32)
    lo_t = RS.alloc([32, 1], F32)
    nb31 = RS.alloc([8, 1], F32)

    P.op("sp", lambda e: e.dma_start(out=antiid, in_=antiid_d[:, :]), w=["antiid"], dma=True)
    P.op("sp", lambda e: e.dma_start(out=dv32, in_=dvec_d[0:1, :].partition_broadcast(32)), w=["dv32"], dma=True)
    P.op("sp", lambda e: e.dma_start(out=lo_t, in_=lo_d[:, :]), w=["lo_t"], dma=True)
    P.op("sp", lambda e: e.dma_start(out=rb0, in_=relb[:, :]), w=["rb0"], dma=True)
    P.op("pool", lambda e: e.memset(rb1, 0.0), w=["rb1"])
    P.op("sp", lambda e: e.dma_start(out=rb1[1:32, :], in_=relb[0:31, :]), w=["rb1"], dma=True)
    P.op("sp", lambda e: e.dma_start(out=nb31, in_=relb31[:, :]), w=["nb31"], dma=True)
    P.op("dve", lambda e: e.tensor_scalar(out=nb31, in0=nb31, scalar1=-1.0, scalar2=None, op0=ALU.mult),
         r=["nb31"], w=["nb31"])
    P.op("dve", lambda e: e.tensor_tensor(out=rb0, in0=rb0, in1=rb1, op=ALU.subtract), r=["rb0", "rb1"], w=["rb0"])
    P.op("dve", lambda e: e.tensor_scalar(out=Sm, in0=dv32, scalar1=lo_t[:, 0:1], scalar2=None, op0=ALU.is_ge),
         r=["dv32", "lo_t"], w=["Sm"])
    P.op("dve", lambda e: e.tensor_scalar(out=c8, in0=dv32[0:8, :], scalar1=0.0, scalar2=None, op0=ALU.is_ge),
         r=["dv32"], w=["c8"])

    def mmw(e):
        e.matmul(psb[2][0:8, 0:512], lhsT=rb0[:, :], rhs=Sm[:, 0:512], start=True, stop=True)
        return e.matmul(psb[3][0:8, 0:256], lhsT=rb0[:, :], rhs=Sm[:, 512:768], start=True, stop=True)
    P.op("pe", mmw, r=["rb0", "Sm"], w=[PSK(2), PSK(3)])
    P.op("act", lambda e: e.activation(out=w8[:, 0:512], in_=psb[2][0:8, 0:512], func=AF.Exp, bias=nb31[:, 0:1], scale=1.0),
         r=[PSK(2), "nb31"], w=["w8a"])
    P.op("act", lambda e: e.activation(out=w8[:, 512:768], in_=psb[3][0:8, 0:256], func=AF.Exp, bias=nb31[:, 0:1], scale=1.0),
         r=[PSK(3), "nb31"], w=["w8b"])
    P.op("dve", lambda e: e.tensor_tensor(out=w8, in0=w8, in1=c8, op=ALU.mult), r=["w8a", "w8b", "c8"], w=["w8"])
    P.op("sp", lambda e: e.dma_start(out=wscr[:, :], in_=w8), r=["w8"], w=["wscr"], dma=True)
    dump("adaT", adaT)
    dump("der", der)
    P.barrier()

    KT = carve(8 * K, [128, 4, S], BF16)
    VA = carve(72 * K, [128, 64, 4, 129], BF16)
    QOFF = 72 * K + 66048 + 512
    qT = carve(QOFF, [128, 4, 2048], BF16)
    RA0 = QOFF + 16 * K

    w_in_src = w_in.rearrange("(fc p) n -> p fc n", p=128)

    def norm_stage(tag, xs, n, sq, rs, hn, sq_eng="pool"):
        if sq_eng == "pool":
            P.op("pool", lambda e: e.tensor_tensor(out=sq, in0=xs, in1=xs, op=ALU.mult),
                 r=[(tag, "xs")], w=[(tag, "sq")])
        else:
            P.op("act", lambda e: e.activation(out=sq, in_=xs, func=AF.Square), r=[(tag, "xs")], w=[(tag, "sq")])

    RW = Region(RA0, 200 * K)
    Wkv = RW.alloc([128, 8, 1024], BF16)
    WKV_END = RW.cur
    Wq = RW.alloc([128, 8, 512], BF16)
    shrep = carve(8 * K, [128, 8, 128], F32)
    wstg = [carve(16 * K + i * 16 * K, [128, 8, 512], F32) for i in range(2)]

    def make_rep(dst, col0, key):
        for fc in range(8):
            P.op("dve", lambda e, fc=fc: e.tensor_scalar(out=dst[:, fc, :], in0=ones_f,
                                                          scalar1=adaT[:, col0 + fc:col0 + fc + 1], scalar2=None,
                                                          op0=ALU.mult), r=["ones_f", "adaT"], w=[(key, fc)])

    make_rep(shrep, SH1, "shrep")
    brow_s = [carve(48 * K + k_ * 2 * K, [1, 512], F32) for k_ in range(2)]

    def cast_piece(stg, stgkey, dst, dcol, gcol, engs=("dve", "act"), width=512):
        for fc in range(8):
            eng = engs[fc % len(engs)]
            if eng == "act":
                P.op("act", lambda e, fc=fc: e.activation(out=dst[:, fc, dcol:dcol + width], in_=stg[:, fc, 0:width],
                                                          func=AF.Identity, scale=gcol[:, fc:fc + 1]),
                     r=[stgkey, "der"], w=[("wdst", id(dst), fc, dcol)])
            else:
                P.op(eng, lambda e, fc=fc: e.tensor_scalar(out=dst[:, fc, dcol:dcol + width], in0=stg[:, fc, 0:width],
                                                           scalar1=gcol[:, fc:fc + 1], scalar2=None, op0=ALU.mult),
                     r=[stgkey, "der"], w=[("wdst", id(dst), fc, dcol)])

    def bias_pp(stg, stgkey, shcol, bias_dst, bcol0, psi, bkey, brow, psi2=None, width=512):
        psi2 = psi if psi2 is None else psi2
        nch = width // 128

        def mm(e):
            ins = None
            for fc in range(8):
                ins = e.matmul(psb[psi][0:1, 0:width], lhsT=adaT[:, shcol + fc:shcol + fc + 1], rhs=stg[:, fc, 0:width],
                               start=(fc == 0), stop=(fc == 7))
            return ins
        P.op("pe", mm, r=[stgkey, "adaT"], w=[PSK(psi)])
        P.op("act", lambda e: e.activation(out=brow[0:1, 0:width], in_=psb[psi][0:1, 0:width], func=AF.Copy), r=[PSK(psi)],
             w=[("brow", id(brow))])

        def mt(e):
            ins = None
            for mc in range(nch):
                ins = e.matmul(psb[psi2][:, mc:mc + 1], lhsT=brow[0:1, mc * 128:(mc + 1) * 128], rhs=ones_f[0:1, 0:1],
                               start=True, stop=True)
            return ins
        P.op("pe", mt, r=[("brow", id(brow)), "ones_f"], w=[PSK(psi2)])
        P.op("act", lambda e: e.activation(out=bias_dst[:, bcol0:bcol0 + nch], in_=psb[psi2][:, 0:nch], func=AF.Copy),
             r=[PSK(psi2)], w=[bkey])

    def bias_bc(stg, stgkey, rep, repkey, ncols, dst, psi, bkey):
        def mm(e):
            ins = None
            for fc in range(8):
                ins = e.matmul(psb[psi][:, 0:ncols], lhsT=rep[:, fc, :], rhs=stg[:, fc, 0:ncols], start=(fc == 0),
                               stop=(fc == 7))
            return ins
        P.op("pe", mm, r=[stgkey] + [(repkey, fc) for fc in range(8)], w=[PSK(psi)])
        P.op("act", lambda e: e.activation(out=dst, in_=psb[psi][:, 0:ncols], func=AF.Copy), r=[PSK(psi)], w=[bkey])

    for pi, (c0, dst, dcol) in enumerate([(0, Wq, 0), (512, Wkv, 0), (1024, Wkv, 512)]):
        sg = wstg[pi % 2]
        sk = ("wstg", pi % 2)
        P.op("sp", lambda e, sg=sg, c0=c0: e.dma_start(out=sg, in_=w_in_src[:, :, c0:c0 + 512]), w=[sk], dma=True)
        cast_piece(sg, sk, dst, dcol, der[:, GSC1:GSC1 + 8])
        if pi == 0:
            bias_pp(sg, sk, SH1, bias_q, 0, 2, "bias_q", brow_s[0], 4)
        elif pi == 1:
            bias_pp(sg, sk, SH1, bias_k, 0, 3, "bias_k", brow_s[1], 5)
        else:
            bias_bc(sg, sk, shrep, "shrep", 512, bias_vb, 6, "bias_vb")
    dump("bias_q", bias_q)
    dump("bias_vb", bias_vb)
    dump("Wq", Wq, BF16)

    RAO = Region(RW.cur, 200 * K)
    xos = [RAO.alloc([128, 8, 130], F32) for _ in range(2)]
    sqo = [RAO.alloc([128, 8, 130], BF16) for _ in range(2)]
    rso = [RAO.alloc([128, 130], F32) for _ in range(2)]
    hno = [RAO.alloc([128, 8, 130], BF16) for _ in range(2)]
    xo_src = xo.rearrange("(fc p) i t -> p fc i t", p=128)

    def own_norm(i, xs_t, sq_t, rs_t, hn_t, psi, hnkey=None, sq_on_act=False):
        b = i % 2
        hnkey = ("hno", b) if hnkey is None else hnkey
        P.op("sp", lambda e: e.dma_start(out=xs_t, in_=xo_src[:, :, i, :]), w=[("xos", b)], dma=True)
        if sq_on_act:
            P.op("act", lambda e: e.activation(out=sq_t, in_=xs_t, func=AF.Square), r=[("xos", b)], w=[("sqo", b)])
        else:
            P.op("pool", lambda e: e.tensor_tensor(out=sq_t, in0=xs_t, in1=xs_t, op=ALU.mult),
                 r=[("xos", b)], w=[("sqo", b)])

        def mm(e):
            ins = None
            for fc in range(8):
                ins = e.matmul(psb[psi][:, 0:130], lhsT=ones_bf, rhs=sq_t[:, fc, :], start=(fc == 0), stop=(fc == 7))
            return ins
        P.op("pe", mm, r=[("sqo", b), "ones_bf"], w=[PSK(psi)])
        P.op("act", lambda e: e.activation(out=rs_t, in_=psb[psi][:, 0:130], func=AF.Sqrt, bias=NORM_EPS, scale=1.0),
             r=[PSK(psi)], w=[("rso", b)])
        P.op("dve", lambda e: e.reciprocal(out=rs_t, in_=rs_t), r=[("rso", b)], w=[("rso", b)])
        P.op("dve", lambda e: e.tensor_tensor(out=hn_t, in0=xs_t, in1=rs_t.unsqueeze(1).to_broadcast([128, 8, 130]),
                                              op=ALU.mult), r=[("xos", b), ("rso", b)], w=[hnkey])

    ada_stg2 = [carve(72 * K + k_ * 16 * K, [128, 8, 512], F32) for k_ in range(2)]
    ada_row2 = [carve(72 * K + 32 * K + k_ * 2 * K, [1, 512], F32) for k_ in range(2)]

    def q_proj(i):
        b = i % 2
        pq = 6 + b

        def mmq(e):
            ins = None
            for mc in range(4):
                for fc in range(8):
                    ins = e.matmul(psb[pq][:, mc * 128:(mc + 1) * 128], lhsT=Wq[:, fc, mc * 128:(mc + 1) * 128],
                                   rhs=hno[b][:, fc, 2:130], start=(fc == 0), stop=(fc == 7))
            return ins
        P.op("pe", mmq, r=[("hno", b)] + [("wdst", id(Wq), fc, 0) for fc in range(8)], w=[PSK(pq)])
        for mc in range(4):
            P.op("act", lambda e, mc=mc: e.activation(
                out=qT[:, mc, i * 128:(i + 1) * 128], in_=psb[pq][:, mc * 128:(mc + 1) * 128], func=AF.Identity,
                bias=bias_q[:, mc:mc + 1], scale=1.0), r=[PSK(pq), "bias_q"], w=[("qT", i, mc)])

    Hs_l = [carve(72 * K + 40 * K + k_ * 4 * K, [128, 8, 128], F32) for k_ in range(2)]
    Mt_stage = carve(72 * K + 48 * K, [128, 5, 1024], BF16)

    def mtile(sp_):
        off = 512 - sp_ * 128
        src = bass.AP(wscr.tensor, off, [[1, 128], [768, 8], [1, 128]])
        Hs = Hs_l[sp_ % 2]
        hk = ("Hs", sp_ % 2)
        P.op("sp", lambda e: e.dma_start(out=Hs, in_=src), w=[hk], dma=True)

        def mmf(e):
            hs2 = Hs.rearrange("p m q -> p (m q)")
            e.matmul(psb[2][:, :], lhsT=antiid, rhs=hs2[:, 0:512], start=True, stop=True)
            return e.matmul(psb[3][:, :], lhsT=antiid, rhs=hs2[:, 512:1024], start=True, stop=True)
        P.op("pe", mmf, r=[hk, "antiid"], w=[PSK(2), PSK(3)])
        for hh in range(2):
            P.op("dve", lambda e, hh=hh: e.tensor_copy(
                out=Mt_stage[:, sp_, :].rearrange("p (c h q) -> p c h q", c=2, h=4)[:, :, 2 * hh:2 * hh + 2, :],
                in_=psb[2 + hh][:, :].rearrange("p (h c q) -> p c h q", h=2, c=2)),
                r=[PSK(2 + hh)], w=[("Mts", sp_, hh)])

    own_norm(0, xos[0], sqo[0], rso[0], hno[0], 4)
    for i in range(NOWN):
        if i + 1 < NOWN:
            b1 = (i + 1) % 2
            own_norm(i + 1, xos[b1], sqo[b1], rso[b1], hno[b1], 4 + b1)
        q_proj(i)
        if i % 3 == 1 and i // 3 < 5:
            mtile(i // 3)
        if i == 14:
            P.op("sp", lambda e: e.dma_start(out=mscr.rearrange("p (s x) -> p s x", s=5), in_=Mt_stage),
                 r=[("Mts", sp_, hh) for sp_ in range(5) for hh in range(2)], w=["mscr"], dma=True)
        if i % 2 == 0:
            pc = 4 + i // 2
            ada_piece(pc, ada_stg2[pc % 2], ("adastg2", pc % 2), ada_row2[pc % 2], ("adarow2", pc % 2))
    ada_flush()
    P.op("dve", lambda e: e.tensor_tensor(out=adaT[:, 16:48], in0=adaT[:, 16:48], in1=psb[1][:, 16:48], op=ALU.add),
         r=[("ps1ada", pc) for pc in range(4, 12)] + ["adaT"], w=["adaT"])
    P.op("dve", lambda e: e.scalar_tensor_tensor(out=der[:, GSC2:GSC2 + 8], in0=adaT[:, SC2:SC2 + 8], scalar=1.0,
                                                  in1=vec[:, N2G:N2G + 8], op0=ALU.add, op1=ALU.mult),
         r=["adaT", "vec", "der"], w=["der"])
    dump("qT", qT, BF16)
    P.barrier()

    RAK = Region(WKV_END, 200 * K)
    NCH = 32
    xs = [RAK.alloc([128, 8, 256], F32) for _ in range(2)]
    sqk1 = RAK.alloc([128, 8, 256], BF16)
    sqk = [sqk1, sqk1]
    rsk = [RAK.alloc([128, 256], F32) for _ in range(2)]
    hnk = [RAK.alloc([128, 8, 256], BF16) for _ in range(2)]
    xT_src = xT.rearrange("(fc p) t -> p fc t", p=128)
    P.op("pool", lambda e: e.memset(VA[:, :, :, 128:129], 1.0), w=["VAones"])

    def kv_load(c):
        b = c % 2
        P.op("sp", lambda e: e.dma_start(out=xs[b], in_=xT_src[:, :, c * 256:(c + 1) * 256]), w=[("xs", b)], dma=True)

    def kv_norm(c):
        b = c % 2
        P.op("act", lambda e: e.activation(out=sqk[b], in_=xs[b], func=AF.Square), r=[("xs", b)], w=["sqk"])

        def mm(e):
            ins = None
            for fc in range(8):
                ins = e.matmul(psb[b][:, 0:256], lhsT=ones_bf, rhs=sqk[b][:, fc, :], start=(fc == 0), stop=(fc == 7))
            return ins
        P.op("pe", mm, r=["sqk", "ones_bf"], w=[PSK(b)])
        P.op("act", lambda e: e.activation(out=rsk[b], in_=psb[b][:, 0:256], func=AF.Sqrt, bias=NORM_EPS, scale=1.0),
             r=[PSK(b)], w=[("rsk", b)])
        P.op("dve", lambda e: e.reciprocal(out=rsk[b], in_=rsk[b]), r=[("rsk", b)], w=[("rsk", b)])
        P.op("dve", lambda e: e.tensor_tensor(out=hnk[b], in0=xs[b], in1=rsk[b].unsqueeze(1).to_broadcast([128, 8, 256]),
                                              op=ALU.mult), r=[("xs", b), ("rsk", b)], w=[("hnk", b)])

    def kv_proj(c):
        b = c % 2
        for half in range(2):
            psi = 2 + half

            def mmk(e, half=half, psi=psi):
                ins = None
                for m2 in range(2):
                    mc = half * 2 + m2
                    for fc in range(8):
                        ins = e.matmul(psb[psi][:, m2 * 256:(m2 + 1) * 256], lhsT=Wkv[:, fc, mc * 128:(mc + 1) * 128],
                                       rhs=hnk[b][:, fc, :], start=(fc == 0), stop=(fc == 7))
                return ins
            P.op("pe", mmk, r=[("hnk", b)], w=[PSK(psi)])
            for m2 in range(2):
                mc = half * 2 + m2
                P.op("act", lambda e, mc=mc, m2=m2, psi=psi: e.activation(
                    out=KT[:, mc, c * 256:(c + 1) * 256], in_=psb[psi][:, m2 * 256:(m2 + 1) * 256], func=AF.Identity,
                    bias=bias_k[:, mc:mc + 1], scale=1.0), r=[PSK(psi), "bias_k"], w=[("KT", c, mc)])
        for tt in range(2):
            psi = 4 + tt

            def mmv(e, tt=tt, psi=psi):
                ins = None
                for fc in range(8):
                    ins = e.matmul(psb[psi][:, :], lhsT=hnk[b][:, fc, tt * 128:(tt + 1) * 128], rhs=Wkv[:, fc, 512:1024],
                                   start=(fc == 0), stop=(fc == 7))
                return ins
            P.op("pe", mmv, r=[("hnk", b)], w=[PSK(psi)])
            P.op("dve", lambda e, tt=tt, psi=psi: e.tensor_tensor(
                out=VA[:, 2 * c + tt, :, 0:128], in0=psb[psi][:, :].rearrange("p (h d) -> p h d", h=4),
                in1=bias_vb.rearrange("p (h d) -> p h d", h=4), op=ALU.add),
                r=[PSK(psi), "bias_vb"], w=[("VA", c, tt)])

    kv_load(0)
    kv_load(1)
    kv_norm(0)
    for c in range(NCH):
        if c + 2 < NCH:
            pass
        if c + 1 < NCH:
            kv_norm(c + 1)
        kv_proj(c)
        if c + 2 < NCH:
            kv_load(c + 2)
    dump("KT", KT, BF16)
    dump("VA", VA, BF16)
    P.barrier()

    RB = Region(RA0, 200 * K)
    attnT = RB.alloc([128, 4, 2048], BF16)
    Mt = RB.alloc([128, 5, 1024], BF16)
    UB0 = RB.cur
    Pt = [RB.alloc([128, 1024], BF16) for _ in range(3)]
    osb = RB.alloc([128, 8, 129], F32)
    att_l = [RB.alloc([128, 4, 128], F32) for _ in range(2)]
    att2_l = [RB.alloc([128, 4, 128], F32) for _ in range(2)]
    rz = RB.alloc([128, 8], F32)
    ssq_l = [RB.alloc([128, 4], F32) for _ in range(2)]
    RB = Region(UB0, 200 * K)
    P.op("sp", lambda e: e.dma_start(out=Mt, in_=mscr.rearrange("p (s x) -> p s x", s=5)), w=["Mt"], dma=True)
    dump("Mt", Mt, BF16)
    dump("w8", w8)
    OB = [5, 6, 7]
    TB = 4

    def omap(m):
        return psb[OB[m // 3]][:, (m % 3) * 129:(m % 3) * 129 + 129]

    pairs = []
    for i in range(NOWN):
        nk = 4 * i + 4
        for kb in range(nk):
            pairs.append((i, kb, kb == 0, kb == nk - 1))
    NP = len(pairs)
    osf = osb.rearrange("p m d -> p (m d)")
    OSK = ["osb0", "osb1", "osb2"]

    def stage_qk(n):
        i, kb, first, last = pairs[n]
        sset = n % 2
        pb = n % 3
        sA, sB = psb[2 * sset], psb[2 * sset + 1]
        gen = kb - (4 * i - 1)

        def mmqk(e):
            ins = None
            for h in range(4):
                e.matmul(sA[:, h * 128:(h + 1) * 128], lhsT=KT[0:64, h, kb * 128:(kb + 1) * 128],
                         rhs=qT[0:64, h, i * 128:(i + 1) * 128], start=True, stop=True)
                ins = e.matmul(sB[:, h * 128:(h + 1) * 128], lhsT=KT[64:128, h, kb * 128:(kb + 1) * 128],
                               rhs=qT[64:128, h, i * 128:(i + 1) * 128], start=True, stop=True)
            return ins
        P.op("pe", mmqk, r=[], w=[PSK(2 * sset), PSK(2 * sset + 1)])
        P.op("act", lambda e: e.activation(out=Pt[pb], in_=psd[sset][:, :], func=AF.Exp, scale=0.125),
             r=[PSK(2 * sset), PSK(2 * sset + 1)], w=[("Pt", pb, 0), ("Pt", pb, 1)])
        if gen >= 0:
            P.op("dve", lambda e: e.tensor_tensor(out=Pt[pb], in0=Pt[pb], in1=Mt[:, gen, :], op=ALU.mult),
                 r=[("Pt", pb, 0), ("Pt", pb, 1), "Mt"], w=[("Pt", pb, 0), ("Pt", pb, 1)])

    def stage_pv(n):
        i, kb, first, last = pairs[n]
        pb = n % 3
        par = i % 2
        att, att2, ssq = att_l[par], att2_l[par], ssq_l[par]
        ATK = [("att", par, h) for h in range(4)]

        def mmpv(e):
            ins = None
            for h in range(4):
                for c in range(2):
                    m = 2 * h + c
                    ins = e.matmul(omap(m), lhsT=Pt[pb][:, c * 512 + h * 128:c * 512 + (h + 1) * 128],
                                   rhs=VA[:, kb, h, :], start=(first and m % 3 == 0), stop=last,
                                   skip_group_check=True)
            return ins
        P.op("pe", mmpv, r=[("Pt", pb, 0), ("Pt", pb, 1)], w=[PSK(5), PSK(6), PSK(7)])
        if last:
            P.op("dve", lambda e: e.tensor_copy(out=osf[:, 0:387], in_=psb[5][:, 0:387]), r=[PSK(5)], w=["osb0"])
            P.op("dve", lambda e: e.tensor_copy(out=osf[:, 387:774], in_=psb[6][:, 0:387]), r=[PSK(6)], w=["osb1"])
            P.op("dve", lambda e: e.tensor_copy(out=osf[:, 774:1032], in_=psb[7][:, 0:258]), r=[PSK(7)], w=["osb2"])
            P.op("dve", lambda e: e.reciprocal(out=rz, in_=osb[:, :, 128]), r=OSK, w=["rz"])
            P.op("dve", lambda e: e.tensor_scalar(out=rz.rearrange("p (h c) -> p h c", c=2)[:, :, 1],
                                                  in0=rz.rearrange("p (h c) -> p h c", c=2)[:, :, 1],
                                                  scalar1=der[:, NLAM:NLAM + 1], scalar2=None, op0=ALU.mult),
                 r=["rz", "der"], w=["rz"])
            for h in range(4):
                P.op("dve", lambda e, h=h: e.tensor_scalar(out=att[:, h, :], in0=osb[:, 2 * h, 0:128],
                                                           scalar1=rz[:, 2 * h:2 * h + 1], scalar2=None, op0=ALU.mult),
                     r=OSK + ["rz"], w=[("att", par, h)])
                P.op("dve", lambda e, h=h: e.scalar_tensor_tensor(out=att[:, h, :], in0=osb[:, 2 * h + 1, 0:128],
                                                                  scalar=rz[:, 2 * h + 1:2 * h + 2], in1=att[:, h, :],
                                                                  op0=ALU.mult, op1=ALU.add),
                     r=OSK + ["rz", ("att", par, h)], w=[("att", par, h)])
            P.op("dve", lambda e: e.tensor_tensor(out=att2, in0=att, in1=att, op=ALU.mult), r=ATK, w=[("att2", par)])
            P.op("dve", lambda e: e.tensor_reduce(out=ssq, in_=att2, axis=AX.X, op=ALU.add), r=[("att2", par)], w=[("ssq", par)])

    def post_a(i):
        par = i % 2
        att, att2, ssq = att_l[par], att2_l[par], ssq_l[par]
        ATK = [("att", par, h) for h in range(4)]
        P.op("act", lambda e: e.activation(out=ssq, in_=ssq, func=AF.Ln, bias=SUBLN_EPS, scale=1.0 / 128.0),
             r=[("ssq", par)], w=[("ssq", par)])
        P.op("act", lambda e: e.activation(out=ssq, in_=ssq, func=AF.Exp, scale=-0.5),
             r=[("ssq", par)], w=[("ssq", par)])
        P.op("dve", lambda e: e.tensor_tensor(out=att2, in0=att, in1=ssq.unsqueeze(2).to_broadcast([128, 4, 128]),
                                              op=ALU.mult), r=ATK + [("ssq", par), ("att2", par)], w=[("att2", par)])

    def post_b(i):
        par = i % 2
        att2 = att2_l[par]
        def mmt(e):
            ins = None
            for h in range(4):
                ins = e.transpose(psb[TB][:, h * 128:(h + 1) * 128], att2[:, h, :], ident)
            return ins
        P.op("pe", mmt, r=[("att2", par), "ident"], w=[PSK(TB)])
        P.op("dve", lambda e: e.tensor_copy(out=attnT[:, :, i * 128:(i + 1) * 128],
                                            in_=psb[TB][:, :].rearrange("p (h q) -> p h q", h=4)),
             r=[PSK(TB)], w=[("attnT", i)])

    sched = {}
    for n in range(NP):
        i, kb, first, last = pairs[n]
        if last:
            sched.setdefault(n + 2 + 8, []).append(("a", i))
            sched.setdefault(n + 2 + 12, []).append(("b", i))
    PVLAG = 2
    for n in range(NP + PVLAG):
        if n < NP:
            stage_qk(n)
        if n >= PVLAG:
            stage_pv(n - PVLAG)
        for kind, i in sched.pop(n, []):
            (post_a if kind == "a" else post_b)(i)
    for n in sorted(sched):
        for kind, i in sched[n]:
            (post_a if kind == "a" else post_b)(i)
    P.barrier()

    dump("attnT", attnT, BF16)
    P.barrier()

    RC1 = Region(8 * K, RA0)
    x2T = RC1.alloc([128, 8, 2048], F32)
    convT = RC1.alloc([128, 4, 2048], BF16)
    Wc = RC1.alloc([128, 8, 1536], BF16)
    Wo = RC1.alloc([128, 8, 1024], BF16)
    cstg = [RC1.alloc([128, 8, 512], F32) for _ in range(1)]
    RC2 = Region(RA0 + 16 * K, ARENA)
    xos2 = [RC2.alloc([128, 8, 130], F32) for _ in range(2)]
    sqo2 = [RC2.alloc([128, 8, 130], BF16) for _ in range(2)]
    rso2 = [RC2.alloc([128, 130], F32) for _ in range(2)]
    hnp = [RC2.alloc([128, 8, 2, 130], BF16) for _ in range(2)]
    gcs = [RC2.alloc([128, 2, 130], F32) for _ in range(2)]
    ut = [RC2.alloc([128, 2, 130], F32) for _ in range(2)]
    tt_ = [RC2.alloc([128, 2, 128], F32) for _ in range(2)]
    hcs = [RC2.alloc([128, 2, 130], F32) for _ in range(2)]
    brow_c = RC2.alloc([1, 512], F32)
    w_out_src = w_out.rearrange("(fc p) n -> p fc n", p=128)

    def load_x2T(fcs):
        for fc in fcs:
            P.op("sp", lambda e, fc=fc: e.dma_start(out=x2T[:, fc, :].rearrange("p (i t) -> p i t", i=NOWN),
                                                    in_=xo_src[:, fc, :, 2:130]), w=[("x2T", fc)], dma=True)

    def load_wo(pi):
        sg = cstg[0]
        sk = ("cstg", 0)
        P.op("sp", lambda e: e.dma_start(out=sg, in_=w_out_src[:, :, pi * 512:(pi + 1) * 512]),
             w=[sk, ("cstgh", 0), ("cstgh", 1)], dma=True)
        for fc in range(8):
            dst = Wo[:, fc, pi * 512:(pi + 1) * 512]
            if fc < 4:
                if fc % 2 == 0:
                    P.op("dve", lambda e, fc=fc, dst=dst: e.tensor_scalar(out=dst, in0=sg[:, fc, :], scalar1=der[:, SUBS:SUBS + 1],
                                                                          scalar2=None, op0=ALU.mult),
                         r=[sk, "der"], w=[("Wo", fc, pi)])
                else:
                    P.op("act", lambda e, fc=fc, dst=dst: e.activation(out=dst, in_=sg[:, fc, :], func=AF.Identity,
                                                                       scale=der[:, SUBS:SUBS + 1]),
                         r=[sk, "der"], w=[("Wo", fc, pi)])
            else:
                if fc % 2 == 0:
                    P.op("dve", lambda e, fc=fc, dst=dst: e.tensor_copy(out=dst, in_=sg[:, fc, :]), r=[sk], w=[("Wo", fc, pi)])
                else:
                    P.op("act", lambda e, fc=fc, dst=dst: e.activation(out=dst, in_=sg[:, fc, :], func=AF.Copy),
                         r=[sk], w=[("Wo", fc, pi)])

    for hp in range(6):
        sgv = cstg[0][:, :, (hp % 2) * 256:(hp % 2) * 256 + 256]
        sk = ("cstgh", hp % 2)
        P.op("sp", lambda e, sgv=sgv, hp=hp: e.dma_start(out=sgv, in_=w_in_src[:, :, 1536 + hp * 256:1536 + (hp + 1) * 256]),
             w=[sk], dma=True)
        cast_piece(sgv, sk, Wc, hp * 256, der[:, GSC1:GSC1 + 8], width=256)
        bias_pp(sgv, sk, SH1, bias_c, hp * 2, 6, ("bias_c", hp), brow_c, 7, width=256)
    BCK = [("bias_c", hp) for hp in range(6)]
    WCK = [("wdst", id(Wc), fc, hp * 256) for fc in range(8) for hp in range(6)]
    def conv_norm(i):
        b = i % 2
        pp = (i // 2) % 2
        own_norm(i, xos2[b], sqo2[b], rso2[b], hnp[pp][:, :, i % 2, :], b, hnkey=("hnp", pp, i % 2), sq_on_act=True)

    conv_norm(0)
    conv_norm(1)
    for p_ in range(NOWN // 2):
        pp = p_ % 2
        if p_ + 1 < NOWN // 2:
            conv_norm(2 * p_ + 2)
            conv_norm(2 * p_ + 3)
        if 1 <= p_ <= 4:
            load_x2T([2 * (p_ - 1), 2 * (p_ - 1) + 1])
        if p_ == 1:
            load_wo(0)
        if p_ == 3:
            load_wo(1)
        for cc in range(4):
            g = cc % 2
            bks = (2, 3, 4) if g == 0 else (5, 6, 7)

            def mmc(e, pp=pp, cc=cc, bks=bks):
                ins = None
                for br in range(3):
                    for fc in range(8):
                        ins = e.matmul(psb[bks[br]][:, 0:260], lhsT=Wc[:, fc, br * 512 + cc * 128:br * 512 + (cc + 1) * 128],
                                       rhs=hnp[pp][:, fc, :, :], start=(fc == 0), stop=(fc == 7))
                return ins
            P.op("pe", mmc, r=[("hnp", pp, 0), ("hnp", pp, 1)] + WCK, w=[PSK(bk) for bk in bks])

            def v3(ap_):
                return ap_.rearrange("p (j t) -> p j t", j=2)
            P.op("act", lambda e, g=g, bks=bks, cc=cc: e.activation(out=gcs[g], in_=v3(psb[bks[1]][:, 0:260]), func=AF.Identity,
                                                                    bias=bias_c[:, 4 + cc:5 + cc], scale=1.0),
                 r=[PSK(bks[1])] + BCK, w=[("gcs", g)])
            P.op("act", lambda e, g=g, bks=bks, cc=cc: e.activation(out=hcs[g], in_=v3(psb[bks[2]][:, 0:260]), func=AF.Identity,
                                                                    bias=bias_c[:, 8 + cc:9 + cc], scale=1.0),
                 r=[PSK(bks[2])] + BCK, w=[("hcs", g)])
            P.op("pool", lambda e, g=g: e.tensor_tensor(out=ut[g], in0=gcs[g], in1=hcs[g], op=ALU.mult),
                 r=[("gcs", g), ("hcs", g)], w=[("ut", g)])
            if p_ == 0:
                P.op("dve", lambda e, g=g: e.tensor_scalar(out=ut[g][:, 0, 0:2], in0=ut[g][:, 0, 0:2], scalar1=vec[:, HM:HM + 1],
                                                           scalar2=None, op0=ALU.mult), r=[("ut", g), "vec"], w=[("ut", g)])
            P.op("act", lambda e, g=g, cc=cc: e.activation(out=tt_[g], in_=ut[g][:, :, 2:130], func=AF.Identity,
                                                           scale=vec[:, CW + cc * 3 + 2:CW + cc * 3 + 3]),
                 r=[("ut", g), "vec"], w=[("tt", g)])
            P.op("dve", lambda e, g=g, cc=cc: e.scalar_tensor_tensor(out=tt_[g], in0=ut[g][:, :, 1:129],
                                                                      scalar=vec[:, CW + cc * 3 + 1:CW + cc * 3 + 2],
                                                                      in1=tt_[g], op0=ALU.mult, op1=ALU.add),
                 r=[("ut", g), "vec", ("tt", g)], w=[("tt", g)])
            P.op("dve", lambda e, g=g, cc=cc: e.scalar_tensor_tensor(out=tt_[g], in0=ut[g][:, :, 0:128],
                                                                      scalar=vec[:, CW + cc * 3:CW + cc * 3 + 1],
                                                                      in1=tt_[g], op0=ALU.mult, op1=ALU.add),
                 r=[("ut", g), "vec", ("tt", g)], w=[("tt", g)])
            for j2 in range(2):
                P.op("dve", lambda e, g=g, cc=cc, bks=bks, p_=p_, j2=j2: e.scalar_tensor_tensor(
                    out=convT[:, cc, (2 * p_ + j2) * 128:(2 * p_ + j2 + 1) * 128],
                    in0=psb[bks[0]][:, j2 * 130 + 2:j2 * 130 + 130], scalar=bias_c[:, cc:cc + 1], in1=tt_[g][:, j2, :],
                    op0=ALU.add, op1=ALU.mult), r=[PSK(bks[0]), ("tt", g)] + BCK, w=[("convT", 2 * p_ + j2, cc)])

    dump("convT", convT, BF16)
    for t in range(4):
        tsl = slice(t * 512, (t + 1) * 512)
        for dc in range(8):
            psi = 6 + (dc % 2)

            def mmo(e, dc=dc, psi=psi, tsl=tsl):
                ins = None
                for fc in range(8):
                    rhs = attnT[:, fc, tsl] if fc < 4 else convT[:, fc - 4, tsl]
                    ins = e.matmul(psb[psi][:, :], lhsT=Wo[:, fc, dc * 128:(dc + 1) * 128], rhs=rhs, start=(fc == 0),
                                   stop=(fc == 7))
                return ins
            P.op("pe", mmo, r=[("attnT", ii) for ii in range(4 * t, 4 * t + 4)] + [("convT", ii, cc) for ii in range(4 * t, 4 * t + 4) for cc in range(4)]
                 + [("Wo", fc, dc // 4) for fc in range(8)], w=[PSK(psi)])
            P.op("dve", lambda e, dc=dc, psi=psi, tsl=tsl: e.scalar_tensor_tensor(
                out=x2T[:, dc, tsl], in0=psb[psi][:, :], scalar=adaT[:, G1 + dc:G1 + dc + 1], in1=x2T[:, dc, tsl],
                op0=ALU.mult, op1=ALU.add), r=[PSK(psi), ("x2T", dc), "adaT"], w=[("x2T", dc)])
    P.barrier()
    dump("x2T", x2T)

    RD = Region(8 * K + 64 * K, ARENA)
    h2T = RD.alloc([128, 8, 2048], BF16)
    We = [[RD.alloc([128, 8, 512], BF16), RD.alloc([128, 8, 512], BF16), RD.alloc([128, 4, 1024], BF16)] for _ in range(2)]
    estg = [RD.alloc([128, 4, 512], F32) for _ in range(2)]
    gatT = RD.alloc([16, 2048], BF16)
    sel = RD.alloc([16, 16, 128], BF16)
    rwf = RD.alloc([128, 8, 20], F32)
    rwb = RD.alloc([128, 8, 20], BF16)
    UNION0 = RD.cur
    sq2 = RD.alloc([128, 8, 512], BF16)
    rs2_l = [RD.alloc([128, 512], F32) for _ in range(2)]
    htmp = [RD.alloc([128, 512], F32) for _ in range(3)]
    rl = RD.alloc([128, 16, 20], F32)
    gat = RD.alloc([128, 16, 16], F32)
    ml = RD.alloc([128, 16, 16], F32)
    ml2 = RD.alloc([128, 16, 16], F32)
    oh1 = RD.alloc([128, 16, 16], F32)
    oh2 = RD.alloc([128, 16, 16], F32)
    g4 = RD.alloc([128, 16, 4], F32)
    g4b = RD.alloc([128, 16, 4], F32)
    sc = RD.alloc([128, 8, 16], F32)
    RU = Region(UNION0, ARENA)
    hid = [[RU.alloc([128, 512], BF16) for _ in range(4)] for _ in range(2)]
    sl = [RU.alloc([128, 512], F32) for _ in range(2)]
    vv = [RU.alloc([128, 512], F32) for _ in range(2)]
    sq_f = RU.alloc([128, 8, 512], BF16)
    rs_f = RU.alloc([128, 512], F32)

    P.op("sp", lambda e: e.dma_start(out=rwf, in_=rw.rearrange("(fc p) n -> p fc n", p=128)), w=["rwf"], dma=True)
    P.op("sp", lambda e: e.dma_start(out=rbias_b, in_=rb[0:1, :].partition_broadcast(128)), w=["rbias_b"], dma=True)
    P.op("dve", lambda e: e.tensor_copy(out=rwb, in_=rwf), r=["rwf"], w=["rwb"])
    P.op("pool", lambda e: e.tensor_copy(out=sel, in_=ident[0:16, 0:16].unsqueeze(2).to_broadcast([16, 16, 128])),
         r=["ident"], w=["sel"])

    def stats_tile(t, par, psi, eps):
        tsl = slice(t * 512, (t + 1) * 512)
        rs2 = rs2_l[par]
        for fc in range(8):
            P.op("act", lambda e, fc=fc: e.activation(out=sq2[:, fc, :], in_=x2T[:, fc, tsl], func=AF.Square),
                 r=[("x2T", fc)], w=[("sq2", fc)])

        def mm(e):
            ins = None
            for fc in range(8):
                ins = e.matmul(psb[psi][:, :], lhsT=ones_bf, rhs=sq2[:, fc, :], start=(fc == 0), stop=(fc == 7))
            return ins
        P.op("pe", mm, r=[("sq2", fc) for fc in range(8)] + ["ones_bf"], w=[PSK(psi)])
        P.op("act", lambda e: e.activation(out=rs2, in_=psb[psi][:, :], func=AF.Sqrt, bias=eps, scale=1.0),
             r=[PSK(psi)], w=[("rs2", par)])
        P.op("dve", lambda e: e.reciprocal(out=rs2, in_=rs2), r=[("rs2", par)], w=[("rs2", par)])

    def route_section():
        stats_tile(0, 0, 1, NORM_EPS)
        for t in range(4):
            tsl = slice(t * 512, (t + 1) * 512)
            if t + 1 < 4:
                stats_tile(t + 1, (t + 1) % 2, (1, 4)[(t + 1) % 2], NORM_EPS)
            rs2 = rs2_l[t % 2]
            for fc in range(8):
                g = fc % 3
                eng = "dve"
                P.op(eng, lambda e, tsl=tsl, fc=fc, g=g, rs2=rs2: e.tensor_tensor(out=htmp[g], in0=x2T[:, fc, tsl], in1=rs2,
                                                                                 op=ALU.mult),
                     r=[("x2T", fc), ("rs2", t % 2)], w=[("htmp", g)])
                P.op("act", lambda e, tsl=tsl, fc=fc, g=g: e.activation(
                    out=h2T[:, fc, tsl], in_=htmp[g], func=AF.Identity, bias=adaT[:, SH2 + fc:SH2 + fc + 1],
                    scale=der[:, GSC2 + fc:GSC2 + fc + 1]),
                    r=[("htmp", g), "adaT", "der"], w=[("h2T", t, fc)])

            def mmr(e, t=t):
                ins = None
                for sub in range(4):
                    st = t * 4 + sub
                    for fc in range(8):
                        ins = e.matmul(psb[2][:, st * 32:st * 32 + 20], lhsT=h2T[:, fc, st * 128:(st + 1) * 128], rhs=rwb[:, fc, :],
                                       start=(fc == 0), stop=(fc == 7))
                return ins
            P.op("pe", mmr, r=[("h2T", t, fc) for fc in range(8)] + ["rwb"], w=[("psR", t)])
            if t == 0:
                chunk_cast(0, 0, eng="act")
                chunk_cast(0, 1, eng="act")
                chunk_dma(0, 2)
                chunk_dma(0, 3)
            if t == 2:
                chunk_cast(0, 2, eng="act")
                chunk_cast(0, 3, eng="act")
                chunk_dma(0, 4)
                chunk_dma(0, 5)
        chunk_cast(0, 4, eng="act")
        chunk_cast(0, 5, eng="act")
        PSR = [("psR", t) for t in range(4)]
        P.op("dve", lambda e: e.tensor_tensor(out=rl, in0=psb[2][:, :].rearrange("p (s c) -> p s c", c=32)[:, :, 0:20],
                                              in1=rbias_b.unsqueeze(1).to_broadcast([128, 16, 20]), op=ALU.add),
             r=PSR + ["rbias_b"], w=["rl"])
        gl = rl[:, :, 0:4]
        el = rl[:, :, 4:20]
        R = "rt"
        P.op("dve", lambda e: e.tensor_reduce(out=sc[:, 0, :], in_=gl, axis=AX.X, op=ALU.max), r=["rl"], w=[R])
        P.op("dve", lambda e: e.tensor_tensor(out=g4, in0=gl, in1=sc[:, 0, :].unsqueeze(2).to_broadcast([128, 16, 4]),
                                              op=ALU.subtract), r=["rl", R], w=[R])
        P.op("act", lambda e: e.activation(out=g4b, in_=g4, func=AF.Exp), r=[R], w=[R])
        P.op("dve", lambda e: e.tensor_reduce(out=sc[:, 1, :], in_=g4b, axis=AX.X, op=ALU.add), r=[R], w=[R])
        P.op("dve", lambda e: e.reciprocal(out=sc[:, 1, :], in_=sc[:, 1, :]), r=[R], w=[R])
        P.op("dve", lambda e: e.tensor_scalar(out=g4, in0=g4, scalar1=0.0, scalar2=None, op0=ALU.is_ge), r=[R], w=[R])
        P.op("dve", lambda e: e.tensor_scalar(out=g4, in0=g4, scalar1=-1.0, scalar2=BIG, op0=ALU.add, op1=ALU.mult),
             r=[R], w=[R])
        P.op("dve", lambda e: e.tensor_tensor(out=ml.rearrange("p s (g x) -> p s g x", g=4),
                                              in0=el.rearrange("p s (g x) -> p s g x", g=4),
                                              in1=g4.unsqueeze(3).to_broadcast([128, 16, 4, 4]), op=ALU.add),
             r=["rl", R], w=[R])
        P.op("dve", lambda e: e.tensor_reduce(out=sc[:, 2, :], in_=ml, axis=AX.X, op=ALU.max), r=[R], w=[R])
        P.op("dve", lambda e: e.tensor_tensor(out=oh1, in0=ml, in1=sc[:, 2, :].unsqueeze(2).to_broadcast([128, 16, 16]),
                                              op=ALU.is_ge), r=[R], w=[R])
        P.op("dve", lambda e: e.scalar_tensor_tensor(out=ml2, in0=oh1, scalar=-BIG, in1=ml, op0=ALU.mult, op1=ALU.add),
             r=[R], w=[R])
        P.op("dve", lambda e: e.tensor_reduce(out=sc[:, 3, :], in_=ml2, axis=AX.X, op=ALU.max), r=[R], w=[R])
        P.op("dve", lambda e: e.tensor_tensor(out=oh2, in0=ml2, in1=sc[:, 3, :].unsqueeze(2).to_broadcast([128, 16, 16]),
                                              op=ALU.is_ge), r=[R], w=[R])
        P.op("dve", lambda e: e.tensor_tensor(out=sc[:, 4, :], in0=sc[:, 3, :], in1=sc[:, 2, :], op=ALU.subtract),
             r=[R], w=[R])
        P.op("act", lambda e: e.activation(out=sc[:, 4, :], in_=sc[:, 4, :], func=AF.Exp), r=[R], w=[R])
        P.op("dve", lambda e: e.tensor_scalar(out=sc[:, 4, :], in0=sc[:, 4, :], scalar1=1.0, scalar2=None, op0=ALU.add),
             r=[R], w=[R])
        P.op("dve", lambda e: e.reciprocal(out=sc[:, 4, :], in_=sc[:, 4, :]), r=[R], w=[R])
        P.op("dve", lambda e: e.tensor_tensor(out=sc[:, 5, :], in0=sc[:, 4, :], in1=sc[:, 1, :], op=ALU.mult), r=[R], w=[R])
        P.op("dve", lambda e: e.tensor_tensor(out=sc[:, 6, :], in0=sc[:, 1, :], in1=sc[:, 5, :], op=ALU.subtract),
             r=[R], w=[R])
        P.op("dve", lambda e: e.tensor_tensor(out=gat, in0=oh1, in1=sc[:, 5, :].unsqueeze(2).to_broadcast([128, 16, 16]),
                                              op=ALU.mult), r=[R], w=[R])
        P.op("dve", lambda e: e.tensor_tensor(out=oh2, in0=oh2, in1=sc[:, 6, :].unsqueeze(2).to_broadcast([128, 16, 16]),
                                              op=ALU.mult), r=[R], w=[R])
        P.op("dve", lambda e: e.tensor_tensor(out=gat, in0=gat, in1=oh2, op=ALU.add), r=[R], w=[R])
        for t in range(4):
            def mmg(e, t=t):
                ins = None
                for sub in range(4):
                    st = t * 4 + sub
                    ins = e.transpose(psb[3][0:16, sub * 128:(sub + 1) * 128], gat[:, st, :], ident)
                return ins
            P.op("pe", mmg, r=[R, "ident"], w=[PSK(3)])
            P.op("act", lambda e, t=t: e.activation(out=gatT[:, t * 512:(t + 1) * 512], in_=psb[3][0:16, :], func=AF.Copy),
                 r=[PSK(3)], w=[("gatT", t)])
        dump("gat", gat)
        dump("h2T", h2T, BF16)

    wg_src = wg.rearrange("x (fc p) n -> p x fc n", p=128)
    wu_src = wu.rearrange("x (fc p) n -> p x fc n", p=128)
    wd_src = wd.rearrange("x (jc p) n -> p x jc n", p=128)
    stg_n = [0]

    def chunk_spec(ex, k):
        wb = We[ex % 2]
        if k < 4:
            nm, hf = k // 2, k % 2
            src = (wg_src, wu_src)[nm][:, ex, hf * 4:(hf + 1) * 4, :]
            dsts = [(wb[nm][:, hf * 4 + f4, :], ("We", ex % 2, nm, hf * 4 + f4)) for f4 in range(4)]
        else:
            hf = k - 4
            src = wd_src[:, ex, :, hf * 512:(hf + 1) * 512]
            dsts = [(wb[2][:, jc, hf * 512:(hf + 1) * 512], ("We", ex % 2, 2, jc, hf)) for jc in range(4)]
        return src, dsts

    def chunk_dma(ex, k):
        src, dsts = chunk_spec(ex, k)
        sg = estg[k % 2]
        P.op("sp", lambda e: e.dma_start(out=sg, in_=src), w=[("estg", k % 2)], dma=True)

    def chunk_cast(ex, k, eng="act"):
        src, dsts = chunk_spec(ex, k)
        sg = estg[k % 2]
        for f4, (dst, key) in enumerate(dsts):
            if eng == "act":
                P.op("act", lambda e, dst=dst, f4=f4: e.activation(out=dst, in_=sg[:, f4, :], func=AF.Copy),
                     r=[("estg", k % 2)], w=[key])
            else:
                P.op(eng, lambda e, dst=dst, f4=f4: e.tensor_copy(out=dst, in_=sg[:, f4, :]), r=[("estg", k % 2)], w=[key])

    def expert_load(ex):
        for k in range(6):
            chunk_dma(ex, k)
            chunk_cast(ex, k, eng=("act", "dve")[k % 2])

    PREF = {4: [("d", 0), ("d", 1)], 6: [("c", 0), ("d", 2)], 8: [("c", 1), ("d", 3)], 10: [("c", 2), ("d", 4)],
            12: [("c", 3), ("d", 5)], 14: [("c", 4)], 15: [("c", 5)]}

    def prefetch_step(ex, sub):
        for kind, k in PREF.get(sub, []):
            if kind == "d":
                chunk_dma(ex, k)
            else:
                chunk_cast(ex, k)

    def g_step(ex, t, jc):
        wb = We[ex % 2]
        WK = [("We", ex % 2, nm, fc) for nm in range(2) for fc in range(8)]
        tsl = slice(t * 512, (t + 1) * 512)
        hb = (ex * 4 + t) % 2
        H2K = [("h2T", t, fc) for fc in range(8)]
        if jc == 0:
            P.op("pe", lambda e: e.matmul(psb[1][:, :], lhsT=sel[:, ex, :], rhs=gatT[:, tsl], start=True, stop=True),
                 r=["sel", ("gatT", t)], w=[PSK(1)])
        pa = 2 + (jc % 2) * 2
        pu = pa + 1

        def mmgu(e):
            ins = None
            for fc in range(8):
                e.matmul(psb[pa][:, :], lhsT=wb[0][:, fc, jc * 128:(jc + 1) * 128], rhs=h2T[:, fc, tsl],
                         start=(fc == 0), stop=(fc == 7))
            for fc in range(8):
                ins = e.matmul(psb[pu][:, :], lhsT=wb[1][:, fc, jc * 128:(jc + 1) * 128], rhs=h2T[:, fc, tsl],
                               start=(fc == 0), stop=(fc == 7))
            return ins
        P.op("pe", mmgu, r=WK + H2K, w=[PSK(pa), PSK(pu)])
        g = jc % 2
        P.op("act", lambda e: e.activation(out=sl[g], in_=psb[pa][:, :], func=AF.Silu), r=[PSK(pa)], w=[("sl", g)])
        P.op("dve", lambda e: e.tensor_tensor(out=vv[g], in0=psb[pu][:, :], in1=sl[g], op=ALU.mult),
             r=[PSK(pu), ("sl", g)], w=[("vv", g)])
        P.op("dve", lambda e: e.tensor_tensor(out=hid[hb][jc], in0=vv[g], in1=psb[1][:, :], op=ALU.mult),
             r=[("vv", g), PSK(1)], w=[("hid", hb, jc)])

    def d_step(ex, t, dc):
        wb = We[ex % 2]
        WDK = [("We", ex % 2, 2, jc, hf) for jc in range(4) for hf in range(2)]
        tsl = slice(t * 512, (t + 1) * 512)
        hb = (ex * 4 + t) % 2
        py = 6 + (dc % 2)

        def mmd(e):
            ins = None
            for jc in range(4):
                ins = e.matmul(psb[py][:, :], lhsT=wb[2][:, jc, dc * 128:(dc + 1) * 128], rhs=hid[hb][jc],
                               start=(jc == 0), stop=(jc == 3))
            return ins
        P.op("pe", mmd, r=WDK + [("hid", hb, jc) for jc in range(4)], w=[PSK(py)])
        P.op("dve", lambda e: e.scalar_tensor_tensor(out=x2T[:, dc, tsl], in0=psb[py][:, :], scalar=adaT[:, G2 + dc:G2 + dc + 1],
                                                      in1=x2T[:, dc, tsl], op0=ALU.mult, op1=ALU.add),
             r=[PSK(py), ("x2T", dc), "adaT"], w=[("x2T", dc)])

    NE = 16
    chunk_dma(0, 0)
    chunk_dma(0, 1)
    route_section()
    P.barrier()
    units = [(ex, t) for ex in range(NE) for t in range(4)]
    yT_src = yT.rearrange("(dc p) t -> p dc t", p=128)

    def final_stats(t):
        tsl = slice(t * 512, (t + 1) * 512)
        for fc in range(8):
            P.op("act", lambda e, fc=fc: e.activation(out=sq_f[:, fc, :], in_=x2T[:, fc, tsl], func=AF.Square),
                 r=[("x2T", fc)], w=[("sqf", fc)])

        def mm(e):
            ins = None
            for fc in range(8):
                ins = e.matmul(psb[0][:, :], lhsT=ones_bf, rhs=sq_f[:, fc, :], start=(fc == 0), stop=(fc == 7))
            return ins
        P.op("pe", mm, r=[("sqf", fc) for fc in range(8)] + ["ones_bf"], w=[PSK(0)])
        P.op("act", lambda e: e.activation(out=rs_f, in_=psb[0][:, :], func=AF.Sqrt, bias=NORM_EPS, scale=1.0),
             r=[PSK(0)], w=["rs_f"])
        P.op("dve", lambda e: e.reciprocal(out=rs_f, in_=rs_f), r=["rs_f"], w=["rs_f"])

    def final_apply(t):
        tsl = slice(t * 512, (t + 1) * 512)
        for dc in range(8):
            P.op("dve", lambda e, dc=dc: e.scalar_tensor_tensor(
                out=x2T[:, dc, tsl], in0=x2T[:, dc, tsl], scalar=vec[:, FG + dc:FG + dc + 1], in1=rs_f,
                op0=ALU.mult, op1=ALU.mult), r=[("x2T", dc), "rs_f", "vec"], w=[("x2T", dc), ("x2Tout", t, dc)])
        P.op("sp", lambda e: e.dma_start(out=yT_src[:, :, tsl], in_=x2T[:, :, tsl]),
             r=[("x2Tout", t, fc) for fc in range(8)], dma=True)

    for u in range(len(units) + 1):
        if u < len(units):
            ex, t = units[u]
        prev = units[u - 1] if u >= 1 else None
        for jc in range(4):
            if u < len(units):
                g_step(ex, t, jc)
            if prev is not None:
                d_step(prev[0], prev[1], 2 * jc)
                d_step(prev[0], prev[1], 2 * jc + 1)
            if u < len(units) and ex + 1 < NE:
                prefetch_step(ex + 1, t * 4 + jc)
        if prev is not None and prev[0] == NE - 1:
            if prev[1] >= 1:
                final_apply(prev[1] - 1)
            final_stats(prev[1])

    final_apply(3)
    P.op("sp", None, after=[o["idx"] for o in P.ops if o["dma"]][-8:])
    P.emit(nc, es)
    es.close()
    return nc, list(dbg_out.keys())


_CACHE = {}


def _host_inputs(x, c, positions, rel_bias, ada_w, ada_b, norm1_g, w_in, lambda_q1, lambda_k1, lambda_q2, lambda_k2,
                 subln_g, conv_w, w_out, norm2_g, router_group_w, router_group_b, router_expert_w, router_expert_b,
                 expert_w_gate, expert_w_up, expert_w_down, final_g):
    f = lambda a: np.ascontiguousarray(np.asarray(a, dtype=np.float32))
    x = f(x)
    pk = lambda v: f(np.asarray(v, np.float32).reshape(-1, 128).T)
    shared = dict(
        adab=pk(ada_b[0]), ada_w=f(ada_w[0]), w_in=f(w_in[0]), w_out=f(w_out[0]),
        lam4=f(np.concatenate([np.asarray(lambda_q1[0]), np.asarray(lambda_k1[0]), np.asarray(lambda_q2[0]),
                               np.asarray(lambda_k2[0])]).reshape(1, 256)),
        rw=f(np.concatenate([np.asarray(router_group_w[0]), np.asarray(router_expert_w[0])], axis=1)),
        rb=f(np.concatenate([np.asarray(router_group_b[0]), np.asarray(router_expert_b[0])]).reshape(1, 20)),
        relb=f(rel_bias), relb31=f(np.asarray(rel_bias)[31].reshape(8, 1)),
        wg=f(expert_w_gate[0]), wu=f(expert_w_up[0]), wd=f(expert_w_down[0]),
        ident=np.eye(128, dtype=np.float32), antiid=np.ascontiguousarray(np.eye(128, dtype=np.float32)[::-1]),
        lo=np.asarray(LO, np.float32).reshape(32, 1),
    )
    cw = np.asarray(conv_w[0], np.float32)
    convw = cw.T.reshape(4, 128, 3).transpose(1, 0, 2).reshape(128, 12)
    in_maps = []
    xTs = [np.ascontiguousarray(x[b].T) for b in range(2)]
    for core in range(8):
        b, j = core // 4, core % 4
        xo = np.zeros((DM, NOWN, 130), np.float32)
        for i in range(NOWN):
            s = (4 * i + j) * 128
            if s == 0:
                xo[:, i, 2:] = xTs[b][:, 0:128]
            else:
                xo[:, i, :] = xTs[b][:, s - 2:s + 128]
        vecs = np.zeros((128, 48), np.float32)
        vecs[:, 0:8] = pk(np.asarray(c)[b])
        vecs[:, 8:16] = pk(norm1_g[0])
        vecs[:, 16:24] = pk(norm2_g[0])
        vecs[:, 24:32] = pk(final_g)
        vecs[:, 32:44] = convw
        vecs[:, 44] = 0.0 if j == 0 else 1.0
        vecs[:, 45] = np.asarray(subln_g[0], np.float32)
        dvec = (np.arange(768, dtype=np.float32) - 639.0 + (j + 1) * 128.0).reshape(1, 768)
        m = dict(shared)
        m.update(xT=xTs[b], xo=xo, vecs=vecs, dvec=dvec)
        in_maps.append(m)
    return in_maps


def kernel(**inputs):
    if "nc" not in _CACHE:
        _CACHE["nc"] = build_program()[0]
    nc = _CACHE["nc"]
    in_maps = _host_inputs(**inputs)
    res = run_bass_kernel_spmd(nc, in_maps, core_ids=list(range(8)))
    y = np.zeros((2, S, DM), np.float32)
    for core in range(8):
        b, j = core // 4, core % 4
        yT = np.asarray(res.results[core]["yT"])
        yv = yT.T.reshape(NOWN, 128, DM)
        for i in range(NOWN):
            s = (4 * i + j) * 128
            y[b, s:s + 128, :] = yv[i]
    return y
```

```python
import contextlib
import math

import numpy as np
import concourse.bass as bass
import concourse.mybir as mybir
from concourse.bass_utils import run_bass_kernel_spmd

F32 = mybir.dt.float32
BF16 = mybir.dt.bfloat16
U8 = mybir.dt.uint8
AF = mybir.ActivationFunctionType
ALU = mybir.AluOpType
AX = mybir.AxisListType

S = 8192
DM = 1024
NOWN = 16
LAM_INIT = 0.8 - 0.6 * math.exp(0.0)
NORM_EPS = 1e-6
SUBLN_EPS = 1e-5
BIG = 1.0e30
LO = [0, 1, 2, 3, 4, 5, 6, 7, 8, 9, 10, 11, 12, 13, 14, 15, 16, 19, 21, 24, 27, 31, 35, 40, 46, 52,
      59, 67, 77, 87, 99, 113]


class Prog:
    ENGS = ("pe", "act", "dve", "pool", "sp")

    def __init__(self, n_dma_sems=48):
        self.ops = []
        self.lw = {}
        self.rd = {}
        self.n_dma_sems = n_dma_sems
        self.bar = set()
        self.dma_since = []
        self.last_eng = {}

    def op(self, eng, fn, r=(), w=(), dma=False, after=()):
        idx = len(self.ops)
        deps = set(after) | self.bar
        for b in r:
            x = self.lw.get(b)
            if x is not None:
                deps.add(x)
        for b in w:
            x = self.lw.get(b)
            if x is not None:
                deps.add(x)
            deps.update(self.rd.get(b, ()))
        for b in r:
            self.rd.setdefault(b, []).append(idx)
        for b in w:
            self.lw[b] = idx
            self.rd[b] = []
        deps.discard(idx)
        self.ops.append(dict(eng=eng, fn=fn, deps=deps, dma=dma, idx=idx))
        if dma:
            self.dma_since.append(idx)
        elif fn is not None:
            self.last_eng[eng] = idx
        return idx

    def barrier(self):
        self.bar = set(self.dma_since) | set(self.last_eng.values())
        self.dma_since = []

    def emit(self, nc, es):
        ops = self.ops
        needed = set()
        for o in ops:
            needed.update(o["deps"])
        eng_sem = {e: es.enter_context(nc.semaphore("s_" + e)) for e in self.ENGS}
        dma_sems = [es.enter_context(nc.semaphore("d%d" % i)) for i in range(self.n_dma_sems)]
        cnt = {e: 0 for e in self.ENGS}
        dcnt = [0] * self.n_dma_sems
        dma_rr = 0
        ev = {}
        for o in ops:
            o["pre"] = []
            if o["dma"]:
                k = dma_rr % self.n_dma_sems
                dma_rr += 1
                if dcnt[k] > 0:
                    o["pre"].append((("d", k), dcnt[k]))
                o["dsem"] = k
                dcnt[k] += 16
                ev[o["idx"]] = (("d", k), dcnt[k])
            elif o["idx"] in needed and o["fn"] is not None:
                cnt[o["eng"]] += 1
                ev[o["idx"]] = (("e", o["eng"]), cnt[o["eng"]])
        per_eng = {e: [] for e in self.ENGS}
        for o in ops:
            per_eng[o["eng"]].append(o)

        def semh(key):
            return eng_sem[key[1]] if key[0] == "e" else dma_sems[key[1]]

        def run(engname, e):
            waited = {}
            for o in per_eng[engname]:
                best = {}
                for key, val in o["pre"]:
                    if val > best.get(key, 0):
                        best[key] = val
                for d in o["deps"]:
                    if d in ev:
                        key, val = ev[d]
                        if val > best.get(key, 0):
                            best[key] = val
                for key, val in best.items():
                    if engname == "pe" and key == ("e", "pe"):
                        continue
                    if waited.get(key, 0) < val:
                        e.wait_ge(semh(key), val)
                        waited[key] = val
                if o["fn"] is None:
                    continue
                ins = o["fn"](e)
                if o["dma"]:
                    ins.then_inc(dma_sems[o["dsem"]], 16)
                elif o["idx"] in ev:
                    ins.then_inc(eng_sem[engname], 1)

        block = es.enter_context(nc.Block())

        @block.tensor
        def _(e):
            run("pe", e)

        @block.scalar
        def _(e):
            run("act", e)

        @block.vector
        def _(e):
            run("dve", e)

        @block.gpsimd
        def _(e):
            run("pool", e)

        @block.sync
        def _(e):
            run("sp", e)


def build_program(dbg=()):
    nc = bass.Bass("TRN2", target_bir_lowering=False)
    P = Prog()

    def DI(name, shape, dt=F32):
        return nc.dram_tensor(name, list(shape), dt, kind="ExternalInput").ap()

    xT = DI("xT", [DM, S])
    xo = DI("xo", [DM, NOWN, 130])
    vecs = DI("vecs", [128, 48])
    adab = DI("adab", [128, 48])
    ada_w = DI("ada_w", [DM, 6 * DM])
    w_in = DI("w_in", [DM, 3072])
    w_out = DI("w_out", [DM, DM])
    lam4 = DI("lam4", [1, 256])
    rw = DI("rw", [DM, 20])
    rb = DI("rb", [1, 20])
    relb = DI("relb", [32, 8])
    relb31 = DI("relb31", [8, 1])
    wg = DI("wg", [16, DM, 512])
    wu = DI("wu", [16, DM, 512])
    wd = DI("wd", [16, 512, DM])
    ident_d = DI("ident", [128, 128])
    antiid_d = DI("antiid", [128, 128])
    lo_d = DI("lo", [32, 1])
    dvec_d = DI("dvec", [1, 768])
    yT = nc.dram_tensor("yT", [DM, 2048], F32, kind="ExternalOutput").ap()
    wscr = nc.dram_tensor("wscr", [8, 768], F32).ap()
    mscr = nc.dram_tensor("mscr", [128, 5 * 1024], BF16).ap()
    dbg_out = {}

    es = contextlib.ExitStack()
    ARENA = 208896
    arena = nc.alloc_sbuf_tensor("arena", [128, ARENA], U8)

    def carve(off, shape, dt):
        n = int(np.prod(shape[1:]))
        bpe = 4 if dt == F32 else 2
        assert off % 4 == 0 and off + n * bpe <= ARENA, (off, shape)
        v = arena[0:shape[0], off:off + n * bpe].bitcast(dt)
        if len(shape) == 3:
            v = v.rearrange("p (a b) -> p a b", a=shape[1])
        elif len(shape) == 4:
            v = v.rearrange("p (a b c) -> p a b c", a=shape[1], b=shape[2])
        return v

    class Region:
        def __init__(self, lo, hi):
            self.lo, self.hi, self.cur = lo, hi, lo

        def alloc(self, shape, dt):
            n = int(np.prod(shape[1:])) * (4 if dt == F32 else 2)
            n = (n + 63) // 64 * 64
            off = self.cur
            self.cur += n
            assert self.cur <= self.hi, ("region overflow", self.cur, self.hi)
            return carve(off, shape, dt)

    K = 1024
    RC = Region(0, 8 * K)
    ident = RC.alloc([128, 128], F32)
    ones_bf = RC.alloc([128, 128], BF16)
    ones_f = RC.alloc([128, 128], F32)
    vec = RC.alloc([128, 48], F32)
    adaT = RC.alloc([128, 48], F32)
    der = RC.alloc([128, 32], F32)
    bias_q = RC.alloc([128, 4], F32)
    bias_k = RC.alloc([128, 4], F32)
    bias_c = RC.alloc([128, 12], F32)
    bias_vb = RC.alloc([128, 512], F32)
    rbias_b = RC.alloc([128, 20], F32)
    bge = RC.alloc([128, 16, 8], F32)
    CP, N1G, N2G, FG, CW, HM, SUBG = 0, 8, 16, 24, 32, 44, 45
    SH1, SC1, G1, SH2, SC2, G2 = 0, 8, 16, 24, 32, 40
    GSC1, GSC2, LAMC, NLAM, SUBS = 0, 8, 16, 17, 18

    psd = [es.enter_context(nc.psum_tensor("psd%d" % i, [128, 1024], F32)) for i in range(4)]
    psb = [psd[i // 2][:, (i % 2) * 512:(i % 2 + 1) * 512] for i in range(8)]

    def PSK(i):
        return ("ps", i)

    def dump(name, ap_, dt=F32):
        if name not in dbg:
            return
        P.barrier()
        shp = list(ap_.shape)
        d = nc.dram_tensor("dbg_" + name, shp, dt, kind="ExternalOutput").ap()
        dbg_out[name] = d
        P.op("sp", lambda e: e.dma_start(out=d, in_=ap_), dma=True)
        P.barrier()

    P.op("sp", lambda e: e.dma_start(out=ident, in_=ident_d[:, :]), w=["ident"], dma=True)
    P.op("sp", lambda e: e.dma_start(out=vec, in_=vecs[:, :]), w=["vec"], dma=True)
    P.op("sp", lambda e: e.dma_start(out=adaT, in_=adab[:, :]), w=["adaT"], dma=True)
    P.op("pool", lambda e: e.memset(ones_bf, 1.0 / 1024.0), w=["ones_bf"])
    P.op("pool", lambda e: e.memset(ones_f, 1.0), w=["ones_f"])
    P.op("act", lambda e: e.activation(out=vec[:, CP:CP + 8], in_=vec[:, CP:CP + 8], func=AF.Silu),
         r=["vec"], w=["vec"])

    RS = Region(8 * K, 200 * K)
    ada_stg = [RS.alloc([128, 8, 512], F32) for _ in range(4)]
    lamt = RS.alloc([128, 256], F32)
    lamp = RS.alloc([128, 128], F32)
    lams = RS.alloc([128, 2], F32)
    ada_src = ada_w.rearrange("(kc p) n -> p kc n", p=128)
    ada_pend = []

    def ada_flush():
        while ada_pend:
            pc, rowbuf, rowkey = ada_pend.pop(0)

            def mt(e, pc=pc, rowbuf=rowbuf):
                ins = None
                for nn in range(4):
                    col = pc * 4 + nn
                    ins = e.matmul(psb[1][:, col:col + 1], lhsT=rowbuf[0:1, nn * 128:(nn + 1) * 128], rhs=ones_f[0:1, 0:1],
                                   start=True, stop=True)
                return ins
            P.op("pe", mt, r=[rowkey, "ones_f"], w=[("ps1ada", pc)])

    def ada_piece(pc, sg, sgkey, rowbuf, rowkey):
        P.op("sp", lambda e: e.dma_start(out=sg, in_=ada_src[:, :, pc * 512:(pc + 1) * 512]), w=[sgkey], dma=True)

        def mm(e):
            ins = None
            for kc in range(8):
                ins = e.matmul(psb[0][0:1, 0:512], lhsT=vec[:, CP + kc:CP + kc + 1], rhs=sg[:, kc, :], start=(kc == 0),
                               stop=(kc == 7))
            return ins
        P.op("pe", mm, r=[sgkey, "vec"], w=["ps0row"])
        P.op("act", lambda e: e.activation(out=rowbuf[0:1, :], in_=psb[0][0:1, 0:512], func=AF.Copy), r=["ps0row"], w=[rowkey])
        ada_flush()
        ada_pend.append((pc, rowbuf, rowkey))

    ada_row = [RS.alloc([1, 512], F32) for _ in range(2)]
    for pc in range(4):
        ada_piece(pc, ada_stg[pc], ("adastg", pc), ada_row[pc % 2], ("adarow", pc % 2))
    ada_flush()
    for pc in []:
        sg = ada_stg[pc % 2]

        def mm(e, sg=sg, pc=pc):
            ins = None
            for nn in range(4):
                col = pc * 4 + nn
                for kc in range(8):
                    ins = e.matmul(psb[0][:, col:col + 1], lhsT=sg[:, kc, nn * 128:(nn + 1) * 128],
                                   rhs=vec[:, CP + kc:CP + kc + 1], start=(kc == 0), stop=(kc == 7))
            return ins
        P.op("pe", mm, r=[("adastg", pc % 2), "vec"], w=[PSK(0)])
    P.op("dve", lambda e: e.tensor_tensor(out=adaT[:, 0:16], in0=adaT[:, 0:16], in1=psb[1][:, 0:16], op=ALU.add),
         r=[("ps1ada", pc) for pc in range(4)] + ["adaT"], w=["adaT"])
    P.op("dve", lambda e: e.scalar_tensor_tensor(out=der[:, GSC1:GSC1 + 8], in0=adaT[:, SC1:SC1 + 8], scalar=1.0,
                                                  in1=vec[:, N1G:N1G + 8], op0=ALU.add, op1=ALU.mult),
         r=["adaT", "vec"], w=["der"])
    P.op("dve", lambda e: e.tensor_scalar(out=der[:, SUBS:SUBS + 1], in0=vec[:, SUBG:SUBG + 1],
                                          scalar1=(1.0 - LAM_INIT), scalar2=None, op0=ALU.mult),
         r=["vec", "der"], w=["der"])
    P.op("sp", lambda e: e.dma_start(out=lamt, in_=lam4[0:1, :].partition_broadcast(128)), w=["lamt"], dma=True)
    P.op("dve", lambda e: e.tensor_tensor(out=lamp.rearrange("p (a d) -> p a d", a=2),
                                          in0=lamt.rearrange("p (a b d) -> p a b d", a=2, b=2)[:, :, 0, :],
                                          in1=lamt.rearrange("p (a b d) -> p a b d", a=2, b=2)[:, :, 1, :],
                                          op=ALU.mult), r=["lamt"], w=["lamp"])
    P.op("dve", lambda e: e.tensor_reduce(out=lams, in_=lamp.rearrange("p (a d) -> p a d", a=2), axis=AX.X,
                                          op=ALU.add), r=["lamp"], w=["lams"])
    P.op("act", lambda e: e.activation(out=lams, in_=lams, func=AF.Exp), r=["lams"], w=["lams"])
    P.op("dve", lambda e: e.scalar_tensor_tensor(out=der[:, LAMC:LAMC + 1], in0=lams[:, 0:1], scalar=LAM_INIT,
                                                  in1=lams[:, 1:2], op0=ALU.add, op1=ALU.subtract),
         r=["lams", "der"], w=["der"])
    P.op("dve", lambda e: e.tensor_scalar(out=der[:, NLAM:NLAM + 1], in0=der[:, LAMC:LAMC + 1], scalar1=-1.0,
                                          scalar2=None, op0=ALU.mult), r=["der"], w=["der"])
    antiid = RC.alloc([128, 128], F32)
    dv32 = RS.alloc([32, 768], F32)
    Sm = RS.alloc([32, 768], F32)
    w8 = RS.alloc([8, 768], F32)
    c8 = RS.alloc([8, 768], F32)
    rb0 = RS.alloc([32, 8], F32)
    rb1 = RS.alloc([32, 8], F32)
    lo_t = RS.alloc([32, 1], F32)
    nb31 = RS.alloc([8, 1], F32)

    P.op("sp", lambda e: e.dma_start(out=antiid, in_=antiid_d[:, :]), w=["antiid"], dma=True)
    P.op("sp", lambda e: e.dma_start(out=dv32, in_=dvec_d[0:1, :].partition_broadcast(32)), w=["dv32"], dma=True)
    P.op("sp", lambda e: e.dma_start(out=lo_t, in_=lo_d[:, :]), w=["lo_t"], dma=True)
    P.op("sp", lambda e: e.dma_start(out=rb0, in_=relb[:, :]), w=["rb0"], dma=True)
    P.op("pool", lambda e: e.memset(rb1, 0.0), w=["rb1"])
    P.op("sp", lambda e: e.dma_start(out=rb1[1:32, :], in_=relb[0:31, :]), w=["rb1"], dma=True)
    P.op("sp", lambda e: e.dma_start(out=nb31, in_=relb31[:, :]), w=["nb31"], dma=True)
    P.op("dve", lambda e: e.tensor_scalar(out=nb31, in0=nb31, scalar1=-1.0, scalar2=None, op0=ALU.mult),
         r=["nb31"], w=["nb31"])
    P.op("dve", lambda e: e.tensor_tensor(out=rb0, in0=rb0, in1=rb1, op=ALU.subtract), r=["rb0", "rb1"], w=["rb0"])
    P.op("dve", lambda e: e.tensor_scalar(out=Sm, in0=dv32, scalar1=lo_t[:, 0:1], scalar2=None, op0=ALU.is_ge),
         r=["dv32", "lo_t"], w=["Sm"])
    P.op("dve", lambda e: e.tensor_scalar(out=c8, in0=dv32[0:8, :], scalar1=0.0, scalar2=None, op0=ALU.is_ge),
         r=["dv32"], w=["c8"])

    def mmw(e):
        e.matmul(psb[2][0:8, 0:512], lhsT=rb0[:, :], rhs=Sm[:, 0:512], start=True, stop=True)
        return e.matmul(psb[3][0:8, 0:256], lhsT=rb0[:, :], rhs=Sm[:, 512:768], start=True, stop=True)
    P.op("pe", mmw, r=["rb0", "Sm"], w=[PSK(2), PSK(3)])
    P.op("act", lambda e: e.activation(out=w8[:, 0:512], in_=psb[2][0:8, 0:512], func=AF.Exp, bias=nb31[:, 0:1], scale=1.0),
         r=[PSK(2), "nb31"], w=["w8a"])
    P.op("act", lambda e: e.activation(out=w8[:, 512:768], in_=psb[3][0:8, 0:256], func=AF.Exp, bias=nb31[:, 0:1], scale=1.0),
         r=[PSK(3), "nb31"], w=["w8b"])
    P.op("dve", lambda e: e.tensor_tensor(out=w8, in0=w8, in1=c8, op=ALU.mult), r=["w8a", "w8b", "c8"], w=["w8"])
    P.op("sp", lambda e: e.dma_start(out=wscr[:, :], in_=w8), r=["w8"], w=["wscr"], dma=True)
    dump("adaT", adaT)
    dump("der", der)
    P.barrier()

    KT = carve(8 * K, [128, 4, S], BF16)
    VA = carve(72 * K, [128, 64, 4, 129], BF16)
    QOFF = 72 * K + 66048 + 512
    qT = carve(QOFF, [128, 4, 2048], BF16)
    RA0 = QOFF + 16 * K

    w_in_src = w_in.rearrange("(fc p) n -> p fc n", p=128)

    def norm_stage(tag, xs, n, sq, rs, hn, sq_eng="pool"):
        if sq_eng == "pool":
            P.op("pool", lambda e: e.tensor_tensor(out=sq, in0=xs, in1=xs, op=ALU.mult),
                 r=[(tag, "xs")], w=[(tag, "sq")])
        else:
            P.op("act", lambda e: e.activation(out=sq, in_=xs, func=AF.Square), r=[(tag, "xs")], w=[(tag, "sq")])

    RW = Region(RA0, 200 * K)
    Wkv = RW.alloc([128, 8, 1024], BF16)
    WKV_END = RW.cur
    Wq = RW.alloc([128, 8, 512], BF16)
    shrep = carve(8 * K, [128, 8, 128], F32)
    wstg = [carve(16 * K + i * 16 * K, [128, 8, 512], F32) for i in range(2)]

    def make_rep(dst, col0, key):
        for fc in range(8):
            P.op("dve", lambda e, fc=fc: e.tensor_scalar(out=dst[:, fc, :], in0=ones_f,
                                                          scalar1=adaT[:, col0 + fc:col0 + fc + 1], scalar2=None,
                                                          op0=ALU.mult), r=["ones_f", "adaT"], w=[(key, fc)])

    make_rep(shrep, SH1, "shrep")
    brow_s = [carve(48 * K + k_ * 2 * K, [1, 512], F32) for k_ in range(2)]

    def cast_piece(stg, stgkey, dst, dcol, gcol, engs=("dve", "act"), width=512):
        for fc in range(8):
            eng = engs[fc % len(engs)]
            if eng == "act":
                P.op("act", lambda e, fc=fc: e.activation(out=dst[:, fc, dcol:dcol + width], in_=stg[:, fc, 0:width],
                                                          func=AF.Identity, scale=gcol[:, fc:fc + 1]),
                     r=[stgkey, "der"], w=[("wdst", id(dst), fc, dcol)])
            else:
                P.op(eng, lambda e, fc=fc: e.tensor_scalar(out=dst[:, fc, dcol:dcol + width], in0=stg[:, fc, 0:width],
                                                           scalar1=gcol[:, fc:fc + 1], scalar2=None, op0=ALU.mult),
                     r=[stgkey, "der"], w=[("wdst", id(dst), fc, dcol)])

    def bias_pp(stg, stgkey, shcol, bias_dst, bcol0, psi, bkey, brow, psi2=None, width=512):
        psi2 = psi if psi2 is None else psi2
        nch = width // 128

        def mm(e):
            ins = None
            for fc in range(8):
                ins = e.matmul(psb[psi][0:1, 0:width], lhsT=adaT[:, shcol + fc:shcol + fc + 1], rhs=stg[:, fc, 0:width],
                               start=(fc == 0), stop=(fc == 7))
            return ins
        P.op("pe", mm, r=[stgkey, "adaT"], w=[PSK(psi)])
        P.op("act", lambda e: e.activation(out=brow[0:1, 0:width], in_=psb[psi][0:1, 0:width], func=AF.Copy), r=[PSK(psi)],
             w=[("brow", id(brow))])

        def mt(e):
            ins = None
            for mc in range(nch):
                ins = e.matmul(psb[psi2][:, mc:mc + 1], lhsT=brow[0:1, mc * 128:(mc + 1) * 128], rhs=ones_f[0:1, 0:1],
                               start=True, stop=True)
            return ins
        P.op("pe", mt, r=[("brow", id(brow)), "ones_f"], w=[PSK(psi2)])
        P.op("act", lambda e: e.activation(out=bias_dst[:, bcol0:bcol0 + nch], in_=psb[psi2][:, 0:nch], func=AF.Copy),
             r=[PSK(psi2)], w=[bkey])

    def bias_bc(stg, stgkey, rep, repkey, ncols, dst, psi, bkey):
        def mm(e):
            ins = None
            for fc in range(8):
                ins = e.matmul(psb[psi][:, 0:ncols], lhsT=rep[:, fc, :], rhs=stg[:, fc, 0:ncols], start=(fc == 0),
                               stop=(fc == 7))
            return ins
        P.op("pe", mm, r=[stgkey] + [(repkey, fc) for fc in range(8)], w=[PSK(psi)])
        P.op("act", lambda e: e.activation(out=dst, in_=psb[psi][:, 0:ncols], func=AF.Copy), r=[PSK(psi)], w=[bkey])

    for pi, (c0, dst, dcol) in enumerate([(0, Wq, 0), (512, Wkv, 0), (1024, Wkv, 512)]):
        sg = wstg[pi % 2]
        sk = ("wstg", pi % 2)
        P.op("sp", lambda e, sg=sg, c0=c0: e.dma_start(out=sg, in_=w_in_src[:, :, c0:c0 + 512]), w=[sk], dma=True)
        cast_piece(sg, sk, dst, dcol, der[:, GSC1:GSC1 + 8])
        if pi == 0:
            bias_pp(sg, sk, SH1, bias_q, 0, 2, "bias_q", brow_s[0], 4)
        elif pi == 1:
            bias_pp(sg, sk, SH1, bias_k, 0, 3, "bias_k", brow_s[1], 5)
        else:
            bias_bc(sg, sk, shrep, "shrep", 512, bias_vb, 6, "bias_vb")
    dump("bias_q", bias_q)
    dump("bias_vb", bias_vb)
    dump("Wq", Wq, BF16)

    RAO = Region(RW.cur, 200 * K)
    xos = [RAO.alloc([128, 8, 130], F32) for _ in range(2)]
    sqo = [RAO.alloc([128, 8, 130], BF16) for _ in range(2)]
    rso = [RAO.alloc([128, 130], F32) for _ in range(2)]
    hno = [RAO.alloc([128, 8, 130], BF16) for _ in range(2)]
    xo_src = xo.rearrange("(fc p) i t -> p fc i t", p=128)

    def own_norm(i, xs_t, sq_t, rs_t, hn_t, psi, hnkey=None):
        b = i % 2
        hnkey = ("hno", b) if hnkey is None else hnkey
        P.op("sp", lambda e: e.dma_start(out=xs_t, in_=xo_src[:, :, i, :]), w=[("xos", b)], dma=True)
        P.op("pool", lambda e: e.tensor_tensor(out=sq_t, in0=xs_t, in1=xs_t, op=ALU.mult),
             r=[("xos", b)], w=[("sqo", b)])

        def mm(e):
            ins = None
            for fc in range(8):
                ins = e.matmul(psb[psi][:, 0:130], lhsT=ones_bf, rhs=sq_t[:, fc, :], start=(fc == 0), stop=(fc == 7))
            return ins
        P.op("pe", mm, r=[("sqo", b), "ones_bf"], w=[PSK(psi)])
        P.op("act", lambda e: e.activation(out=rs_t, in_=psb[psi][:, 0:130], func=AF.Sqrt, bias=NORM_EPS, scale=1.0),
             r=[PSK(psi)], w=[("rso", b)])
        P.op("dve", lambda e: e.reciprocal(out=rs_t, in_=rs_t), r=[("rso", b)], w=[("rso", b)])
        P.op("dve", lambda e: e.tensor_tensor(out=hn_t, in0=xs_t, in1=rs_t.unsqueeze(1).to_broadcast([128, 8, 130]),
                                              op=ALU.mult), r=[("xos", b), ("rso", b)], w=[hnkey])

    ada_stg2 = [carve(72 * K + k_ * 16 * K, [128, 8, 512], F32) for k_ in range(2)]
    ada_row2 = [carve(72 * K + 32 * K + k_ * 2 * K, [1, 512], F32) for k_ in range(2)]

    def q_proj(i):
        b = i % 2
        pq = 6 + b

        def mmq(e):
            ins = None
            for mc in range(4):
                for fc in range(8):
                    ins = e.matmul(psb[pq][:, mc * 128:(mc + 1) * 128], lhsT=Wq[:, fc, mc * 128:(mc + 1) * 128],
                                   rhs=hno[b][:, fc, 2:130], start=(fc == 0), stop=(fc == 7))
            return ins
        P.op("pe", mmq, r=[("hno", b)] + [("wdst", id(Wq), fc, 0) for fc in range(8)], w=[PSK(pq)])
        for mc in range(4):
            P.op("act", lambda e, mc=mc: e.activation(
                out=qT[:, mc, i * 128:(i + 1) * 128], in_=psb[pq][:, mc * 128:(mc + 1) * 128], func=AF.Identity,
                bias=bias_q[:, mc:mc + 1], scale=1.0), r=[PSK(pq), "bias_q"], w=[("qT", i, mc)])

    Hs_l = [carve(72 * K + 40 * K + k_ * 4 * K, [128, 8, 128], F32) for k_ in range(2)]
    Mt_stage = carve(72 * K + 48 * K, [128, 5, 1024], BF16)

    def mtile(sp_):
        off = 512 - sp_ * 128
        src = bass.AP(wscr.tensor, off, [[1, 128], [768, 8], [1, 128]])
        Hs = Hs_l[sp_ % 2]
        hk = ("Hs", sp_ % 2)
        P.op("sp", lambda e: e.dma_start(out=Hs, in_=src), w=[hk], dma=True)

        def mmf(e):
            hs2 = Hs.rearrange("p m q -> p (m q)")
            e.matmul(psb[2][:, :], lhsT=antiid, rhs=hs2[:, 0:512], start=True, stop=True)
            return e.matmul(psb[3][:, :], lhsT=antiid, rhs=hs2[:, 512:1024], start=True, stop=True)
        P.op("pe", mmf, r=[hk, "antiid"], w=[PSK(2), PSK(3)])
        for hh in range(2):
            P.op("dve", lambda e, hh=hh: e.tensor_copy(
                out=Mt_stage[:, sp_, :].rearrange("p (c h q) -> p c h q", c=2, h=4)[:, :, 2 * hh:2 * hh + 2, :],
                in_=psb[2 + hh][:, :].rearrange("p (h c q) -> p c h q", h=2, c=2)),
                r=[PSK(2 + hh)], w=[("Mts", sp_, hh)])

    own_norm(0, xos[0], sqo[0], rso[0], hno[0], 4)
    for i in range(NOWN):
        if i + 1 < NOWN:
            b1 = (i + 1) % 2
            own_norm(i + 1, xos[b1], sqo[b1], rso[b1], hno[b1], 4 + b1)
        q_proj(i)
        if i % 3 == 1 and i // 3 < 5:
            mtile(i // 3)
        if i == 14:
            P.op("sp", lambda e: e.dma_start(out=mscr.rearrange("p (s x) -> p s x", s=5), in_=Mt_stage),
                 r=[("Mts", sp_, hh) for sp_ in range(5) for hh in range(2)], w=["mscr"], dma=True)
        if i % 2 == 0:
            pc = 4 + i // 2
            ada_piece(pc, ada_stg2[pc % 2], ("adastg2", pc % 2), ada_row2[pc % 2], ("adarow2", pc % 2))
    ada_flush()
    P.op("dve", lambda e: e.tensor_tensor(out=adaT[:, 16:48], in0=adaT[:, 16:48], in1=psb[1][:, 16:48], op=ALU.add),
         r=[("ps1ada", pc) for pc in range(4, 12)] + ["adaT"], w=["adaT"])
    P.op("dve", lambda e: e.scalar_tensor_tensor(out=der[:, GSC2:GSC2 + 8], in0=adaT[:, SC2:SC2 + 8], scalar=1.0,
                                                  in1=vec[:, N2G:N2G + 8], op0=ALU.add, op1=ALU.mult),
         r=["adaT", "vec", "der"], w=["der"])
    dump("qT", qT, BF16)
    P.barrier()

    RAK = Region(WKV_END, 200 * K)
    NCH = 32
    xs = [RAK.alloc([128, 8, 256], F32) for _ in range(2)]
    sqk1 = RAK.alloc([128, 8, 256], BF16)
    sqk = [sqk1, sqk1]
    rsk = [RAK.alloc([128, 256], F32) for _ in range(2)]
    hnk = [RAK.alloc([128, 8, 256], BF16) for _ in range(2)]
    xT_src = xT.rearrange("(fc p) t -> p fc t", p=128)
    P.op("pool", lambda e: e.memset(VA[:, :, :, 128:129], 1.0), w=["VAones"])

    def kv_load(c):
        b = c % 2
        P.op("sp", lambda e: e.dma_start(out=xs[b], in_=xT_src[:, :, c * 256:(c + 1) * 256]), w=[("xs", b)], dma=True)

    def kv_norm(c):
        b = c % 2
        P.op("act", lambda e: e.activation(out=sqk[b], in_=xs[b], func=AF.Square), r=[("xs", b)], w=["sqk"])

        def mm(e):
            ins = None
            for fc in range(8):
                ins = e.matmul(psb[b][:, 0:256], lhsT=ones_bf, rhs=sqk[b][:, fc, :], start=(fc == 0), stop=(fc == 7))
            return ins
        P.op("pe", mm, r=["sqk", "ones_bf"], w=[PSK(b)])
        P.op("act", lambda e: e.activation(out=rsk[b], in_=psb[b][:, 0:256], func=AF.Sqrt, bias=NORM_EPS, scale=1.0),
             r=[PSK(b)], w=[("rsk", b)])
        P.op("dve", lambda e: e.reciprocal(out=rsk[b], in_=rsk[b]), r=[("rsk", b)], w=[("rsk", b)])
        P.op("dve", lambda e: e.tensor_tensor(out=hnk[b], in0=xs[b], in1=rsk[b].unsqueeze(1).to_broadcast([128, 8, 256]),
                                              op=ALU.mult), r=[("xs", b), ("rsk", b)], w=[("hnk", b)])

    def kv_proj(c):
        b = c % 2
        for half in range(2):
            psi = 2 + half

            def mmk(e, half=half, psi=psi):
                ins = None
                for m2 in range(2):
                    mc = half * 2 + m2
                    for fc in range(8):
                        ins = e.matmul(psb[psi][:, m2 * 256:(m2 + 1) * 256], lhsT=Wkv[:, fc, mc * 128:(mc + 1) * 128],
                                       rhs=hnk[b][:, fc, :], start=(fc == 0), stop=(fc == 7))
                return ins
            P.op("pe", mmk, r=[("hnk", b)], w=[PSK(psi)])
            for m2 in range(2):
                mc = half * 2 + m2
                P.op("act", lambda e, mc=mc, m2=m2, psi=psi: e.activation(
                    out=KT[:, mc, c * 256:(c + 1) * 256], in_=psb[psi][:, m2 * 256:(m2 + 1) * 256], func=AF.Identity,
                    bias=bias_k[:, mc:mc + 1], scale=1.0), r=[PSK(psi), "bias_k"], w=[("KT", c, mc)])
        for tt in range(2):
            psi = 4 + tt

            def mmv(e, tt=tt, psi=psi):
                ins = None
                for fc in range(8):
                    ins = e.matmul(psb[psi][:, :], lhsT=hnk[b][:, fc, tt * 128:(tt + 1) * 128], rhs=Wkv[:, fc, 512:1024],
                                   start=(fc == 0), stop=(fc == 7))
                return ins
            P.op("pe", mmv, r=[("hnk", b)], w=[PSK(psi)])
            P.op("dve", lambda e, tt=tt, psi=psi: e.tensor_tensor(
                out=VA[:, 2 * c + tt, :, 0:128], in0=psb[psi][:, :].rearrange("p (h d) -> p h d", h=4),
                in1=bias_vb.rearrange("p (h d) -> p h d", h=4), op=ALU.add),
                r=[PSK(psi), "bias_vb"], w=[("VA", c, tt)])

    kv_load(0)
    kv_load(1)
    kv_norm(0)
    for c in range(NCH):
        if c + 2 < NCH:
            pass
        if c + 1 < NCH:
            kv_norm(c + 1)
        kv_proj(c)
        if c + 2 < NCH:
            kv_load(c + 2)
    dump("KT", KT, BF16)
    dump("VA", VA, BF16)
    P.barrier()

    RB = Region(RA0, 200 * K)
    attnT = RB.alloc([128, 4, 2048], BF16)
    Mt = RB.alloc([128, 5, 1024], BF16)
    UB0 = RB.cur
    Pt = [RB.alloc([128, 1024], BF16) for _ in range(3)]
    osb = RB.alloc([128, 8, 129], F32)
    att_l = [RB.alloc([128, 4, 128], F32) for _ in range(2)]
    att2_l = [RB.alloc([128, 4, 128], F32) for _ in range(2)]
    rz = RB.alloc([128, 8], F32)
    ssq_l = [RB.alloc([128, 4], F32) for _ in range(2)]
    RB = Region(UB0, 200 * K)
    P.op("sp", lambda e: e.dma_start(out=Mt, in_=mscr.rearrange("p (s x) -> p s x", s=5)), w=["Mt"], dma=True)
    dump("Mt", Mt, BF16)
    dump("w8", w8)
    OB = [5, 6, 7]
    TB = 4

    def omap(m):
        return psb[OB[m // 3]][:, (m % 3) * 129:(m % 3) * 129 + 129]

    pairs = []
    for i in range(NOWN):
        nk = 4 * i + 4
        for kb in range(nk):
            pairs.append((i, kb, kb == 0, kb == nk - 1))
    NP = len(pairs)
    osf = osb.rearrange("p m d -> p (m d)")
    OSK = ["osb0", "osb1", "osb2"]

    def stage_qk(n):
        i, kb, first, last = pairs[n]
        sset = n % 2
        pb = n % 3
        sA, sB = psb[2 * sset], psb[2 * sset + 1]
        gen = kb - (4 * i - 1)

        def mmqk(e):
            ins = None
            for h in range(4):
                e.matmul(sA[:, h * 128:(h + 1) * 128], lhsT=KT[0:64, h, kb * 128:(kb + 1) * 128],
                         rhs=qT[0:64, h, i * 128:(i + 1) * 128], start=True, stop=True)
                ins = e.matmul(sB[:, h * 128:(h + 1) * 128], lhsT=KT[64:128, h, kb * 128:(kb + 1) * 128],
                               rhs=qT[64:128, h, i * 128:(i + 1) * 128], start=True, stop=True)
            return ins
        P.op("pe", mmqk, r=[], w=[PSK(2 * sset), PSK(2 * sset + 1)])
        P.op("act", lambda e: e.activation(out=Pt[pb], in_=psd[sset][:, :], func=AF.Exp, scale=0.125),
             r=[PSK(2 * sset), PSK(2 * sset + 1)], w=[("Pt", pb, 0), ("Pt", pb, 1)])
        if gen >= 0:
            P.op("dve", lambda e: e.tensor_tensor(out=Pt[pb], in0=Pt[pb], in1=Mt[:, gen, :], op=ALU.mult),
                 r=[("Pt", pb, 0), ("Pt", pb, 1), "Mt"], w=[("Pt", pb, 0), ("Pt", pb, 1)])

    def stage_pv(n):
        i, kb, first, last = pairs[n]
        pb = n % 3
        par = i % 2
        att, att2, ssq = att_l[par], att2_l[par], ssq_l[par]
        ATK = [("att", par, h) for h in range(4)]

        def mmpv(e):
            ins = None
            for h in range(4):
                for c in range(2):
                    m = 2 * h + c
                    ins = e.matmul(omap(m), lhsT=Pt[pb][:, c * 512 + h * 128:c * 512 + (h + 1) * 128],
                                   rhs=VA[:, kb, h, :], start=(first and m % 3 == 0), stop=last,
                                   skip_group_check=True)
            return ins
        P.op("pe", mmpv, r=[("Pt", pb, 0), ("Pt", pb, 1)], w=[PSK(5), PSK(6), PSK(7)])
        if last:
            P.op("dve", lambda e: e.tensor_copy(out=osf[:, 0:387], in_=psb[5][:, 0:387]), r=[PSK(5)], w=["osb0"])
            P.op("dve", lambda e: e.tensor_copy(out=osf[:, 387:774], in_=psb[6][:, 0:387]), r=[PSK(6)], w=["osb1"])
            P.op("dve", lambda e: e.tensor_copy(out=osf[:, 774:1032], in_=psb[7][:, 0:258]), r=[PSK(7)], w=["osb2"])
            P.op("dve", lambda e: e.reciprocal(out=rz, in_=osb[:, :, 128]), r=OSK, w=["rz"])
            P.op("dve", lambda e: e.tensor_scalar(out=rz.rearrange("p (h c) -> p h c", c=2)[:, :, 1],
                                                  in0=rz.rearrange("p (h c) -> p h c", c=2)[:, :, 1],
                                                  scalar1=der[:, NLAM:NLAM + 1], scalar2=None, op0=ALU.mult),
                 r=["rz", "der"], w=["rz"])
            for h in range(4):
                P.op("dve", lambda e, h=h: e.tensor_scalar(out=att[:, h, :], in0=osb[:, 2 * h, 0:128],
                                                           scalar1=rz[:, 2 * h:2 * h + 1], scalar2=None, op0=ALU.mult),
                     r=OSK + ["rz"], w=[("att", par, h)])
                P.op("dve", lambda e, h=h: e.scalar_tensor_tensor(out=att[:, h, :], in0=osb[:, 2 * h + 1, 0:128],
                                                                  scalar=rz[:, 2 * h + 1:2 * h + 2], in1=att[:, h, :],
                                                                  op0=ALU.mult, op1=ALU.add),
                     r=OSK + ["rz", ("att", par, h)], w=[("att", par, h)])
            P.op("dve", lambda e: e.tensor_tensor(out=att2, in0=att, in1=att, op=ALU.mult), r=ATK, w=[("att2", par)])
            P.op("dve", lambda e: e.tensor_reduce(out=ssq, in_=att2, axis=AX.X, op=ALU.add), r=[("att2", par)], w=[("ssq", par)])

    def post_a(i):
        par = i % 2
        att, att2, ssq = att_l[par], att2_l[par], ssq_l[par]
        ATK = [("att", par, h) for h in range(4)]
        P.op("act", lambda e: e.activation(out=ssq, in_=ssq, func=AF.Ln, bias=SUBLN_EPS, scale=1.0 / 128.0),
             r=[("ssq", par)], w=[("ssq", par)])
        P.op("act", lambda e: e.activation(out=ssq, in_=ssq, func=AF.Exp, scale=-0.5),
             r=[("ssq", par)], w=[("ssq", par)])
        P.op("dve", lambda e: e.tensor_tensor(out=att2, in0=att, in1=ssq.unsqueeze(2).to_broadcast([128, 4, 128]),
                                              op=ALU.mult), r=ATK + [("ssq", par), ("att2", par)], w=[("att2", par)])

    def post_b(i):
        par = i % 2
        att2 = att2_l[par]
        def mmt(e):
            ins = None
            for h in range(4):
                ins = e.transpose(psb[TB][:, h * 128:(h + 1) * 128], att2[:, h, :], ident)
            return ins
        P.op("pe", mmt, r=[("att2", par), "ident"], w=[PSK(TB)])
        P.op("dve", lambda e: e.tensor_copy(out=attnT[:, :, i * 128:(i + 1) * 128],
                                            in_=psb[TB][:, :].rearrange("p (h q) -> p h q", h=4)),
             r=[PSK(TB)], w=[("attnT", i)])

    sched = {}
    for n in range(NP):
        i, kb, first, last = pairs[n]
        if last:
            sched.setdefault(n + 2 + 8, []).append(("a", i))
            sched.setdefault(n + 2 + 12, []).append(("b", i))
    PVLAG = 2
    for n in range(NP + PVLAG):
        if n < NP:
            stage_qk(n)
        if n >= PVLAG:
            stage_pv(n - PVLAG)
        for kind, i in sched.pop(n, []):
            (post_a if kind == "a" else post_b)(i)
    for n in sorted(sched):
        for kind, i in sched[n]:
            (post_a if kind == "a" else post_b)(i)
    P.barrier()

    dump("attnT", attnT, BF16)
    P.barrier()

    RC1 = Region(8 * K, RA0)
    x2T = RC1.alloc([128, 8, 2048], F32)
    convT = RC1.alloc([128, 4, 2048], BF16)
    Wc = RC1.alloc([128, 8, 1536], BF16)
    Wo = RC1.alloc([128, 8, 1024], BF16)
    cstg = [RC1.alloc([128, 8, 512], F32) for _ in range(1)]
    RC2 = Region(RA0 + 16 * K, ARENA)
    xos2 = [RC2.alloc([128, 8, 130], F32) for _ in range(2)]
    sqo2 = [RC2.alloc([128, 8, 130], BF16) for _ in range(2)]
    rso2 = [RC2.alloc([128, 130], F32) for _ in range(2)]
    hnp = [RC2.alloc([128, 8, 2, 130], BF16) for _ in range(2)]
    gcs = [RC2.alloc([128, 2, 130], F32) for _ in range(2)]
    ut = [RC2.alloc([128, 2, 130], F32) for _ in range(2)]
    tt_ = [RC2.alloc([128, 2, 128], F32) for _ in range(2)]
    hcs = [RC2.alloc([128, 2, 130], F32) for _ in range(2)]
    brow_c = RC2.alloc([1, 512], F32)
    w_out_src = w_out.rearrange("(fc p) n -> p fc n", p=128)

    def load_x2T(fcs):
        for fc in fcs:
            P.op("sp", lambda e, fc=fc: e.dma_start(out=x2T[:, fc, :].rearrange("p (i t) -> p i t", i=NOWN),
                                                    in_=xo_src[:, fc, :, 2:130]), w=[("x2T", fc)], dma=True)

    def load_wo(pi):
        sg = cstg[0]
        sk = ("cstg", 0)
        P.op("sp", lambda e: e.dma_start(out=sg, in_=w_out_src[:, :, pi * 512:(pi + 1) * 512]),
             w=[sk, ("cstgh", 0), ("cstgh", 1)], dma=True)
        for fc in range(8):
            dst = Wo[:, fc, pi * 512:(pi + 1) * 512]
            if fc < 4:
                if fc % 2 == 0:
                    P.op("dve", lambda e, fc=fc, dst=dst: e.tensor_scalar(out=dst, in0=sg[:, fc, :], scalar1=der[:, SUBS:SUBS + 1],
                                                                          scalar2=None, op0=ALU.mult),
                         r=[sk, "der"], w=[("Wo", fc, pi)])
                else:
                    P.op("act", lambda e, fc=fc, dst=dst: e.activation(out=dst, in_=sg[:, fc, :], func=AF.Identity,
                                                                       scale=der[:, SUBS:SUBS + 1]),
                         r=[sk, "der"], w=[("Wo", fc, pi)])
            else:
                if fc % 2 == 0:
                    P.op("dve", lambda e, fc=fc, dst=dst: e.tensor_copy(out=dst, in_=sg[:, fc, :]), r=[sk], w=[("Wo", fc, pi)])
                else:
                    P.op("act", lambda e, fc=fc, dst=dst: e.activation(out=dst, in_=sg[:, fc, :], func=AF.Copy),
                         r=[sk], w=[("Wo", fc, pi)])

    def conv_norm(i):
        b = i % 2
        pp = (i // 2) % 2
        own_norm(i, xos2[b], sqo2[b], rso2[b], hnp[pp][:, :, i % 2, :], b, hnkey=("hnp", pp, i % 2))

    ada_bank_dummy = None
    conv_norm(0)
    conv_norm(1)
    for hn_, hp in enumerate((0, 2, 4, 1, 3, 5)):
        sgv = cstg[0][:, :, (hn_ % 2) * 256:(hn_ % 2) * 256 + 256]
        sk = ("cstgh", hn_ % 2)
        P.op("sp", lambda e, sgv=sgv, hp=hp: e.dma_start(out=sgv, in_=w_in_src[:, :, 1536 + hp * 256:1536 + (hp + 1) * 256]),
             w=[sk], dma=True)
        cast_piece(sgv, sk, Wc, hp * 256, der[:, GSC1:GSC1 + 8], width=256)
        bias_pp(sgv, sk, SH1, bias_c, hp * 2, 6, ("bias_c", hp), brow_c, 7, width=256)
    BCK = [("bias_c", hp) for hp in range(6)]
    WCK = [("wdst", id(Wc), fc, hp * 256) for fc in range(8) for hp in range(6)]
    for p_ in range(NOWN // 2):
        pp = p_ % 2
        if p_ + 1 < NOWN // 2:
            conv_norm(2 * p_ + 2)
            conv_norm(2 * p_ + 3)
        if 1 <= p_ <= 4:
            load_x2T([2 * (p_ - 1), 2 * (p_ - 1) + 1])
        if p_ == 1:
            load_wo(0)
        if p_ == 3:
            load_wo(1)
        for cc in range(4):
            g = cc % 2
            bks = (2, 3, 4) if g == 0 else (5, 6, 7)

            def mmc(e, pp=pp, cc=cc, bks=bks):
                ins = None
                for br in range(3):
                    for fc in range(8):
                        ins = e.matmul(psb[bks[br]][:, 0:260], lhsT=Wc[:, fc, br * 512 + cc * 128:br * 512 + (cc + 1) * 128],
                                       rhs=hnp[pp][:, fc, :, :], start=(fc == 0), stop=(fc == 7))
                return ins
            P.op("pe", mmc, r=[("hnp", pp, 0), ("hnp", pp, 1)] + WCK, w=[PSK(bk) for bk in bks])

            def v3(ap_):
                return ap_.rearrange("p (j t) -> p j t", j=2)
            P.op("act", lambda e, g=g, bks=bks, cc=cc: e.activation(out=gcs[g], in_=v3(psb[bks[1]][:, 0:260]), func=AF.Identity,
                                                                    bias=bias_c[:, 4 + cc:5 + cc], scale=1.0),
                 r=[PSK(bks[1])] + BCK, w=[("gcs", g)])
            P.op("act", lambda e, g=g, bks=bks, cc=cc: e.activation(out=hcs[g], in_=v3(psb[bks[2]][:, 0:260]), func=AF.Identity,
                                                                    bias=bias_c[:, 8 + cc:9 + cc], scale=1.0),
                 r=[PSK(bks[2])] + BCK, w=[("hcs", g)])
            P.op("pool", lambda e, g=g: e.tensor_tensor(out=ut[g], in0=gcs[g], in1=hcs[g], op=ALU.mult),
                 r=[("gcs", g), ("hcs", g)], w=[("ut", g)])
            if p_ == 0:
                P.op("dve", lambda e, g=g: e.tensor_scalar(out=ut[g][:, 0, 0:2], in0=ut[g][:, 0, 0:2], scalar1=vec[:, HM:HM + 1],
                                                           scalar2=None, op0=ALU.mult), r=[("ut", g), "vec"], w=[("ut", g)])
            P.op("act", lambda e, g=g, cc=cc: e.activation(out=tt_[g], in_=ut[g][:, :, 2:130], func=AF.Identity,
                                                           scale=vec[:, CW + cc * 3 + 2:CW + cc * 3 + 3]),
                 r=[("ut", g), "vec"], w=[("tt", g)])
            P.op("dve", lambda e, g=g, cc=cc: e.scalar_tensor_tensor(out=tt_[g], in0=ut[g][:, :, 1:129],
                                                                      scalar=vec[:, CW + cc * 3 + 1:CW + cc * 3 + 2],
                                                                      in1=tt_[g], op0=ALU.mult, op1=ALU.add),
                 r=[("ut", g), "vec", ("tt", g)], w=[("tt", g)])
            P.op("dve", lambda e, g=g, cc=cc: e.scalar_tensor_tensor(out=tt_[g], in0=ut[g][:, :, 0:128],
                                                                      scalar=vec[:, CW + cc * 3:CW + cc * 3 + 1],
                                                                      in1=tt_[g], op0=ALU.mult, op1=ALU.add),
                 r=[("ut", g), "vec", ("tt", g)], w=[("tt", g)])
            for j2 in range(2):
                P.op("dve", lambda e, g=g, cc=cc, bks=bks, p_=p_, j2=j2: e.scalar_tensor_tensor(
                    out=convT[:, cc, (2 * p_ + j2) * 128:(2 * p_ + j2 + 1) * 128],
                    in0=psb[bks[0]][:, j2 * 130 + 2:j2 * 130 + 130], scalar=bias_c[:, cc:cc + 1], in1=tt_[g][:, j2, :],
                    op0=ALU.add, op1=ALU.mult), r=[PSK(bks[0]), ("tt", g)] + BCK, w=[("convT", 2 * p_ + j2, cc)])

    dump("convT", convT, BF16)
    for t in range(4):
        tsl = slice(t * 512, (t + 1) * 512)
        for dc in range(8):
            psi = 6 + (dc % 2)

            def mmo(e, dc=dc, psi=psi, tsl=tsl):
                ins = None
                for fc in range(8):
                    rhs = attnT[:, fc, tsl] if fc < 4 else convT[:, fc - 4, tsl]
                    ins = e.matmul(psb[psi][:, :], lhsT=Wo[:, fc, dc * 128:(dc + 1) * 128], rhs=rhs, start=(fc == 0),
                                   stop=(fc == 7))
                return ins
            P.op("pe", mmo, r=[("attnT", ii) for ii in range(4 * t, 4 * t + 4)] + [("convT", ii, cc) for ii in range(4 * t, 4 * t + 4) for cc in range(4)]
                 + [("Wo", fc, dc // 4) for fc in range(8)], w=[PSK(psi)])
            P.op("dve", lambda e, dc=dc, psi=psi, tsl=tsl: e.scalar_tensor_tensor(
                out=x2T[:, dc, tsl], in0=psb[psi][:, :], scalar=adaT[:, G1 + dc:G1 + dc + 1], in1=x2T[:, dc, tsl],
                op0=ALU.mult, op1=ALU.add), r=[PSK(psi), ("x2T", dc), "adaT"], w=[("x2T", dc)])
    P.barrier()
    dump("x2T", x2T)

    RD = Region(8 * K + 64 * K, ARENA)
    h2T = RD.alloc([128, 8, 2048], BF16)
    We = [[RD.alloc([128, 8, 512], BF16), RD.alloc([128, 8, 512], BF16), RD.alloc([128, 4, 1024], BF16)] for _ in range(2)]
    estg = [RD.alloc([128, 4, 512], F32) for _ in range(2)]
    gatT = RD.alloc([16, 2048], BF16)
    sel = RD.alloc([16, 16, 128], BF16)
    rwf = RD.alloc([128, 8, 20], F32)
    rwb = RD.alloc([128, 8, 20], BF16)
    UNION0 = RD.cur
    sq2 = RD.alloc([128, 8, 512], BF16)
    rs2_l = [RD.alloc([128, 512], F32) for _ in range(2)]
    htmp = [RD.alloc([128, 512], F32) for _ in range(3)]
    rl = RD.alloc([128, 16, 20], F32)
    gat = RD.alloc([128, 16, 16], F32)
    ml = RD.alloc([128, 16, 16], F32)
    ml2 = RD.alloc([128, 16, 16], F32)
    oh1 = RD.alloc([128, 16, 16], F32)
    oh2 = RD.alloc([128, 16, 16], F32)
    g4 = RD.alloc([128, 16, 4], F32)
    g4b = RD.alloc([128, 16, 4], F32)
    sc = RD.alloc([128, 8, 16], F32)
    RU = Region(UNION0, ARENA)
    hid = [[RU.alloc([128, 512], BF16) for _ in range(4)] for _ in range(2)]
    sl = [RU.alloc([128, 512], F32) for _ in range(2)]
    vv = [RU.alloc([128, 512], F32) for _ in range(2)]
    sq_f = RU.alloc([128, 8, 512], BF16)
    rs_f = RU.alloc([128, 512], F32)

    P.op("sp", lambda e: e.dma_start(out=rwf, in_=rw.rearrange("(fc p) n -> p fc n", p=128)), w=["rwf"], dma=True)
    P.op("sp", lambda e: e.dma_start(out=rbias_b, in_=rb[0:1, :].partition_broadcast(128)), w=["rbias_b"], dma=True)
    P.op("dve", lambda e: e.tensor_copy(out=rwb, in_=rwf), r=["rwf"], w=["rwb"])
    P.op("pool", lambda e: e.tensor_copy(out=sel, in_=ident[0:16, 0:16].unsqueeze(2).to_broadcast([16, 16, 128])),
         r=["ident"], w=["sel"])

    def stats_tile(t, par, psi, eps):
        tsl = slice(t * 512, (t + 1) * 512)
        rs2 = rs2_l[par]
        for fc in range(8):
            P.op("act", lambda e, fc=fc: e.activation(out=sq2[:, fc, :], in_=x2T[:, fc, tsl], func=AF.Square),
                 r=[("x2T", fc)], w=[("sq2", fc)])

        def mm(e):
            ins = None
            for fc in range(8):
                ins = e.matmul(psb[psi][:, :], lhsT=ones_bf, rhs=sq2[:, fc, :], start=(fc == 0), stop=(fc == 7))
            return ins
        P.op("pe", mm, r=[("sq2", fc) for fc in range(8)] + ["ones_bf"], w=[PSK(psi)])
        P.op("act", lambda e: e.activation(out=rs2, in_=psb[psi][:, :], func=AF.Sqrt, bias=eps, scale=1.0),
             r=[PSK(psi)], w=[("rs2", par)])
        P.op("dve", lambda e: e.reciprocal(out=rs2, in_=rs2), r=[("rs2", par)], w=[("rs2", par)])

    def route_section():
        stats_tile(0, 0, 1, NORM_EPS)
        for t in range(4):
            tsl = slice(t * 512, (t + 1) * 512)
            if t + 1 < 4:
                stats_tile(t + 1, (t + 1) % 2, (1, 4)[(t + 1) % 2], NORM_EPS)
            rs2 = rs2_l[t % 2]
            for fc in range(8):
                g = fc % 3
                eng = "pool" if fc % 2 == 0 else "dve"
                P.op(eng, lambda e, tsl=tsl, fc=fc, g=g, rs2=rs2: e.tensor_tensor(out=htmp[g], in0=x2T[:, fc, tsl], in1=rs2,
                                                                                 op=ALU.mult),
                     r=[("x2T", fc), ("rs2", t % 2)], w=[("htmp", g)])
                P.op("act", lambda e, tsl=tsl, fc=fc, g=g: e.activation(
                    out=h2T[:, fc, tsl], in_=htmp[g], func=AF.Identity, bias=adaT[:, SH2 + fc:SH2 + fc + 1],
                    scale=der[:, GSC2 + fc:GSC2 + fc + 1]),
                    r=[("htmp", g), "adaT", "der"], w=[("h2T", t, fc)])

            def mmr(e, t=t):
                ins = None
                for sub in range(4):
                    st = t * 4 + sub
                    for fc in range(8):
                        ins = e.matmul(psb[2][:, st * 32:st * 32 + 20], lhsT=h2T[:, fc, st * 128:(st + 1) * 128], rhs=rwb[:, fc, :],
                                       start=(fc == 0), stop=(fc == 7))
                return ins
            P.op("pe", mmr, r=[("h2T", t, fc) for fc in range(8)] + ["rwb"], w=[("psR", t)])
            if t == 0:
                chunk_cast(0, 0, eng="act")
                chunk_cast(0, 1, eng="act")
                chunk_dma(0, 2)
                chunk_dma(0, 3)
            if t == 2:
                chunk_cast(0, 2, eng="act")
                chunk_cast(0, 3, eng="act")
                chunk_dma(0, 4)
                chunk_dma(0, 5)
        chunk_cast(0, 4, eng="act")
        chunk_cast(0, 5, eng="act")
        PSR = [("psR", t) for t in range(4)]
        P.op("dve", lambda e: e.tensor_tensor(out=rl, in0=psb[2][:, :].rearrange("p (s c) -> p s c", c=32)[:, :, 0:20],
                                              in1=rbias_b.unsqueeze(1).to_broadcast([128, 16, 20]), op=ALU.add),
             r=PSR + ["rbias_b"], w=["rl"])
        gl = rl[:, :, 0:4]
        el = rl[:, :, 4:20]
        R = "rt"
        P.op("dve", lambda e: e.tensor_reduce(out=sc[:, 0, :], in_=gl, axis=AX.X, op=ALU.max), r=["rl"], w=[R])
        P.op("dve", lambda e: e.tensor_tensor(out=g4, in0=gl, in1=sc[:, 0, :].unsqueeze(2).to_broadcast([128, 16, 4]),
                                              op=ALU.subtract), r=["rl", R], w=[R])
        P.op("act", lambda e: e.activation(out=g4b, in_=g4, func=AF.Exp), r=[R], w=[R])
        P.op("dve", lambda e: e.tensor_reduce(out=sc[:, 1, :], in_=g4b, axis=AX.X, op=ALU.add), r=[R], w=[R])
        P.op("dve", lambda e: e.reciprocal(out=sc[:, 1, :], in_=sc[:, 1, :]), r=[R], w=[R])
        P.op("dve", lambda e: e.tensor_scalar(out=g4, in0=g4, scalar1=0.0, scalar2=None, op0=ALU.is_ge), r=[R], w=[R])
        P.op("dve", lambda e: e.tensor_scalar(out=g4, in0=g4, scalar1=-1.0, scalar2=BIG, op0=ALU.add, op1=ALU.mult),
             r=[R], w=[R])
        P.op("dve", lambda e: e.tensor_tensor(out=ml.rearrange("p s (g x) -> p s g x", g=4),
                                              in0=el.rearrange("p s (g x) -> p s g x", g=4),
                                              in1=g4.unsqueeze(3).to_broadcast([128, 16, 4, 4]), op=ALU.add),
             r=["rl", R], w=[R])
        P.op("dve", lambda e: e.tensor_reduce(out=sc[:, 2, :], in_=ml, axis=AX.X, op=ALU.max), r=[R], w=[R])
        P.op("dve", lambda e: e.tensor_tensor(out=oh1, in0=ml, in1=sc[:, 2, :].unsqueeze(2).to_broadcast([128, 16, 16]),
                                              op=ALU.is_ge), r=[R], w=[R])
        P.op("dve", lambda e: e.scalar_tensor_tensor(out=ml2, in0=oh1, scalar=-BIG, in1=ml, op0=ALU.mult, op1=ALU.add),
             r=[R], w=[R])
        P.op("dve", lambda e: e.tensor_reduce(out=sc[:, 3, :], in_=ml2, axis=AX.X, op=ALU.max), r=[R], w=[R])
        P.op("dve", lambda e: e.tensor_tensor(out=oh2, in0=ml2, in1=sc[:, 3, :].unsqueeze(2).to_broadcast([128, 16, 16]),
                                              op=ALU.is_ge), r=[R], w=[R])
        P.op("dve", lambda e: e.tensor_tensor(out=sc[:, 4, :], in0=sc[:, 3, :], in1=sc[:, 2, :], op=ALU.subtract),
             r=[R], w=[R])
        P.op("act", lambda e: e.activation(out=sc[:, 4, :], in_=sc[:, 4, :], func=AF.Exp), r=[R], w=[R])
        P.op("dve", lambda e: e.tensor_scalar(out=sc[:, 4, :], in0=sc[:, 4, :], scalar1=1.0, scalar2=None, op0=ALU.add),
             r=[R], w=[R])
        P.op("dve", lambda e: e.reciprocal(out=sc[:, 4, :], in_=sc[:, 4, :]), r=[R], w=[R])
        P.op("dve", lambda e: e.tensor_tensor(out=sc[:, 5, :], in0=sc[:, 4, :], in1=sc[:, 1, :], op=ALU.mult), r=[R], w=[R])
        P.op("dve", lambda e: e.tensor_tensor(out=sc[:, 6, :], in0=sc[:, 1, :], in1=sc[:, 5, :], op=ALU.subtract),
             r=[R], w=[R])
        P.op("dve", lambda e: e.tensor_tensor(out=gat, in0=oh1, in1=sc[:, 5, :].unsqueeze(2).to_broadcast([128, 16, 16]),
                                              op=ALU.mult), r=[R], w=[R])
        P.op("dve", lambda e: e.tensor_tensor(out=oh2, in0=oh2, in1=sc[:, 6, :].unsqueeze(2).to_broadcast([128, 16, 16]),
                                              op=ALU.mult), r=[R], w=[R])
        P.op("dve", lambda e: e.tensor_tensor(out=gat, in0=gat, in1=oh2, op=ALU.add), r=[R], w=[R])
        for t in range(4):
            def mmg(e, t=t):
                ins = None
                for sub in range(4):
                    st = t * 4 + sub
                    ins = e.transpose(psb[3][0:16, sub * 128:(sub + 1) * 128], gat[:, st, :], ident)
                return ins
            P.op("pe", mmg, r=[R, "ident"], w=[PSK(3)])
            P.op("act", lambda e, t=t: e.activation(out=gatT[:, t * 512:(t + 1) * 512], in_=psb[3][0:16, :], func=AF.Copy),
                 r=[PSK(3)], w=[("gatT", t)])
        dump("gat", gat)
        dump("h2T", h2T, BF16)

    wg_src = wg.rearrange("x (fc p) n -> p x fc n", p=128)
    wu_src = wu.rearrange("x (fc p) n -> p x fc n", p=128)
    wd_src = wd.rearrange("x (jc p) n -> p x jc n", p=128)
    stg_n = [0]

    def chunk_spec(ex, k):
        wb = We[ex % 2]
        if k < 4:
            nm, hf = k // 2, k % 2
            src = (wg_src, wu_src)[nm][:, ex, hf * 4:(hf + 1) * 4, :]
            dsts = [(wb[nm][:, hf * 4 + f4, :], ("We", ex % 2, nm, hf * 4 + f4)) for f4 in range(4)]
        else:
            hf = k - 4
            src = wd_src[:, ex, :, hf * 512:(hf + 1) * 512]
            dsts = [(wb[2][:, jc, hf * 512:(hf + 1) * 512], ("We", ex % 2, 2, jc, hf)) for jc in range(4)]
        return src, dsts

    def chunk_dma(ex, k):
        src, dsts = chunk_spec(ex, k)
        sg = estg[k % 2]
        P.op("sp", lambda e: e.dma_start(out=sg, in_=src), w=[("estg", k % 2)], dma=True)

    def chunk_cast(ex, k, eng="act"):
        src, dsts = chunk_spec(ex, k)
        sg = estg[k % 2]
        for f4, (dst, key) in enumerate(dsts):
            if eng == "act":
                P.op("act", lambda e, dst=dst, f4=f4: e.activation(out=dst, in_=sg[:, f4, :], func=AF.Copy),
                     r=[("estg", k % 2)], w=[key])
            else:
                P.op(eng, lambda e, dst=dst, f4=f4: e.tensor_copy(out=dst, in_=sg[:, f4, :]), r=[("estg", k % 2)], w=[key])

    def expert_load(ex):
        for k in range(6):
            chunk_dma(ex, k)
            chunk_cast(ex, k, eng=("act", "dve")[k % 2])

    PREF = {4: [("d", 0), ("d", 1)], 6: [("c", 0), ("d", 2)], 8: [("c", 1), ("d", 3)], 10: [("c", 2), ("d", 4)],
            12: [("c", 3), ("d", 5)], 14: [("c", 4)], 15: [("c", 5)]}

    def prefetch_step(ex, sub):
        for kind, k in PREF.get(sub, []):
            if kind == "d":
                chunk_dma(ex, k)
            else:
                chunk_cast(ex, k)

    def g_step(ex, t, jc):
        wb = We[ex % 2]
        WK = [("We", ex % 2, nm, fc) for nm in range(2) for fc in range(8)]
        tsl = slice(t * 512, (t + 1) * 512)
        hb = (ex * 4 + t) % 2
        H2K = [("h2T", t, fc) for fc in range(8)]
        if jc == 0:
            P.op("pe", lambda e: e.matmul(psb[1][:, :], lhsT=sel[:, ex, :], rhs=gatT[:, tsl], start=True, stop=True),
                 r=["sel", ("gatT", t)], w=[PSK(1)])
        pa = 2 + (jc % 2) * 2
        pu = pa + 1

        def mmgu(e):
            ins = None
            for fc in range(8):
                e.matmul(psb[pa][:, :], lhsT=wb[0][:, fc, jc * 128:(jc + 1) * 128], rhs=h2T[:, fc, tsl],
                         start=(fc == 0), stop=(fc == 7))
            for fc in range(8):
                ins = e.matmul(psb[pu][:, :], lhsT=wb[1][:, fc, jc * 128:(jc + 1) * 128], rhs=h2T[:, fc, tsl],
                               start=(fc == 0), stop=(fc == 7))
            return ins
        P.op("pe", mmgu, r=WK + H2K, w=[PSK(pa), PSK(pu)])
        g = jc % 2
        P.op("act", lambda e: e.activation(out=sl[g], in_=psb[pa][:, :], func=AF.Silu), r=[PSK(pa)], w=[("sl", g)])
        P.op("dve", lambda e: e.tensor_tensor(out=vv[g], in0=psb[pu][:, :], in1=sl[g], op=ALU.mult),
             r=[PSK(pu), ("sl", g)], w=[("vv", g)])
        P.op("dve", lambda e: e.tensor_tensor(out=hid[hb][jc], in0=vv[g], in1=psb[1][:, :], op=ALU.mult),
             r=[("vv", g), PSK(1)], w=[("hid", hb, jc)])

    def d_step(ex, t, dc):
        wb = We[ex % 2]
        WDK = [("We", ex % 2, 2, jc, hf) for jc in range(4) for hf in range(2)]
        tsl = slice(t * 512, (t + 1) * 512)
        hb = (ex * 4 + t) % 2
        py = 6 + (dc % 2)

        def mmd(e):
            ins = None
            for jc in range(4):
                ins = e.matmul(psb[py][:, :], lhsT=wb[2][:, jc, dc * 128:(dc + 1) * 128], rhs=hid[hb][jc],
                               start=(jc == 0), stop=(jc == 3))
            return ins
        P.op("pe", mmd, r=WDK + [("hid", hb, jc) for jc in range(4)], w=[PSK(py)])
        P.op("dve", lambda e: e.scalar_tensor_tensor(out=x2T[:, dc, tsl], in0=psb[py][:, :], scalar=adaT[:, G2 + dc:G2 + dc + 1],
                                                      in1=x2T[:, dc, tsl], op0=ALU.mult, op1=ALU.add),
             r=[PSK(py), ("x2T", dc), "adaT"], w=[("x2T", dc)])

    NE = 16
    chunk_dma(0, 0)
    chunk_dma(0, 1)
    route_section()
    P.barrier()
    units = [(ex, t) for ex in range(NE) for t in range(4)]
    yT_src = yT.rearrange("(dc p) t -> p dc t", p=128)

    def final_stats(t):
        tsl = slice(t * 512, (t + 1) * 512)
        for fc in range(8):
            P.op("act", lambda e, fc=fc: e.activation(out=sq_f[:, fc, :], in_=x2T[:, fc, tsl], func=AF.Square),
                 r=[("x2T", fc)], w=[("sqf", fc)])

        def mm(e):
            ins = None
            for fc in range(8):
                ins = e.matmul(psb[0][:, :], lhsT=ones_bf, rhs=sq_f[:, fc, :], start=(fc == 0), stop=(fc == 7))
            return ins
        P.op("pe", mm, r=[("sqf", fc) for fc in range(8)] + ["ones_bf"], w=[PSK(0)])
        P.op("act", lambda e: e.activation(out=rs_f, in_=psb[0][:, :], func=AF.Sqrt, bias=NORM_EPS, scale=1.0),
             r=[PSK(0)], w=["rs_f"])
        P.op("dve", lambda e: e.reciprocal(out=rs_f, in_=rs_f), r=["rs_f"], w=["rs_f"])

    def final_apply(t):
        tsl = slice(t * 512, (t + 1) * 512)
        for dc in range(8):
            P.op("dve", lambda e, dc=dc: e.scalar_tensor_tensor(
                out=x2T[:, dc, tsl], in0=x2T[:, dc, tsl], scalar=vec[:, FG + dc:FG + dc + 1], in1=rs_f,
                op0=ALU.mult, op1=ALU.mult), r=[("x2T", dc), "rs_f", "vec"], w=[("x2T", dc), ("x2Tout", t, dc)])
        P.op("sp", lambda e: e.dma_start(out=yT_src[:, :, tsl], in_=x2T[:, :, tsl]),
             r=[("x2Tout", t, fc) for fc in range(8)], dma=True)

    for u in range(len(units) + 1):
        if u < len(units):
            ex, t = units[u]
        prev = units[u - 1] if u >= 1 else None
        for jc in range(4):
            if u < len(units):
                g_step(ex, t, jc)
            if prev is not None:
                d_step(prev[0], prev[1], 2 * jc)
                d_step(prev[0], prev[1], 2 * jc + 1)
            if u < len(units) and ex + 1 < NE:
                prefetch_step(ex + 1, t * 4 + jc)
        if prev is not None and prev[0] == NE - 1:
            if prev[1] >= 1:
                final_apply(prev[1] - 1)
            final_stats(prev[1])

    final_apply(3)
    P.op("sp", None, after=[o["idx"] for o in P.ops if o["dma"]][-8:])
    P.emit(nc, es)
    es.close()
    return nc, list(dbg_out.keys())


_CACHE = {}


def _host_inputs(x, c, positions, rel_bias, ada_w, ada_b, norm1_g, w_in, lambda_q1, lambda_k1, lambda_q2, lambda_k2,
                 subln_g, conv_w, w_out, norm2_g, router_group_w, router_group_b, router_expert_w, router_expert_b,
                 expert_w_gate, expert_w_up, expert_w_down, final_g):
    f = lambda a: np.ascontiguousarray(np.asarray(a, dtype=np.float32))
    x = f(x)
    pk = lambda v: f(np.asarray(v, np.float32).reshape(-1, 128).T)
    shared = dict(
        adab=pk(ada_b[0]), ada_w=f(ada_w[0]), w_in=f(w_in[0]), w_out=f(w_out[0]),
        lam4=f(np.concatenate([np.asarray(lambda_q1[0]), np.asarray(lambda_k1[0]), np.asarray(lambda_q2[0]),
                               np.asarray(lambda_k2[0])]).reshape(1, 256)),
        rw=f(np.concatenate([np.asarray(router_group_w[0]), np.asarray(router_expert_w[0])], axis=1)),
        rb=f(np.concatenate([np.asarray(router_group_b[0]), np.asarray(router_expert_b[0])]).reshape(1, 20)),
        relb=f(rel_bias), relb31=f(np.asarray(rel_bias)[31].reshape(8, 1)),
        wg=f(expert_w_gate[0]), wu=f(expert_w_up[0]), wd=f(expert_w_down[0]),
        ident=np.eye(128, dtype=np.float32), antiid=np.ascontiguousarray(np.eye(128, dtype=np.float32)[::-1]),
        lo=np.asarray(LO, np.float32).reshape(32, 1),
    )
    cw = np.asarray(conv_w[0], np.float32)
    convw = cw.T.reshape(4, 128, 3).transpose(1, 0, 2).reshape(128, 12)
    in_maps = []
    xTs = [np.ascontiguousarray(x[b].T) for b in range(2)]
    for core in range(8):
        b, j = core // 4, core % 4
        xo = np.zeros((DM, NOWN, 130), np.float32)
        for i in range(NOWN):
            s = (4 * i + j) * 128
            if s == 0:
                xo[:, i, 2:] = xTs[b][:, 0:128]
            else:
                xo[:, i, :] = xTs[b][:, s - 2:s + 128]
        vecs = np.zeros((128, 48), np.float32)
        vecs[:, 0:8] = pk(np.asarray(c)[b])
        vecs[:, 8:16] = pk(norm1_g[0])
        vecs[:, 16:24] = pk(norm2_g[0])
        vecs[:, 24:32] = pk(final_g)
        vecs[:, 32:44] = convw
        vecs[:, 44] = 0.0 if j == 0 else 1.0
        vecs[:, 45] = np.asarray(subln_g[0], np.float32)
        dvec = (np.arange(768, dtype=np.float32) - 639.0 + (j + 1) * 128.0).reshape(1, 768)
        m = dict(shared)
        m.update(xT=xTs[b], xo=xo, vecs=vecs, dvec=dvec)
        in_maps.append(m)
    return in_maps


def kernel(**inputs):
    if "nc" not in _CACHE:
        _CACHE["nc"] = build_program()[0]
    nc = _CACHE["nc"]
    in_maps = _host_inputs(**inputs)
    res = run_bass_kernel_spmd(nc, in_maps, core_ids=list(range(8)))
    y = np.zeros((2, S, DM), np.float32)
    for core in range(8):
        b, j = core // 4, core % 4
        yT = np.asarray(res.results[core]["yT"])
        yv = yT.T.reshape(NOWN, 128, DM)
        for i in range(NOWN):
            s = (4 * i + j) * 128
            y[b, s:s + 128, :] = yv[i]
    return y
```
